# Optimizing a Trainium2 kernel written in Bass

```python
import jax, jax.numpy as jnp
from jax import lax
import numpy as np

D_MODEL = 1024
BATCH = 8
SEQ = 4096
DEPTH = 2

HEAD_DIM = 64
A_HEADS = D_MODEL // HEAD_DIM
A_CONFIGS = ((128, 1), (512, 4), (2048, 16))
N_A_GROUPS = len(A_CONFIGS)
B_Q_HEADS = D_MODEL // HEAD_DIM
B_KV_HEADS = 2
B_GROUP = B_Q_HEADS // B_KV_HEADS
B_WINDOW = 128
BLOCK = 128
D_FF = 2816
N_EXPERTS = 8
TOP_K = 2
D_EXPERT = 3584
PLE_DIM = 256
ROPE_THETA = 10000.0
EPS = 1e-6
NEG_INF = -1e30
N_A_LAYERS = (DEPTH + 1) // 2
N_B_LAYERS = DEPTH // 2
N_DENSE = (DEPTH + 1) // 2
N_MOE = DEPTH // 2

kernel_name = "yoco_dilated_swa_sink_moe_ple"


def _rmsnorm(x, g):
    xf = x.astype(jnp.float32)
    y = xf * lax.rsqrt(jnp.mean(xf * xf, axis=-1, keepdims=True) + EPS)
    return (y * g.astype(jnp.float32)).astype(x.dtype)


def _rope(t, pos):
    half = t.shape[-1] // 2
    inv = 1.0 / (ROPE_THETA ** (jnp.arange(half, dtype=jnp.float32) / half))
    ang = pos[:, None] * inv[None, :]
    cos = jnp.cos(ang)[:, None, :]
    sin = jnp.sin(ang)[:, None, :]
    tf = t.astype(jnp.float32)
    t1, t2 = tf[..., :half], tf[..., half:]
    return jnp.concatenate([t1 * cos - t2 * sin, t2 * cos + t1 * sin], axis=-1).astype(t.dtype)


def _band_keys(t, blk_axis):
    prev = lax.slice_in_dim(t, 0, t.shape[blk_axis] - 1, axis=blk_axis)
    pad = [(0, 0)] * t.ndim
    pad[blk_axis] = (1, 0)
    prev = jnp.pad(prev, pad)
    return jnp.concatenate([prev, t], axis=blk_axis + 1)


def _band_mask(nb, max_dist):
    qi = jnp.arange(BLOCK)[:, None] + BLOCK
    kj = jnp.arange(2 * BLOCK)[None, :]
    rel = qi - kj
    key_idx = jnp.arange(nb)[:, None, None] * BLOCK + kj[None] - BLOCK
    return (rel >= 0)[None] & (rel <= max_dist)[None] & (key_idx >= 0)


def _dilated_group(q, k, v, dil, steps):
    B, S, H, Dh = q.shape
    n = S // dil
    nb = -(-n // BLOCK)
    n_pad = nb * BLOCK

    def to_sub(t):
        t = jnp.moveaxis(t.reshape(B, n, dil, H, Dh), 2, 1)
        t = jnp.pad(t, ((0, 0), (0, 0), (0, n_pad - n), (0, 0), (0, 0)))
        return t.reshape(B, dil, nb, BLOCK, H, Dh)

    def from_sub(t):
        tail = t.shape[4:]
        t = t.reshape((B, dil, n_pad) + tail)[:, :, :n]
        return jnp.moveaxis(t, 1, 2).reshape((B, S) + tail)

    qs = to_sub(q)
    ks = _band_keys(to_sub(k), 2)
    vs = _band_keys(to_sub(v), 2)
    s = jnp.einsum('brnqhd,brnkhd->brnhqk', qs, ks,
                   preferred_element_type=jnp.float32) * (Dh ** -0.5)
    mask = _band_mask(nb, steps)[None, None, :, None]
    s = jnp.where(mask, s, NEG_INF)
    m = jnp.max(s, axis=-1, keepdims=True)
    pexp = jnp.exp(s - m)
    den = jnp.sum(pexp, axis=-1)
    o = jnp.einsum('brnhqk,brnkhd->brnqhd', pexp, vs.astype(jnp.float32))
    den_t = jnp.swapaxes(den, -1, -2)
    o = o / den_t[..., None]
    lse = jnp.swapaxes(m[..., 0] + jnp.log(den), -1, -2)
    return from_sub(o), from_sub(lse)


def _dilated_mixer(h, w_qkv, w_o, pos):
    B, S, _ = h.shape
    qkv = (h @ w_qkv).reshape(B, S, N_A_GROUPS, 3, A_HEADS, HEAD_DIM)
    outs, lses = [], []
    for g, (win, dil) in enumerate(A_CONFIGS):
        q = _rope(qkv[:, :, g, 0], pos)
        k = _rope(qkv[:, :, g, 1], pos)
        o, lse = _dilated_group(q, k, qkv[:, :, g, 2], dil, win // dil)
        outs.append(o)
        lses.append(lse)
    wts = jax.nn.softmax(jnp.stack(lses), axis=0)
    o = jnp.sum(wts[..., None] * jnp.stack(outs), axis=0)
    return o.reshape(B, S, A_HEADS * HEAD_DIM).astype(h.dtype) @ w_o


def _shared_kv(x, g, w_kv, pos):
    B, S, _ = x.shape
    kv = (_rmsnorm(x, g) @ w_kv).reshape(B, S, 2, B_KV_HEADS, HEAD_DIM)
    return _rope(kv[:, :, 0], pos), kv[:, :, 1]


def _swa_sink_mixer(h, k, v, w_q, sinks, w_o, pos):
    B, S, _ = h.shape
    nb = S // BLOCK
    q = _rope((h @ w_q).reshape(B, S, B_Q_HEADS, HEAD_DIM), pos)
    qb = q.reshape(B, nb, BLOCK, B_KV_HEADS, B_GROUP, HEAD_DIM)
    kb = _band_keys(k.reshape(B, nb, BLOCK, B_KV_HEADS, HEAD_DIM), 1)
    vb = _band_keys(v.reshape(B, nb, BLOCK, B_KV_HEADS, HEAD_DIM), 1)
    s = jnp.einsum('bnqhgd,bnkhd->bnhgqk', qb, kb,
                   preferred_element_type=jnp.float32) * (HEAD_DIM ** -0.5)
    mask = _band_mask(nb, B_WINDOW - 1)[None, :, None, None]
    s = jnp.where(mask, s, NEG_INF)
    sink = sinks.astype(jnp.float32).reshape(1, 1, B_KV_HEADS, B_GROUP, 1, 1)
    m = jnp.maximum(jnp.max(s, axis=-1, keepdims=True), sink)
    pexp = jnp.exp(s - m)
    den = jnp.sum(pexp, axis=-1, keepdims=True) + jnp.exp(sink - m)
    o = jnp.einsum('bnhgqk,bnkhd->bnqhgd', pexp / den, vb.astype(jnp.float32))
    return o.reshape(B, S, B_Q_HEADS * HEAD_DIM).astype(h.dtype) @ w_o


def _swiglu(h, w1, w3, w2):
    return (jax.nn.silu(h @ w1) * (h @ w3)) @ w2


def _moe(h, w_router, w1, w3, w2):
    logits = (h @ w_router).astype(jnp.float32)
    top_v, top_i = lax.top_k(logits, TOP_K)
    wts = jax.nn.softmax(top_v, axis=-1)
    gates = jnp.einsum('bske,bsk->bse',
                       jax.nn.one_hot(top_i, N_EXPERTS, dtype=jnp.float32), wts).astype(h.dtype)
    y = jnp.zeros_like(h)
    for e in range(N_EXPERTS):
        y = y + gates[..., e:e + 1] * _swiglu(h, w1[e], w3[e], w2[e])
    return y


def setup_inputs(seed: int = 0) -> dict:
    key = jax.random.key(seed)
    ks = iter(jax.random.split(key, 32))
    f32 = jnp.float32

    def w(shape, fan_in):
        return jax.random.normal(next(ks), shape, f32) * (fan_in ** -0.5)

    def gain(shape):
        return 1.0 + 0.05 * jax.random.normal(next(ks), shape, f32)

    qkv_cols = N_A_GROUPS * 3 * A_HEADS * HEAD_DIM
    return {
        "x": jax.random.normal(next(ks), (BATCH, SEQ, D_MODEL), f32),
        "p": jax.random.normal(next(ks), (DEPTH, BATCH, SEQ, PLE_DIM), f32),
        "attn_norm": gain((DEPTH, D_MODEL)),
        "ffn_norm": gain((DEPTH, D_MODEL)),
        "a_w_qkv": w((N_A_LAYERS, D_MODEL, qkv_cols), D_MODEL),
        "a_w_o": w((N_A_LAYERS, A_HEADS * HEAD_DIM, D_MODEL), A_HEADS * HEAD_DIM),
        "kv_norm": gain((D_MODEL,)),
        "kv_w": w((D_MODEL, 2 * B_KV_HEADS * HEAD_DIM), D_MODEL),
        "b_w_q": w((N_B_LAYERS, D_MODEL, B_Q_HEADS * HEAD_DIM), D_MODEL),
        "b_sinks": jax.random.normal(next(ks), (N_B_LAYERS, B_Q_HEADS), f32),
        "b_w_o": w((N_B_LAYERS, B_Q_HEADS * HEAD_DIM, D_MODEL), B_Q_HEADS * HEAD_DIM),
        "dense_w1": w((N_DENSE, D_MODEL, D_FF), D_MODEL),
        "dense_w3": w((N_DENSE, D_MODEL, D_FF), D_MODEL),
        "dense_w2": w((N_DENSE, D_FF, D_MODEL), D_FF),
        "moe_router": w((N_MOE, D_MODEL, N_EXPERTS), D_MODEL),
        "moe_w1": w((N_MOE, N_EXPERTS, D_MODEL, D_EXPERT), D_MODEL),
        "moe_w3": w((N_MOE, N_EXPERTS, D_MODEL, D_EXPERT), D_MODEL),
        "moe_w2": w((N_MOE, N_EXPERTS, D_EXPERT, D_MODEL), D_EXPERT),
        "ple_norm": gain((DEPTH, D_MODEL)),
        "ple_w_gate": w((DEPTH, D_MODEL, D_MODEL), D_MODEL),
        "ple_w_proj": w((DEPTH, PLE_DIM, D_MODEL), PLE_DIM),
        "final_norm": gain((D_MODEL,)),
    }


def reference(x, p, attn_norm, ffn_norm, a_w_qkv, a_w_o, kv_norm, kv_w, b_w_q, b_sinks,
              b_w_o, dense_w1, dense_w3, dense_w2, moe_router, moe_w1, moe_w3, moe_w2,
              ple_norm, ple_w_gate, ple_w_proj, final_norm):
    S = x.shape[1]
    pos = jnp.arange(S, dtype=jnp.float32)
    k_shared = v_shared = None
    for i in range(DEPTH):
        h = _rmsnorm(x, attn_norm[i])
        if i < N_A_LAYERS:
            x = x + _dilated_mixer(h, a_w_qkv[i], a_w_o[i], pos)
        else:
            j = i - N_A_LAYERS
            if j == 0:
                k_shared, v_shared = _shared_kv(x, kv_norm, kv_w, pos)
                h = _rmsnorm(x, attn_norm[i])
            x = x + _swa_sink_mixer(h, k_shared, v_shared, b_w_q[j], b_sinks[j], b_w_o[j], pos)
        h = _rmsnorm(x, ffn_norm[i])
        if i % 2 == 0:
            x = x + _swiglu(h, dense_w1[i // 2], dense_w3[i // 2], dense_w2[i // 2])
        else:
            x = x + _moe(h, moe_router[i // 2], moe_w1[i // 2], moe_w3[i // 2], moe_w2[i // 2])
        gate = jax.nn.sigmoid(_rmsnorm(x, ple_norm[i]) @ ple_w_gate[i])
        x = x + (p[i].astype(x.dtype) @ ple_w_proj[i]) * gate
    return _rmsnorm(x, final_norm)
```

```python
import numpy as np
from contextlib import ExitStack
import concourse.bass as bass
import concourse.mybir as mybir
from concourse.bass_utils import run_bass_kernel_spmd

F32 = mybir.dt.float32
BF16 = mybir.dt.bfloat16
AF = mybir.ActivationFunctionType
ALU = mybir.AluOpType
AX = mybir.AxisListType

T = 4096
D = 1024
NT = T // 128
DFF = 2816
DEXP = 3584
NEXP = 8
EPS = 1e-6
COMPUTE = ("pe", "act", "dve", "pool")
EPOCH = 20000
A_CONFIGS = ((128, 1), (512, 4), (2048, 16))


class _Op:
    __slots__ = ("eng", "reads", "writes", "dma", "semkey", "waits", "sig", "idx", "fence")

    def __init__(self, eng, reads, writes, dma, semkey):
        self.eng = eng
        self.reads = reads
        self.writes = writes
        self.dma = dma
        self.semkey = semkey
        self.waits = None
        self.sig = None
        self.fence = False


class Prog:
    def __init__(self):
        self.meta = []
        self.mode = "record"
        self.count = 0
        self.cur = []
        self.sems = None
        self.phase_map = {}

    def add(self, eng, fn, reads=(), writes=(), dma=False, semkey=None):
        if self.mode == "record":
            if dma:
                pm = self.phase_map.setdefault(eng, {})
                semkey = ("dq", eng, pm.setdefault(semkey, len(pm)))
            op = _Op(eng, tuple(reads), tuple(writes), dma, semkey)
            op.idx = len(self.meta)
            self.meta.append(op)
        else:
            op = self.meta[self.count]
            assert op.eng == eng and op.dma == dma, (op.idx, op.eng, eng)
            self.cur.append((op, fn))
        self.count += 1

    def pe(self, fn, reads=(), writes=()):
        self.add("pe", fn, reads, writes)

    def act(self, fn, reads=(), writes=()):
        self.add("act", fn, reads, writes)

    def dve(self, fn, reads=(), writes=()):
        self.add("dve", fn, reads, writes)

    def pool(self, fn, reads=(), writes=()):
        self.add("pool", fn, reads, writes)

    def dma(self, q, fn, reads=(), writes=(), semkey=None):
        assert semkey is not None
        self.add(q, fn, reads, writes, dma=True, semkey=semkey)

    def fence(self):
        if self.mode == "record":
            op = _Op(None, (), (), False, None)
            op.fence = True
            op.idx = len(self.meta)
            self.meta.append(op)
            self.phase_map = {}
        self.count += 1

    def analyze(self):
        ops = self.meta
        last_w, readers = {}, {}
        last_dma_on_sem, dma_count = {}, {}
        last_on_eng = {}
        pending_fence = {}
        needs_sig = [False] * len(ops)
        deps_list = [None] * len(ops)
        for op in ops:
            i = op.idx
            if op.fence:
                fd = set(last_on_eng.values()) | set(last_dma_on_sem.values())
                for e in COMPUTE + ("sp",):
                    pending_fence[e] = set(fd)
                deps_list[i] = []
                continue
            raw, other = set(), set()
            xr = tuple(r for r in op.reads if isinstance(r, tuple) and r[0] == "PS" and r not in op.writes)
            for r in op.reads:
                w = last_w.get(r)
                if w is not None:
                    raw.add(w)
            for w_ in op.writes + xr:
                w = last_w.get(w_)
                if w is not None:
                    other.add(w)
                for rd in readers.get(w_, ()):
                    other.add(rd)
            if op.eng in pending_fence:
                raw |= pending_fence.pop(op.eng)
            if op.dma:
                p = last_dma_on_sem.get(op.semkey)
                if p is not None:
                    raw.add(p)
                last_dma_on_sem[op.semkey] = i
                dma_count[op.semkey] = dma_count.get(op.semkey, 0) + 1
                op.sig = ("d", op.semkey, dma_count[op.semkey] * 16)
            else:
                last_on_eng[op.eng] = i
            deps = set()
            for d in raw | other:
                if d == i:
                    continue
                dop = ops[d]
                if dop.dma:
                    deps.add(d)
                    continue
                if (not op.dma) and dop.eng == op.eng:
                    if op.eng != "pe":
                        deps.add(d)
                    continue
                deps.add(d)
            best, final = {}, []
            for d in deps:
                dop = ops[d]
                if dop.dma:
                    final.append(d)
                elif dop.eng not in best or best[dop.eng] < d:
                    best[dop.eng] = d
            final.extend(best.values())
            for d in final:
                needs_sig[d] = True
            deps_list[i] = final
            for r in op.reads:
                readers.setdefault(r, []).append(i)
            for w_ in op.writes + xr:
                last_w[w_] = i
                readers[w_] = []
        seq = {e: 0 for e in COMPUTE + ("sp",)}
        for op in ops:
            if op.fence or op.dma:
                continue
            if needs_sig[op.idx]:
                seq[op.eng] += 1
                s = seq[op.eng]
                op.sig = ("e", op.eng, (s - 1) // EPOCH, s - ((s - 1) // EPOCH) * EPOCH)
        known = {}
        for op in ops:
            if op.fence:
                continue
            ws = []
            kn = known.setdefault(op.eng, {})
            for d in deps_list[op.idx]:
                sg = ops[d].sig
                if sg[0] == "d":
                    key, val = ("d", sg[1]), sg[2]
                else:
                    key, val = ("e", sg[1], sg[2]), sg[3]
                if kn.get(key, 0) >= val:
                    continue
                kn[key] = val
                ws.append((key, val))
            op.waits = ws
        self.n_epochs = {e: (seq[e] + EPOCH - 1) // EPOCH for e in seq}
        self.semkeys = list(dma_count.keys())

    def alloc_sems(self, nc, es):
        sems = {}
        for e, n in self.n_epochs.items():
            for k in range(n):
                sems[("e", e, k)] = es.enter_context(nc.semaphore(f"s_{e}_{k}"))
        for j, sk in enumerate(self.semkeys):
            sems[("d", sk)] = es.enter_context(nc.semaphore(f"d_{j}"))
        self.sems = sems

    def flush(self, nc):
        if self.mode != "emit" or not self.cur:
            self.cur = []
            return
        per = {}
        for op, fn in self.cur:
            per.setdefault(op.eng, []).append((op, fn))
        self.cur = []
        sems = self.sems

        def run(engobj, lst):
            for op, fn in lst:
                for key, val in op.waits:
                    engobj.wait_ge(sems[key], val)
                ins = fn(engobj)
                sg = op.sig
                if sg is not None:
                    if sg[0] == "d":
                        ins.then_inc(sems[("d", sg[1])], 16)
                    else:
                        ins.then_inc(sems[("e", sg[1], sg[2])], 1)

        with nc.Block() as block:
            if "pe" in per:
                @block.tensor
                def _(t):
                    run(t, per["pe"])
            if "act" in per:
                @block.scalar
                def _(a):
                    run(a, per["act"])
            if "dve" in per:
                @block.vector
                def _(v):
                    run(v, per["dve"])
            if "pool" in per:
                @block.gpsimd
                def _(g):
                    run(g, per["pool"])
            if "sp" in per:
                @block.sync
                def _(s):
                    run(s, per["sp"])


_UID = [0]


def _sbt(nc, name, shape, dt):
    _UID[0] += 1
    return nc.sbuf_tensor(f"{name}_u{_UID[0]}", shape, dt)


def _pst(nc, name, shape, dt):
    _UID[0] += 1
    return nc.psum_tensor(f"{name}_u{_UID[0]}", shape, dt)


def tokview(ap2d, dil):
    if dil == 1:
        return ap2d.unsqueeze(1)
    return ap2d.rearrange("p (m d) -> p d m", d=dil)


class Ctx:
    pass


def declare_dram(nc):
    g = Ctx()
    ei = lambda n, s: nc.dram_tensor(n, s, F32, kind="ExternalInput").ap()
    g.x = ei("x", [T, D])
    g.pT = ei("pT", [2, 256, T])
    g.wqkv = ei("wqkv", [D, 9216])
    g.wo0 = ei("wo0", [D, D])
    g.kvw = ei("kvw", [D, 256])
    g.wq1 = ei("wq1", [D, D])
    g.wo1 = ei("wo1", [D, D])
    g.w1d = ei("w1d", [D, DFF])
    g.w3d = ei("w3d", [D, DFF])
    g.w2d = ei("w2d", [DFF, D])
    g.wr = ei("wr", [D, NEXP])
    g.w1m = ei("w1m", [NEXP, D, DEXP])
    g.w3m = ei("w3m", [NEXP, D, DEXP])
    g.w2m = ei("w2m", [NEXP, DEXP, D])
    g.wg = ei("wg", [2, D, D])
    g.wp = ei("wp", [2, 256, D])
    g.gT = ei("gT", [128, 7 * 8])
    g.fgain = ei("fgain", [128, D])
    g.sinks = ei("sinks", [128, 16])
    g.ropeC = ei("ropeC", [128, T])
    g.ropeS = ei("ropeS", [128, T])
    g.masks = ei("masks", [2, 128, 256])
    g.ident = ei("ident", [128, 128])
    g.pswap = ei("pswap", [128, 128])
    g.out = nc.dram_tensor("out", [T, D], F32, kind="ExternalOutput").ap()
    g.xs = nc.dram_tensor("xs", [T, D], F32).ap()
    g.oT = nc.dram_tensor("oTs", [8, 128, T], BF16).ap()
    g.hsc = nc.dram_tensor("hsc", [128, 8, T], BF16).ap()
    g.hkv = nc.dram_tensor("hkv", [128, 8, T], BF16).ap()
    g.hat = nc.dram_tensor("hat", [128, 8, T], BF16).ap()
    return g


class NormUnit:
    def __init__(self, nc, es, P, tag, idb, gT, nslots=2):
        sb = lambda n, s, d: es.enter_context(_sbt(nc, f"{tag}_{n}", s, d))
        self.P = P
        self.tag = tag
        self.idb = idb
        self.gT = gT
        self.ns = nslots
        self.junk = sb("junk", [128, D], BF16)
        self.ss = sb("ss", [128, nslots], F32)
        self.rr = sb("rr", [128, nslots], F32)
        self.eps = sb("eps", [128, 1], F32)
        self.xn = [sb(f"xn{i}", [128, D], BF16) for i in range(nslots)]
        self.pt = [es.enter_context(_pst(nc, f"{tag}_pt{i}", [128, 8, 128], BF16)) for i in range(2)]
        self.n = 0
        self.pn = 0
        P.pool(lambda e: e.memset(self.eps[:], EPS), writes=[(tag, "eps")])

    def stats(self, xt, xkey):
        P, tag = self.P, self.tag
        s = self.n % self.ns
        self.n += 1
        P.act(lambda e: e.activation(out=self.junk[:], in_=xt, func=AF.Square, accum_out=self.ss[:, s:s + 1]),
              reads=[xkey], writes=[(tag, "junk"), (tag, "ss", s)])
        P.act(lambda e: e.activation(out=self.rr[:, s:s + 1], in_=self.ss[:, s:s + 1], func=AF.Sqrt,
                                     scale=1.0 / D, bias=self.eps[:]),
              reads=[(tag, "ss", s), (tag, "eps")], writes=[(tag, "rr", s)])
        P.dve(lambda e: e.reciprocal(self.rr[:, s:s + 1], self.rr[:, s:s + 1]),
              reads=[(tag, "rr", s)], writes=[(tag, "rr", s)])
        return s

    def run_a(self, xt, xkey):
        P, tag = self.P, self.tag
        s = self.stats(xt, xkey)
        xn = self.xn[s]
        P.dve(lambda e: e.tensor_scalar(xn[:], xt, self.rr[:, s:s + 1], None, ALU.mult),
              reads=[xkey, (tag, "rr", s)], writes=[(tag, "xn", s)])
        return s

    def run_b(self, s, outs):
        P, tag = self.P, self.tag
        xn = self.xn[s]
        ps = self.pn % len(self.pt)
        self.pn += 1
        pt = self.pt[ps]
        for c in range(8):
            P.pe(lambda e, c=c: e.transpose(pt[:, c, :], xn[:, c * 128:(c + 1) * 128], self.idb[:]),
                 reads=[(tag, "xn", s), "idb"], writes=[("PS", tag, "pt", ps)])
        for gi, oap, okey in outs:
            P.dve(lambda e, gi=gi, oap=oap: e.tensor_tensor(
                oap, pt[:], self.gT[:, gi * 8:(gi + 1) * 8].unsqueeze(2).to_broadcast([128, 8, 128]), ALU.mult),
                reads=[("PS", tag, "pt", ps), "gT"], writes=[okey])
        return ps

    def run(self, xt, xkey, outs):
        s = self.run_a(xt, xkey)
        self.run_b(s, outs)
        return s


def load_consts(nc, es, P, g):
    sb = lambda n, s, d: es.enter_context(_sbt(nc, n, s, d))
    c = Ctx()
    c.idb = sb("idb", [128, 128], BF16)
    c.psw = sb("pswb", [128, 128], BF16)
    c.gT = sb("gTs", [128, 56], F32)
    c.masks = sb("masksb", [128, 2, 256], BF16)
    c.gates = sb("gates", [128, NT, NEXP], F32)
    P.dma("pool", lambda e: e.dma_start(out=c.idb[:], in_=g.ident[:, :]), writes=["idb"], semkey="c_idb")
    P.dma("pool", lambda e: e.dma_start(out=c.psw[:], in_=g.pswap[:, :]), writes=["psw"], semkey="c_psw")
    P.dma("sp", lambda e: e.dma_start(out=c.gT[:], in_=g.gT[:, :]), writes=["gT"], semkey="c_gT")
    for l in range(2):
        P.dma("pool", lambda e, l=l: e.dma_start(out=c.masks[:, l, :], in_=g.masks[l, :, :]),
              writes=["masks"], semkey=("c_mask", l))
    return c


def rope_proj(P, nc, B, tag, nslab, mm_fn, mm_reads, Ctab, Stab, out_fn, out_key, psw, idb, col0=0, pre=None):
    L = 1

    def stage1(s):
        sl = s % 2
        cs = slice(col0 + s * 512, col0 + (s + 1) * 512)
        hx = pre(s) if pre is not None else None
        for c in range(8):
            if pre is not None:
                P.pe(lambda e, s=s, c=c, sl=sl, hx=hx: mm_fn(e, s, c, B.pq[sl][:, :], hx), reads=mm_reads(s, hx),
                     writes=[("PS", "bk", sl)])
            else:
                P.pe(lambda e, s=s, c=c, sl=sl: mm_fn(e, s, c, B.pq[sl][:, :]), reads=mm_reads(s), writes=[("PS", "bk", sl)])
        P.dve(lambda e, sl=sl, cs=cs: e.tensor_tensor(B.ua[sl][:], B.pq[sl][:, :], Ctab[:, cs], ALU.mult),
              reads=[("PS", "bk", sl), "ropeC"], writes=[("ua", sl)])
        P.dve(lambda e, sl=sl, cs=cs: e.tensor_tensor(B.ub[sl][:], B.pq[sl][:, :], Stab[:, cs], ALU.mult),
              reads=[("PS", "bk", sl), "ropeS"], writes=[("ub", sl)])

    def stage2(s):
        sl = s % 2
        P.pe(lambda e, sl=sl: e.matmul(B.psw[sl][:, :], lhsT=idb[:], rhs=B.ua[sl][:], start=True, stop=False),
             reads=[("ua", sl), "idb"], writes=[("PS", "bk", 2 + sl)])
        P.pe(lambda e, sl=sl: e.matmul(B.psw[sl][:, :], lhsT=psw[:], rhs=B.ub[sl][:], start=False, stop=True),
             reads=[("ub", sl), "psw"], writes=[("PS", "bk", 2 + sl)])
        for oap, rows in out_fn(s):
            P.act(lambda e, sl=sl, oap=oap, rows=rows: e.copy(oap, B.psw[sl][rows, :]), reads=[("PS", "bk", 2 + sl)],
                  writes=[out_key])

    for i in range(nslab + L):
        if i < nslab:
            stage1(i)
        if i >= L:
            stage2(i - L)


def attention(P, nc, B, tag, dil, kt_fn, qt_fn, v_fn, kv_reads, q_reads, mask, evac_fn, idb, LOOK=3):
    nsub = T // dil
    nb = nsub // 128
    blocks = [(r, n) for r in range(dil) for n in range(nb)]

    def s1(idx):
        r, n = blocks[idx]
        nq = 256 if n < nb - 1 else 128
        sl = idx % 4
        stv = B.st[sl][:, :].rearrange("p (h q) -> p h q", h=2)[:, :, 0:nq]
        ptv = B.pt[sl][:, :, 0:nq]
        if nq == 256:
            P.pe(lambda e: e.matmul(stv, lhsT=kt_fn(r, 128 * n, 128), rhs=qt_fn(r, 128 * n, nq),
                                    start=True, stop=True), reads=kv_reads + q_reads, writes=[("PS", "bk", sl)])
        else:
            for hh in range(2):
                P.pe(lambda e, hh=hh: e.matmul(stv[:, hh, :], lhsT=kt_fn(r, 128 * n, 128), rhs=qt_fn(r, 128 * n, nq)[:, hh, :],
                                               start=True, stop=True), reads=kv_reads + q_reads, writes=[("PS", "bk", sl)])
        P.act(lambda e: e.activation(out=ptv, in_=stv, func=AF.Exp, scale=0.125),
              reads=[("PS", "bk", sl)], writes=[("ptile", sl)])
        P.dve(lambda e: e.tensor_tensor(ptv, ptv, mask[:, 0:nq].unsqueeze(1).to_broadcast([128, 2, nq]), ALU.mult),
              reads=[("ptile", sl), "masks"], writes=[("ptile", sl)])

    def s2(idx):
        r, n = blocks[idx]
        b = r * nb + n
        sl = idx % 4
        bank, pos = (b // 4) % 2, b % 4
        for hh in range(2):
            kb = 4 + hh * 2 + bank
            P.pe(lambda e, hh=hh, kb=kb: e.matmul(B.bk[kb][:, pos * 128:(pos + 1) * 128], lhsT=v_fn(hh, b),
                                                  rhs=B.pt[sl][:, hh, 0:128], start=(n == 0), stop=True),
                 reads=kv_reads + [("ptile", sl)], writes=[("PS", "bk", kb)])
            if n < nb - 1:
                b2 = b + 1
                bank2, pos2 = (b2 // 4) % 2, b2 % 4
                kb2 = 4 + hh * 2 + bank2
                P.pe(lambda e, hh=hh, kb2=kb2, pos2=pos2: e.matmul(
                    B.bk[kb2][:, pos2 * 128:(pos2 + 1) * 128], lhsT=v_fn(hh, b), rhs=B.pt[sl][:, hh, 128:256],
                    start=True, stop=False), reads=kv_reads + [("ptile", sl)], writes=[("PS", "bk", kb2)])
            if pos == 3:
                evac_fn(hh, b // 4, B.bk[kb], ("PS", "bk", kb))

    nblk = len(blocks)
    for i in range(nblk + LOOK):
        if i < nblk:
            s1(i)
        if i >= LOOK:
            s2(i - LOOK)


def v_from_vt(P, B, dil, VTs, vkey):
    nb = T // dil // 128
    vv = tokview(VTs[:, :], dil)
    for b in range(32):
        r, n = b // nb, b % nb
        half, pos = (b // 4) % 2, b % 4
        P.pe(lambda e, r=r, n=n, half=half, pos=pos: e.transpose(
            B.vtr[half][:, pos, :], vv[:, r, 128 * n:128 * (n + 1)], B.idb[:]),
            reads=[vkey, "idb"], writes=[("PS", "bk", 2 + half)])
        if pos == 3:
            b0 = b - 3
            P.dve(lambda e, b0=b0, half=half: e.tensor_copy(
                B.V[:, b0:b0 + 4, :, 0:64], B.vtr[half].rearrange("p b (h d) -> p b h d", h=2)),
                reads=[("PS", "bk", 2 + half)], writes=["V"])


def attn_buffers(nc, es, tag):
    sb = lambda n, s, d: es.enter_context(_sbt(nc, f"{tag}_{n}", s, d))
    ps = lambda n, s, d: es.enter_context(_pst(nc, f"{tag}_{n}", s, d))
    B = Ctx()
    bk = [ps(f"bk{i}", [128, 512], F32) for i in range(8)]
    B.bk = bk
    B.pq = [bk[0], bk[1]]
    B.psw = [bk[2], bk[3]]
    B.st = [bk[i] for i in range(4)]
    B.vtr = [bk[2 + i][:, :].bitcast(BF16)[:, 0:512].rearrange("p (b d) -> p b d", b=4) for i in range(2)]
    B.ua = [sb(f"ua{i}", [128, 512], BF16) for i in range(2)]
    B.ub = [sb(f"ub{i}", [128, 512], BF16) for i in range(2)]
    B.pt = [sb(f"ptl{i}", [128, 2, 256], BF16) for i in range(4)]
    B.ropeC = sb("ropeC", [128, T], BF16)
    B.ropeS = sb("ropeS", [128, T], BF16)
    B.V = sb("V", [128, 32, 2, 128], BF16)
    return B


def load_rope(P, g, B):
    for h in range(2):
        cs = slice(h * 2048, (h + 1) * 2048)
        P.dma("pool", lambda e, cs=cs: e.dma_start(out=B.ropeC[:, cs], in_=g.ropeC[:, cs]), writes=["ropeC"],
              semkey=("ropeC", h))
        P.dma("pool", lambda e, cs=cs: e.dma_start(out=B.ropeS[:, cs], in_=g.ropeS[:, cs]), writes=["ropeS"],
              semkey=("ropeS", h))


def phase_A(P, nc, g, C):
    with ExitStack() as es:
        sb = lambda n, s, d: es.enter_context(_sbt(nc, n, s, d))
        hT0 = sb("A_hT0", [128, 8, T], BF16)
        with ExitStack() as es2:
            sb2 = lambda n, s, d: es2.enter_context(_sbt(nc, n, s, d))
            xt = [sb2(f"A_xt{i}", [128, D], F32) for i in range(3)]
            NU = NormUnit(nc, es2, P, "An", C.idb, C.gT)
            for t in range(NT):
                s = t % 3
                P.dma("sp", lambda e, t=t, s=s: e.dma_start(out=xt[s][:], in_=g.x[t * 128:(t + 1) * 128, :]),
                      writes=[("A_xt", s)], semkey=("A_xt", s))
                NU.run(xt[s][:], ("A_xt", s), [(0, hT0[:, :, t * 128:(t + 1) * 128], "hT0")])
            P.fence()
            P.flush(nc)
        B = attn_buffers(nc, es, "A")
        B.idb = C.idb
        load_rope(P, g, B)
        wq = [sb(f"A_wq{i}", [128, 8, 384], BF16) for i in range(2)]
        QTd = sb("A_QTd", [128, 2, T], BF16)
        KT = sb("A_KT", [128, T], BF16)
        VTs = sb("A_VTs", [128, T], BF16)
        P.pool(lambda e: e.memset(QTd[:], 0.0), writes=["QT"])
        acc = [sb(f"A_acc{i}", [128, T], F32) for i in range(2)]
        rec = sb("A_rec", [128, 512], F32)
        lnd = sb("A_lnd", [128, 512], F32)
        obf = sb("A_obf", [128, T], BF16)
        P.pool(lambda e: e.memset(B.V[:, :, :, 64:128], 1.0), writes=["V"])
        wv_ = g.wqkv.rearrange("(c p) n -> p c n", p=128)
        it = 0
        for pair in range(STOP.get("a_pairs", 8)):
            for gi, (win, dil) in enumerate(A_CONFIGS):
                ws = it % 2
                it += 1
                if it > STOP.get("a_iters", 99):
                    continue
                for k in range(3):
                    c0 = gi * 3072 + k * 1024 + pair * 128
                    P.dma("pool", lambda e, ws=ws, k=k, c0=c0: e.dma_start(
                        out=wq[ws][:, :, k * 128:(k + 1) * 128], in_=wv_[:, :, c0:c0 + 128]),
                        writes=[("wq", ws)], semkey=("wq", ws, k))
                nb = T // dil // 128
                rope_proj(P, nc, B, "A", 8,
                          lambda e, s, c, o, ws=ws: e.matmul(o, lhsT=wq[ws][:, c, 0:128], rhs=hT0[:, c, s * 512:(s + 1) * 512],
                                                             start=(c == 0), stop=(c == 7)),
                          lambda s, ws=ws: [("wq", ws), "hT0"], B.ropeC, B.ropeS,
                          lambda s: [(QTd[0:64, 0, s * 512:(s + 1) * 512], slice(0, 64)),
                                     (QTd[64:128, 1, s * 512:(s + 1) * 512], slice(64, 128))], "QT", C.psw, C.idb)
                rope_proj(P, nc, B, "A", 8,
                          lambda e, s, c, o, ws=ws: e.matmul(o, lhsT=wq[ws][:, c, 128:256], rhs=hT0[:, c, s * 512:(s + 1) * 512],
                                                             start=(c == 0), stop=(c == 7)),
                          lambda s, ws=ws: [("wq", ws), "hT0"], B.ropeC, B.ropeS,
                          lambda s: [(KT[:, s * 512:(s + 1) * 512], slice(0, 128))], "KT", C.psw, C.idb)
                if STOP.get("a_part", 9) < 1:
                    continue
                for sv in range(8):
                    sl = sv % 2
                    for c in range(8):
                        P.pe(lambda e, c=c, sv=sv, sl=sl, ws=ws: e.matmul(
                            B.pq[sl][:, :], lhsT=wq[ws][:, c, 256:384], rhs=hT0[:, c, sv * 512:(sv + 1) * 512],
                            start=(c == 0), stop=(c == 7)), reads=[("wq", ws), "hT0"], writes=[("PS", "bk", sl)])
                    P.act(lambda e, sv=sv, sl=sl: e.copy(VTs[:, sv * 512:(sv + 1) * 512], B.pq[sl][:, :]),
                          reads=[("PS", "bk", sl)], writes=["VTs"])
                v_from_vt(P, B, dil, VTs, "VTs")
                if STOP.get("a_part", 9) < 2:
                    continue
                def evac(hh, k, pvb, pvkey, gi=gi, dil=dil, nb=nb):
                    accv = tokview(acc[hh][:, :], dil)
                    if nb >= 4:
                        r, n0 = (4 * k) // nb, (4 * k) % nb
                        dst = accv[:, r, 128 * n0:128 * n0 + 512]
                        src = pvb[:, :]
                    else:
                        dst = accv[:, 2 * k:2 * k + 2, 0:256]
                        src = pvb[:, :].rearrange("p (a b) -> p a b", a=2)
                    if gi == 0:
                        P.dve(lambda e: e.tensor_copy(dst, src), reads=[pvkey], writes=[("acc", hh)])
                    else:
                        P.dve(lambda e: e.tensor_tensor(dst, src, dst, ALU.add), reads=[pvkey, ("acc", hh)],
                              writes=[("acc", hh)])

                if dil == 1:
                    qtf = lambda r, n0, cnt: QTd[:, :, n0:n0 + cnt]
                else:
                    qtf = lambda r, n0, cnt, dil=dil: QTd[:, :, :].rearrange("p h (m d) -> p h d m", d=dil)[:, :, r, n0:n0 + cnt]
                attention(P, nc, B, "A", dil,
                          lambda r, n0, cnt, dil=dil: tokview(KT[:, :], dil)[:, r, n0:n0 + cnt],
                          qtf, lambda hh, b: B.V[:, b, hh, :],
                          ["KT", "V"], ["QT"], C.masks[:, 0, :], evac, C.idb)
            for hh in range(2):
                for s in range(8):
                    cs = slice(s * 512, (s + 1) * 512)
                    P.act(lambda e, hh=hh, cs=cs: e.activation(out=lnd[64:128, :], in_=acc[hh][64:128, cs], func=AF.Ln),
                          reads=[("acc", hh)], writes=["lnd"])
                    P.act(lambda e: e.activation(out=rec[0:64, :], in_=lnd[64:128, :], func=AF.Exp, scale=-1.0),
                          reads=["lnd"], writes=["rec"])
                    P.dve(lambda e, hh=hh, cs=cs: e.tensor_tensor(obf[hh * 64:(hh + 1) * 64, cs], acc[hh][0:64, cs],
                                                                   rec[0:64, :], ALU.mult),
                           reads=[("acc", hh), "rec"], writes=["obf"])
            P.dma("sp", lambda e, pair=pair: e.dma_start(out=g.oT[pair, :, :], in_=obf[:]), reads=["obf"],
                  writes=[("oT", pair)], semkey="obf_st")
        P.fence()
        P.flush(nc)


def phase_B(P, nc, g, C, wo_dram, x_src, gain_idx, h_dst, router=False):
    tag = "B%d" % gain_idx
    with ExitStack() as es:
        sb = lambda n, s, d: es.enter_context(_sbt(nc, f"{tag}_{n}", s, d))
        ps = lambda n, s, d: es.enter_context(_pst(nc, f"{tag}_{n}", s, d))
        wo = sb("wo", [128, 8, D], BF16)
        og = [sb(f"og{i}", [128, 8, 1024], BF16) for i in range(2)]
        xt = [sb(f"xt{i}", [128, D], F32) for i in range(3)]
        x1 = [sb(f"x1{i}", [128, D], F32) for i in range(3)]
        hg = [sb(f"hg{i}", [128, 8, 1024], BF16) for i in range(2)]
        py = [ps(f"py{i}", [128, D], F32) for i in range(2)]
        NU = NormUnit(nc, es, P, tag + "n", C.idb, C.gT, nslots=3)
        wov = wo_dram.rearrange("(c p) n -> p c n", p=128)
        for h in range(2):
            P.dma("pool", lambda e, h=h: e.dma_start(out=wo[:, :, h * 512:(h + 1) * 512], in_=wov[:, :, h * 512:(h + 1) * 512]),
                  writes=[(tag, "wo")], semkey=(tag, "wo", h))
        if router:
            wr32 = sb("wr32", [128, 8, NEXP], F32)
            wrg = sb("wrg", [128, 8, NEXP], F32)
            wrh = sb("wrh", [128, 8, NEXP], BF16)
            wrh32 = sb("wrh32", [128, 8, NEXP], F32)
            wrl = sb("wrl", [128, 8, NEXP], BF16)
            xlo = [sb(f"xlo{i}", [128, D], BF16) for i in range(2)]
            hiT = [sb(f"hiT{i}", [128, 8, 128], BF16) for i in range(2)]
            loT = [sb(f"loT{i}", [128, 8, 128], BF16) for i in range(2)]
            plo = ps("plo", [128, 8, 128], BF16)
            plg = ps("plg", [128, NEXP], F32)
            lga = sb("lga", [128, NT, NEXP], F32)
            l2a = sb("l2a", [128, NT, NEXP], F32)
            q1 = sb("q1", [128, NT, NEXP], F32)
            q2 = sb("q2", [128, NT, NEXP], F32)
            m1 = sb("m1", [128, NT], F32)
            m2 = sb("m2", [128, NT], F32)
            ee = sb("ee", [128, NT], F32)
            P.dma("sp", lambda e: e.dma_start(out=wr32[:], in_=g.wr.rearrange("(c p) n -> p c n", p=128)),
                  writes=["wr32"], semkey="wr32")
            P.dve(lambda e: e.tensor_tensor(wrg[:], wr32[:], C.gT[:, gain_idx * 8:(gain_idx + 1) * 8].unsqueeze(2).to_broadcast([128, 8, NEXP]), ALU.mult),
                  reads=["wr32", "gT"], writes=["wrg"])
            P.dve(lambda e: e.tensor_copy(wrh[:], wrg[:]), reads=["wrg"], writes=["wrh"])
            P.dve(lambda e: e.tensor_copy(wrh32[:], wrh[:]), reads=["wrh"], writes=["wrh32"])
            P.dve(lambda e: e.tensor_tensor(wrl[:], wrg[:], wrh32[:], ALU.subtract), reads=["wrg", "wrh32"], writes=["wrl"])
        slots = {}

        def stage1(t):
            gq, tt = t // 8, t % 8
            gs = gq % 2
            if tt == 0:
                P.dma("sp", lambda e: e.dma_start(
                    out=og[gs][:], in_=g.oT[:, :, gq * 1024:(gq + 1) * 1024].rearrange("c p t -> p c t")),
                    reads=[("oT", c) for c in range(8)], writes=[(tag, "og", gs)], semkey=(tag, "og", gs))
            s = t % 3
            P.dma("sp", lambda e: e.dma_start(out=xt[s][:], in_=x_src[t * 128:(t + 1) * 128, :]),
                  reads=[("xs", t)], writes=[(tag, "xt", s)], semkey=(tag, "xt", s))
            pb = t % 2
            for half in range(2):
                for c in range(8):
                    P.pe(lambda e, c=c, half=half: e.matmul(
                        py[pb][:, half * 512:(half + 1) * 512], lhsT=og[gs][:, c, tt * 128:(tt + 1) * 128],
                        rhs=wo[:, c, half * 512:(half + 1) * 512], start=(c == 0), stop=(c == 7)),
                        reads=[(tag, "og", gs), (tag, "wo")], writes=[("PS", tag, "py", pb)])
            P.dve(lambda e: e.tensor_tensor(x1[s][:], py[pb][:, :], xt[s][:], ALU.add),
                  reads=[("PS", tag, "py", pb), (tag, "xt", s)], writes=[(tag, "x1", s)])
            P.dma("sp", lambda e: e.dma_start(out=g.xs[t * 128:(t + 1) * 128, :], in_=x1[s][:]),
                  reads=[(tag, "x1", s)], writes=[("xs", t)], semkey=(tag, "x1st", s))
            slots[t] = NU.run_a(x1[s][:], (tag, "x1", s))

        def stage2(t):
            gq, tt = t // 8, t % 8
            gs = gq % 2
            s = t % 3
            ns = slots[t]
            ps_ = NU.run_b(ns, [(gain_idx, hg[gs][:, :, tt * 128:(tt + 1) * 128], (tag, "hg", gs))])
            if router:
                rs = t % 2
                ntag = tag + "n"
                P.act(lambda e: e.copy(hiT[rs][:], NU.pt[ps_][:]), reads=[("PS", ntag, "pt", ps_)], writes=[("hiT", rs)])
                P.dve(lambda e: e.scalar_tensor_tensor(
                    out=xlo[rs][:], in0=x1[s][:], scalar=NU.rr[:, ns:ns + 1], in1=NU.xn[ns][:], op0=ALU.mult, op1=ALU.subtract),
                    reads=[(tag, "x1", s), (ntag, "rr", ns), (ntag, "xn", ns)], writes=[("xlo", rs)])
                for c in range(8):
                    P.pe(lambda e, c=c: e.transpose(plo[:, c, :], xlo[rs][:, c * 128:(c + 1) * 128], C.idb[:]),
                         reads=[("xlo", rs), "idb"], writes=[("PS", "plo")])
                P.act(lambda e: e.copy(loT[rs][:], plo[:]), reads=[("PS", "plo")], writes=[("loT", rs)])
                k = 0
                for (aT, akey, wmat, wkey) in ((hiT, "hiT", wrh, "wrh"), (loT, "loT", wrh, "wrh"), (hiT, "hiT", wrl, "wrl")):
                    for c in range(8):
                        P.pe(lambda e, c=c, aT=aT, wmat=wmat, k=k: e.matmul(
                            plg[:, :], lhsT=aT[rs][:, c, :], rhs=wmat[:, c, :], start=(k == 0), stop=(k == 23)),
                            reads=[(akey, rs), wkey], writes=[("PS", "plg")])
                        k += 1
                P.dve(lambda e: e.tensor_copy(lga[:, t, :], plg[:, :]), reads=[("PS", "plg")], writes=["lga"])
            if tt == 7:
                P.dma("sp", lambda e: e.dma_start(out=h_dst[:, :, gq * 1024:(gq + 1) * 1024], in_=hg[gs][:]),
                      reads=[(tag, "hg", gs)], writes=[("hsc", gq)], semkey=(tag, "hgst", gs))

        for i in range(NT + 1):
            if i < NT:
                stage1(i)
            if i >= 1:
                stage2(i - 1)
        if router:
            bc = lambda ap: ap.unsqueeze(2).to_broadcast([128, NT, NEXP])
            P.dve(lambda e: e.reduce_max(m1[:], lga[:], axis=AX.X), reads=["lga"], writes=["m1"])
            P.dve(lambda e: e.tensor_tensor(q1[:], lga[:], bc(m1[:]), ALU.is_equal), reads=["lga", "m1"], writes=["q1"])
            P.dve(lambda e: e.scalar_tensor_tensor(out=l2a[:], in0=q1[:], scalar=-1e30, in1=lga[:], op0=ALU.mult, op1=ALU.add),
                  reads=["q1", "lga"], writes=["l2a"])
            P.dve(lambda e: e.reduce_max(m2[:], l2a[:], axis=AX.X), reads=["l2a"], writes=["m2"])
            P.dve(lambda e: e.tensor_tensor(q2[:], l2a[:], bc(m2[:]), ALU.is_equal), reads=["l2a", "m2"], writes=["q2"])
            P.dve(lambda e: e.tensor_tensor(m2[:], m2[:], m1[:], ALU.subtract), reads=["m1", "m2"], writes=["m2"])
            P.act(lambda e: e.activation(out=ee[:], in_=m2[:], func=AF.Exp), reads=["m2"], writes=["ee"])
            P.dve(lambda e: e.tensor_scalar(m1[:], ee[:], 1.0, None, ALU.add), reads=["ee"], writes=["m1"])
            P.dve(lambda e: e.reciprocal(m1[:], m1[:]), reads=["m1"], writes=["m1"])
            P.dve(lambda e: e.tensor_tensor(ee[:], ee[:], m1[:], ALU.mult), reads=["ee", "m1"], writes=["ee"])
            P.dve(lambda e: e.tensor_tensor(q1[:], q1[:], bc(m1[:]), ALU.mult), reads=["q1", "m1"], writes=["q1"])
            P.dve(lambda e: e.tensor_tensor(q2[:], q2[:], bc(ee[:]), ALU.mult), reads=["q2", "ee"], writes=["q2"])
            P.dve(lambda e: e.tensor_tensor(C.gates[:], q1[:], q2[:], ALU.add), reads=["q1", "q2"], writes=["gates"])
        P.fence()
        P.flush(nc)


def ffn_stage(P, nc, tag, hTg, hkey, nchunk, wsrc1, wsrc3, wb1, wb3, pa, pb, sa, actT, wit):
    nblk = (nchunk + 3) // 4
    cnt = 0
    for fb in range(nblk):
        ncols = min(512, nchunk * 128 - fb * 512)
        ws = wit["w"] % 2
        wit["w"] += 1
        P.dma("pool", lambda e, fb=fb, ncols=ncols, ws=ws: e.dma_start(out=wb1[ws][:, :, 0:ncols], in_=wsrc1(fb * 512, ncols)),
              writes=[(tag, "wb1", ws)], semkey=(tag, "wb1", ws))
        P.dma("pool", lambda e, fb=fb, ncols=ncols, ws=ws: e.dma_start(out=wb3[ws][:, :, 0:ncols], in_=wsrc3(fb * 512, ncols)),
              writes=[(tag, "wb3", ws)], semkey=(tag, "wb3", ws))
        for jj in range(ncols // 128):
            j = fb * 4 + jj
            for th in range(2):
                sl = wit["p"] % 2
                wit["p"] += 1
                ts_ = slice(th * 512, (th + 1) * 512)
                for c in range(8):
                    P.pe(lambda e, c=c, jj=jj, ws=ws, sl=sl, ts_=ts_: e.matmul(
                        pa[sl][:, :], lhsT=wb1[ws][:, c, jj * 128:(jj + 1) * 128], rhs=hTg[:, c, ts_],
                        start=(c == 0), stop=(c == 7)), reads=[(tag, "wb1", ws), hkey], writes=[("PS", tag, "pa", sl)])
                for c in range(8):
                    P.pe(lambda e, c=c, jj=jj, ws=ws, sl=sl, ts_=ts_: e.matmul(
                        pb[sl][:, :], lhsT=wb3[ws][:, c, jj * 128:(jj + 1) * 128], rhs=hTg[:, c, ts_],
                        start=(c == 0), stop=(c == 7)), reads=[(tag, "wb3", ws), hkey], writes=[("PS", tag, "pb", sl)])
                P.act(lambda e, sl=sl: e.activation(out=sa[sl][:], in_=pa[sl][:, :], func=AF.Silu),
                      reads=[("PS", tag, "pa", sl)], writes=[(tag, "sa", sl)])
                P.dve(lambda e, sl=sl, j=j, ts_=ts_: e.tensor_tensor(actT[:, j, ts_], sa[sl][:], pb[sl][:, :], ALU.mult),
                      reads=[(tag, "sa", sl), ("PS", tag, "pb", sl)], writes=[(tag, "actT")])


def phase_C1(P, nc, g, C):
    tag = "C1"
    NCH = DFF // 128
    with ExitStack() as es:
        sb = lambda n, s, d: es.enter_context(_sbt(nc, f"{tag}_{n}", s, d))
        ps = lambda n, s, d: es.enter_context(_pst(nc, f"{tag}_{n}", s, d))
        w2 = sb("w2", [128, NCH, D], BF16)
        hTg = [sb(f"hTg{i}", [128, 8, 1024], BF16) for i in range(2)]
        actT = sb("actT", [128, NCH, 1024], BF16)
        wb1 = [sb(f"wb1{i}", [128, 8, 512], BF16) for i in range(2)]
        wb3 = [sb(f"wb3{i}", [128, 8, 512], BF16) for i in range(2)]
        sa = [sb(f"sa{i}", [128, 512], BF16) for i in range(2)]
        xt = [sb(f"xt{i}", [128, D], F32) for i in range(3)]
        pa = [ps(f"pa{i}", [128, 512], F32) for i in range(2)]
        pb = [ps(f"pb{i}", [128, 512], F32) for i in range(2)]
        py = [ps(f"py{i}", [128, D], F32) for i in range(2)]
        w2v = g.w2d.rearrange("(j p) n -> p j n", p=128)
        for q in range(0, NCH, 4):
            n = min(4, NCH - q)
            P.dma("pool", lambda e, q=q, n=n: e.dma_start(out=w2[:, q:q + n, :], in_=w2v[:, q:q + n, :]),
                  writes=[(tag, "w2")], semkey=(tag, "w2", q))
        w1v = g.w1d.rearrange("(c p) n -> p c n", p=128)
        w3v = g.w3d.rearrange("(c p) n -> p c n", p=128)
        wit = {"w": 0, "p": 0}
        for gq in range(4):
            gs = gq % 2
            P.dma("sp", lambda e, gq=gq, gs=gs: e.dma_start(out=hTg[gs][:], in_=g.hsc[:, :, gq * 1024:(gq + 1) * 1024]),
                  reads=[("hsc", gq)], writes=[(tag, "hTg", gs)], semkey=(tag, "hTg", gs))
            ffn_stage(P, nc, tag, hTg[gs], (tag, "hTg", gs), NCH,
                      lambda c0, n: w1v[:, :, c0:c0 + n], lambda c0, n: w3v[:, :, c0:c0 + n],
                      wb1, wb3, pa, pb, sa, actT, wit)
            for tt in range(8):
                t = gq * 8 + tt
                s = t % 3
                pbk = t % 2
                P.dma("sp", lambda e, t=t, s=s: e.dma_start(out=xt[s][:], in_=g.xs[t * 128:(t + 1) * 128, :]),
                      reads=[("xs", t)], writes=[(tag, "xt", s)], semkey=(tag, "xt", s))
                for half in range(2):
                    for j in range(NCH):
                        P.pe(lambda e, j=j, half=half, tt=tt, pbk=pbk: e.matmul(
                            py[pbk][:, half * 512:(half + 1) * 512], lhsT=actT[:, j, tt * 128:(tt + 1) * 128],
                            rhs=w2[:, j, half * 512:(half + 1) * 512], start=(j == 0), stop=(j == NCH - 1)),
                            reads=[(tag, "actT"), (tag, "w2")], writes=[("PS", tag, "py", pbk)])
                P.dve(lambda e, s=s, pbk=pbk: e.tensor_tensor(xt[s][:], py[pbk][:, :], xt[s][:], ALU.add),
                      reads=[("PS", tag, "py", pbk), (tag, "xt", s)], writes=[(tag, "xt", s)])
                P.dma("sp", lambda e, t=t, s=s: e.dma_start(out=g.xs[t * 128:(t + 1) * 128, :], in_=xt[s][:]),
                      reads=[(tag, "xt", s)], writes=[("xs", t)], semkey=(tag, "xst", s))
        P.fence()
        P.flush(nc)


def phase_PLE(P, nc, g, C, layer, gain_idx, final):
    tag = "E%d" % layer
    with ExitStack() as es:
        sb = lambda n, s, d: es.enter_context(_sbt(nc, f"{tag}_{n}", s, d))
        ps = lambda n, s, d: es.enter_context(_pst(nc, f"{tag}_{n}", s, d))
        wg = sb("wg", [128, 8, D], BF16)
        wp = sb("wp", [128, 2, D], BF16)
        pTg = [sb(f"pTg{i}", [128, 2, 1024], BF16) for i in range(2)]
        xt = [sb(f"xt{i}", [128, D], F32) for i in range(4)]
        gTt = [sb(f"gTt{i}", [128, 8, 128], BF16) for i in range(3)]
        sg = [sb(f"sg{i}", [128, 512], F32) for i in range(2)]
        pg = [ps(f"pg{i}", [128, 512], F32) for i in range(2)]
        pp = [ps(f"pp{i}", [128, 512], F32) for i in range(2)]
        NU = NormUnit(nc, es, P, tag + "n", C.idb, C.gT, nslots=3)
        NU2 = NormUnit(nc, es, P, tag + "m", C.idb, C.gT, nslots=3)
        if final:
            fg = sb("fg", [128, D], F32)
            ot = [sb(f"ot{i}", [128, D], F32) for i in range(2)]
            P.dma("sp", lambda e: e.dma_start(out=fg[:], in_=g.fgain[:, :]), writes=[(tag, "fg")], semkey=(tag, "fg"))
        else:
            hk = [sb(f"hk{i}", [128, 8, 1024], BF16) for i in range(2)]
            ha = [sb(f"ha{i}", [128, 8, 1024], BF16) for i in range(2)]
        wgv = g.wg[layer].rearrange("(c p) n -> p c n", p=128)
        for h in range(2):
            P.dma("pool", lambda e, h=h: e.dma_start(out=wg[:, :, h * 512:(h + 1) * 512], in_=wgv[:, :, h * 512:(h + 1) * 512]),
                  writes=[(tag, "wg")], semkey=(tag, "wg", h))
        P.dma("pool", lambda e: e.dma_start(out=wp[:], in_=g.wp[layer].rearrange("(c p) n -> p c n", p=128)),
              writes=[(tag, "wp")], semkey=(tag, "wp"))
        sl1, sl2 = {}, {}
        hcnt = {"n": 0}

        def s1(t):
            gq, tt = t // 8, t % 8
            gs = gq % 2
            if tt == 0:
                P.dma("pool", lambda e: e.dma_start(
                    out=pTg[gs][:], in_=g.pT[layer, :, gq * 1024:(gq + 1) * 1024].rearrange("(c p) t -> p c t", p=128)),
                    writes=[(tag, "pTg", gs)], semkey=(tag, "pTg", gs))
            s = t % 4
            P.dma("sp", lambda e: e.dma_start(out=xt[s][:], in_=g.xs[t * 128:(t + 1) * 128, :]),
                  reads=[("xs", t)], writes=[(tag, "xt", s)], semkey=(tag, "xt", s))
            sl1[t] = NU.run_a(xt[s][:], (tag, "xt", s))

        def s2(t):
            s3_ = t % 3
            NU.run_b(sl1[t], [(gain_idx, gTt[s3_][:], (tag, "gTt", s3_))])

        def s3(t):
            gq, tt = t // 8, t % 8
            gs = gq % 2
            s = t % 4
            s3_ = t % 3
            for half in range(2):
                hs_ = slice(half * 512, (half + 1) * 512)
                k = hcnt["n"] % 2
                hcnt["n"] += 1
                for c in range(8):
                    P.pe(lambda e, c=c, hs_=hs_, k=k: e.matmul(pg[k][:, :], lhsT=gTt[s3_][:, c, :], rhs=wg[:, c, hs_],
                                                              start=(c == 0), stop=(c == 7)),
                         reads=[(tag, "gTt", s3_), (tag, "wg")], writes=[("PS", tag, "pg", k)])
                for c in range(2):
                    P.pe(lambda e, c=c, hs_=hs_, k=k: e.matmul(pp[k][:, :], lhsT=pTg[gs][:, c, tt * 128:(tt + 1) * 128],
                                                              rhs=wp[:, c, hs_], start=(c == 0), stop=(c == 1)),
                         reads=[(tag, "pTg", gs), (tag, "wp")], writes=[("PS", tag, "pp", k)])
                P.act(lambda e, k=k: e.activation(out=sg[k][:], in_=pg[k][:, :], func=AF.Sigmoid),
                      reads=[("PS", tag, "pg", k)], writes=[(tag, "sg", k)])
                P.dve(lambda e, k=k: e.tensor_tensor(sg[k][:], sg[k][:], pp[k][:, :], ALU.mult),
                      reads=[(tag, "sg", k), ("PS", tag, "pp", k)], writes=[(tag, "sg", k)])
                P.dve(lambda e, k=k, hs_=hs_: e.tensor_tensor(xt[s][:, hs_], xt[s][:, hs_], sg[k][:], ALU.add),
                      reads=[(tag, "xt", s), (tag, "sg", k)], writes=[(tag, "xt", s)])
            xkey = (tag, "xt", s)
            if final:
                s2_ = t % 2
                ns = NU2.stats(xt[s][:], xkey)
                P.dve(lambda e: e.scalar_tensor_tensor(out=ot[s2_][:], in0=xt[s][:], scalar=NU2.rr[:, ns:ns + 1], in1=fg[:],
                                                       op0=ALU.mult, op1=ALU.mult),
                      reads=[xkey, (tag + "m", "rr", ns), (tag, "fg")], writes=[(tag, "ot", s2_)])
                P.dma("sp", lambda e: e.dma_start(out=g.out[t * 128:(t + 1) * 128, :], in_=ot[s2_][:]),
                      reads=[(tag, "ot", s2_)], writes=[("out", t)], semkey=(tag, "ost", s2_))
            else:
                P.dma("sp", lambda e: e.dma_start(out=g.xs[t * 128:(t + 1) * 128, :], in_=xt[s][:]),
                      reads=[xkey], writes=[("xs", t)], semkey=(tag, "xst", s))
                sl2[t] = NU2.run_a(xt[s][:], xkey)

        def s4(t):
            if final:
                return
            gq, tt = t // 8, t % 8
            gs = gq % 2
            NU2.run_b(sl2[t], [(3, hk[gs][:, :, tt * 128:(tt + 1) * 128], (tag, "hk", gs)),
                               (4, ha[gs][:, :, tt * 128:(tt + 1) * 128], (tag, "ha", gs))])
            if tt == 7:
                cs = slice(gq * 1024, (gq + 1) * 1024)
                P.dma("sp", lambda e: e.dma_start(out=g.hkv[:, :, cs], in_=hk[gs][:]), reads=[(tag, "hk", gs)],
                      writes=[("hkv", gq)], semkey=(tag, "hkst", gs))
                P.dma("sp", lambda e: e.dma_start(out=g.hat[:, :, cs], in_=ha[gs][:]), reads=[(tag, "ha", gs)],
                      writes=[("hat", gq)], semkey=(tag, "hast", gs))

        for i in range(NT + 3):
            if i < NT:
                s1(i)
            if 0 <= i - 1 < NT:
                s2(i - 1)
            if 0 <= i - 2 < NT:
                s3(i - 2)
            if 0 <= i - 3 < NT:
                s4(i - 3)
        P.fence()
        P.flush(nc)


def phase_D(P, nc, g, C):
    tag = "D"
    with ExitStack() as es:
        sb = lambda n, s, d: es.enter_context(_sbt(nc, f"{tag}_{n}", s, d))
        B = attn_buffers(nc, es, "D")
        B.idb = C.idb
        load_rope(P, g, B)
        wk2 = sb("wk2", [128, 8, 2, 128], BF16)
        wv = sb("wv", [128, 8, 128], BF16)
        wq = sb("wq", [128, 8, D], BF16)
        KT2 = [sb(f"KT{i}", [128, T], BF16) for i in range(2)]
        QTd = [sb(f"QTd{i}", [128, 2, T], BF16) for i in range(2)]
        VTs = sb("VTs", [128, T], BF16)
        for i in range(2):
            P.pool(lambda e, i=i: e.memset(QTd[i][:], 0.0), writes=[("QT", i)])
        obf = [sb(f"obf{i}", [128, T], BF16) for i in range(2)]
        hs = [sb(f"hs{i}", [128, 8, 512], BF16) for i in range(3)]
        esk = sb("esk", [128, 16], F32)
        tmp = sb("tmp", [128, 512], F32)
        rec = sb("rec", [128, 512], F32)
        kvv = g.kvw.rearrange("(c p) n -> p c n", p=128)
        for kvh in range(2):
            for dup in range(2):
                P.dma("pool", lambda e, kvh=kvh, dup=dup: e.dma_start(
                    out=wk2[:, :, kvh, dup * 64:(dup + 1) * 64], in_=kvv[:, :, kvh * 64:(kvh + 1) * 64]),
                    writes=["wk2"], semkey=("wk2", kvh, dup))
        P.dma("pool", lambda e: e.dma_start(out=wv[:], in_=kvv[:, :, 128:256]), writes=["wv"], semkey="wv")
        wqv = g.wq1.rearrange("(c p) n -> p c n", p=128)
        for h in range(2):
            P.dma("pool", lambda e, h=h: e.dma_start(out=wq[:, :, h * 512:(h + 1) * 512], in_=wqv[:, :, h * 512:(h + 1) * 512]),
                  writes=["wq1"], semkey=("wq1", h))
        P.dma("sp", lambda e: e.dma_start(out=esk[:], in_=g.sinks[:, :]), writes=["esk"], semkey="esk")
        P.act(lambda e: e.activation(out=esk[:], in_=esk[:], func=AF.Exp), reads=["esk"], writes=["esk"])
        P.pool(lambda e: e.memset(B.V[:, :, :, 64:128], 1.0), writes=["V"])
        hst = {"cnt": 0, "slot": {}}

        def mk_pre(src, srckey):
            def pre(s_):
                sl = hst["cnt"] % 3
                hst["cnt"] += 1
                hst["slot"][s_] = sl
                P.dma("sp", lambda e: e.dma_start(out=hs[sl][:], in_=src[:, :, s_ * 512:(s_ + 1) * 512]),
                      reads=[(srckey, s_ // 2)], writes=[("hs", sl)], semkey=("hs", sl))
                return sl
            return pre

        for kvh in range(2):
            rope_proj(P, nc, B, "D", 8,
                      lambda e, s_, c, o, hx, kvh=kvh: e.matmul(o, lhsT=wk2[:, c, kvh, :], rhs=hs[hx][:, c, :],
                                                                start=(c == 0), stop=(c == 7)),
                      lambda s_, hx: ["wk2", ("hs", hx)], B.ropeC, B.ropeS,
                      lambda s_, kvh=kvh: [(KT2[kvh][:, s_ * 512:(s_ + 1) * 512], slice(0, 128))], ("KT", kvh), C.psw, C.idb,
                      pre=mk_pre(g.hkv, "hkv"))
        prev = mk_pre(g.hkv, "hkv")
        for sv in range(8):
            hx = prev(sv)
            sl = sv % 2
            for c in range(8):
                P.pe(lambda e, c=c, sl=sl, hx=hx: e.matmul(B.pq[sl][:, :], lhsT=wv[:, c, :], rhs=hs[hx][:, c, :],
                                                          start=(c == 0), stop=(c == 7)),
                     reads=[("hs", hx), "wv"], writes=[("PS", "bk", sl)])
            P.act(lambda e, sv=sv, sl=sl: e.copy(VTs[:, sv * 512:(sv + 1) * 512], B.pq[sl][:, :]),
                  reads=[("PS", "bk", sl)], writes=["VTs"])
        v_from_vt(P, B, 1, VTs, "VTs")
        for pair in range(8):
            qs = pair % 2
            rope_proj(P, nc, B, "D", 8,
                      lambda e, s_, c, o, hx, pair=pair: e.matmul(o, lhsT=wq[:, c, pair * 128:(pair + 1) * 128],
                                                                  rhs=hs[hx][:, c, :], start=(c == 0), stop=(c == 7)),
                      lambda s_, hx: ["wq1", ("hs", hx)], B.ropeC, B.ropeS,
                      lambda s_, qs=qs: [(QTd[qs][0:64, 0, s_ * 512:(s_ + 1) * 512], slice(0, 64)),
                                         (QTd[qs][64:128, 1, s_ * 512:(s_ + 1) * 512], slice(64, 128))], ("QT", qs), C.psw, C.idb,
                      pre=mk_pre(g.hat, "hat"))
            kvh = pair // 4

            def evac(hh, k, pvb, pvkey, pair=pair, qs=qs):
                head = pair * 2 + hh
                cs = slice(k * 512, (k + 1) * 512)
                P.act(lambda e: e.activation(out=tmp[64:128, :], in_=pvb[64:128, :], func=AF.Ln, bias=esk[64:128, head:head + 1]),
                      reads=[pvkey, "esk"], writes=["tmp"])
                P.act(lambda e: e.activation(out=rec[0:64, :], in_=tmp[64:128, :], func=AF.Exp, scale=-1.0),
                      reads=["tmp"], writes=["rec"])
                P.dve(lambda e: e.tensor_tensor(obf[qs][hh * 64:(hh + 1) * 64, cs], pvb[0:64, :], rec[0:64, :], ALU.mult),
                      reads=[pvkey, "rec"], writes=[("obf", qs)])

            attention(P, nc, B, "D", 1,
                      lambda r, n0, cnt, kvh=kvh: KT2[kvh][:, n0:n0 + cnt],
                      lambda r, n0, cnt, qs=qs: QTd[qs][:, :, n0:n0 + cnt],
                      lambda hh, b, kvh=kvh: B.V[:, b, kvh, :],
                      [("KT", kvh), "V"], [("QT", qs)], C.masks[:, 1, :], evac, C.idb)
            P.dma("sp", lambda e, pair=pair, qs=qs: e.dma_start(out=g.oT[pair, :, :], in_=obf[qs][:]), reads=[("obf", qs)],
                  writes=[("oT", pair)], semkey=("obf_st1", qs))
        P.fence()
        P.flush(nc)


def phase_F1(P, nc, g, C):
    tag = "F1"
    NH = 14
    with ExitStack() as es:
        sb = lambda n, s, d: es.enter_context(_sbt(nc, f"{tag}_{n}", s, d))
        ps = lambda n, s, d: es.enter_context(_pst(nc, f"{tag}_{n}", s, d))
        xg = sb("xg", [128, 8, D], F32)
        hTg = [sb(f"hTg{i}", [128, 8, 1024], BF16) for i in range(2)]
        actT = sb("actT", [128, NH, 1024], BF16)
        w2h = [sb(f"w2h{i}", [128, NH, D], BF16) for i in range(2)]
        wb1 = [sb(f"wb1{i}", [128, 8, 512], BF16) for i in range(2)]
        wb3 = [sb(f"wb3{i}", [128, 8, 512], BF16) for i in range(2)]
        sa = [sb(f"sa{i}", [128, 512], BF16) for i in range(2)]
        pa = [ps(f"pa{i}", [128, 512], F32) for i in range(2)]
        pb = [ps(f"pb{i}", [128, 512], F32) for i in range(2)]
        py = [ps(f"py{i}", [128, D], F32) for i in range(2)]
        wit = {"w": 0, "p": 0}
        it = 0
        pyc = 0
        for gq in range(4):
            gs = gq % 2
            P.dma("sp", lambda e, gq=gq, gs=gs: e.dma_start(out=hTg[gs][:], in_=g.hsc[:, :, gq * 1024:(gq + 1) * 1024]),
                  reads=[("hsc", gq)], writes=[(tag, "hTg", gs)], semkey=(tag, "hTg", gs))
            for q in range(2):
                P.dma("sp", lambda e, gq=gq, q=q: e.dma_start(
                    out=xg[:, q * 4:(q + 1) * 4, :],
                    in_=g.xs[gq * 1024 + q * 512:gq * 1024 + (q + 1) * 512, :].rearrange("(t p) n -> p t n", p=128)),
                    reads=[("xs", gq * 8 + q * 4 + i) for i in range(4)], writes=[(tag, "xg")], semkey=(tag, "xg", q))
            for ex in range(NEXP):
                for hf in range(2):
                    w2s = it % 2
                    it += 1
                    f0 = hf * NH * 128
                    w2v = g.w2m[ex].rearrange("(j p) n -> p j n", p=128)
                    for q in range(0, NH, 7):
                        P.dma("pool", lambda e, q=q, w2s=w2s, w2v=w2v, hf=hf: e.dma_start(
                            out=w2h[w2s][:, q:q + 7, :], in_=w2v[:, hf * NH + q:hf * NH + q + 7, :]),
                            writes=[(tag, "w2h", w2s)], semkey=(tag, "w2h", w2s, q))
                    w1v = g.w1m[ex].rearrange("(c p) n -> p c n", p=128)
                    w3v = g.w3m[ex].rearrange("(c p) n -> p c n", p=128)
                    ffn_stage(P, nc, tag, hTg[gs], (tag, "hTg", gs), NH,
                              lambda c0, n, w1v=w1v, f0=f0: w1v[:, :, f0 + c0:f0 + c0 + n],
                              lambda c0, n, w3v=w3v, f0=f0: w3v[:, :, f0 + c0:f0 + c0 + n],
                              wb1, wb3, pa, pb, sa, actT, wit)
                    for tt in range(8):
                        t = gq * 8 + tt
                        pbk = pyc % 2
                        pyc += 1
                        for half in range(2):
                            for j in range(NH):
                                P.pe(lambda e, j=j, half=half, tt=tt, pbk=pbk, w2s=w2s: e.matmul(
                                    py[pbk][:, half * 512:(half + 1) * 512], lhsT=actT[:, j, tt * 128:(tt + 1) * 128],
                                    rhs=w2h[w2s][:, j, half * 512:(half + 1) * 512], start=(j == 0), stop=(j == NH - 1)),
                                    reads=[(tag, "actT"), (tag, "w2h", w2s)], writes=[("PS", tag, "py", pbk)])
                        P.dve(lambda e, tt=tt, t=t, pbk=pbk, ex=ex: e.scalar_tensor_tensor(
                            out=xg[:, tt, :], in0=py[pbk][:, :], scalar=C.gates[:, t, ex:ex + 1], in1=xg[:, tt, :],
                            op0=ALU.mult, op1=ALU.add), reads=[("PS", tag, "py", pbk), "gates", (tag, "xg")], writes=[(tag, "xg")])
            for q in range(2):
                P.dma("sp", lambda e, gq=gq, q=q: e.dma_start(
                    out=g.xs[gq * 1024 + q * 512:gq * 1024 + (q + 1) * 512, :].rearrange("(t p) n -> p t n", p=128),
                    in_=xg[:, q * 4:(q + 1) * 4, :]),
                    reads=[(tag, "xg")], writes=[("xs", gq * 8 + q * 4 + i) for i in range(4)], semkey=(tag, "xgst", q))
        P.fence()
        P.flush(nc)


STOP = {"n": 99}
import os, json
if os.environ.get("KSTOP"):
    STOP.update(json.loads(os.environ["KSTOP"]))


def program(P, nc, g):
    with ExitStack() as es:
        C = load_consts(nc, es, P, g)
        stages = [
            lambda: phase_A(P, nc, g, C),
            lambda: phase_B(P, nc, g, C, g.wo0, g.x, 1, g.hsc),
            lambda: phase_C1(P, nc, g, C),
            lambda: phase_PLE(P, nc, g, C, 0, 2, False),
            lambda: phase_D(P, nc, g, C),
            lambda: phase_B(P, nc, g, C, g.wo1, g.xs, 5, g.hsc, router=True),
            lambda: phase_F1(P, nc, g, C),
            lambda: phase_PLE(P, nc, g, C, 1, 6, True),
        ]
        nst = min(STOP["n"], len(stages))
        for st in stages[:nst]:
            st()
        if nst < len(stages):
            for q in range(4):
                P.dma("sp", lambda e, q=q: e.dma_start(out=g.out[q * 1024:(q + 1) * 1024, :], in_=g.xs[q * 1024:(q + 1) * 1024, :]),
                      reads=[("xs", q * 8 + i) for i in range(8)], writes=[("out", q * 8 + i) for i in range(8)],
                      semkey=("dbg", q))
        P.add("sp", lambda e: e.nop(), reads=[("out", t) for t in range(NT)])
        P.flush(nc)


def build_nc():
    nc = bass.Bass("TRN2", target_bir_lowering=False)
    g = declare_dram(nc)
    P = Prog()
    program(P, nc, g)
    P.analyze()
    P.mode = "emit"
    P.count = 0
    with ExitStack() as es:
        P.alloc_sems(nc, es)
        program(P, nc, g)
    return nc


def _consts():
    half = 32
    inv = (1.0 / (np.float32(10000.0) ** (np.arange(half, dtype=np.float32) / np.float32(half)))).astype(np.float32)
    pos = np.arange(T, dtype=np.float32)
    ang = (pos[:, None] * inv[None, :]).astype(np.float32)
    cos = np.cos(ang).astype(np.float32).T
    sin = np.sin(ang).astype(np.float32).T
    Ct = np.ascontiguousarray(np.tile(cos, (4, 1)))
    sign = np.where((np.arange(128) % 64) < 32, -1.0, 1.0).astype(np.float32)[:, None]
    St = np.ascontiguousarray(np.tile(sin, (4, 1)) * (-sign))
    k = np.arange(128)[:, None]
    q = np.arange(128)[None, :]
    m_a = (q >= k)
    masks = np.stack([np.concatenate([m_a, q <= k], 1), np.concatenate([m_a, q < k], 1)]).astype(np.float32)
    ident = np.eye(128, dtype=np.float32)
    m = np.arange(128)
    sw = np.where((m % 64) < 32, m + 32, m - 32)
    pswap = np.zeros((128, 128), np.float32)
    pswap[sw, m] = 1.0
    return Ct, St, masks, ident, pswap


def make_in_maps(inp):
    f = lambda a: np.ascontiguousarray(np.asarray(a, dtype=np.float32))
    Ct, St, masks, ident, pswap = _consts()
    gl = [inp["attn_norm"][0], inp["ffn_norm"][0], inp["ple_norm"][0], inp["kv_norm"], inp["attn_norm"][1],
          inp["ffn_norm"][1], inp["ple_norm"][1]]
    gT = np.concatenate([f(v).reshape(8, 128).T for v in gl], axis=1)
    shared = dict(
        wqkv=f(inp["a_w_qkv"][0]), wo0=f(inp["a_w_o"][0]), kvw=f(inp["kv_w"]), wq1=f(inp["b_w_q"][0]),
        wo1=f(inp["b_w_o"][0]), w1d=f(inp["dense_w1"][0]), w3d=f(inp["dense_w3"][0]), w2d=f(inp["dense_w2"][0]),
        wr=f(inp["moe_router"][0]), w1m=f(inp["moe_w1"][0]), w3m=f(inp["moe_w3"][0]), w2m=f(inp["moe_w2"][0]),
        wg=f(inp["ple_w_gate"]), wp=f(inp["ple_w_proj"]), gT=f(gT),
        fgain=f(np.broadcast_to(f(inp["final_norm"])[None, :], (128, D))),
        sinks=f(np.broadcast_to(f(inp["b_sinks"][0])[None, :], (128, 16))),
        ropeC=Ct, ropeS=St, masks=masks, ident=ident, pswap=pswap)
    x = np.asarray(inp["x"], dtype=np.float32)
    p = np.asarray(inp["p"], dtype=np.float32)
    maps = []
    for b in range(x.shape[0]):
        m = dict(shared)
        m["x"] = f(x[b])
        m["pT"] = f(np.transpose(p[:, b], (0, 2, 1)))
        maps.append(m)
    return maps


_NC_CACHE = {}


def kernel(**inputs):
    maps = make_in_maps(inputs)
    if "nc" not in _NC_CACHE:
        _NC_CACHE["nc"] = build_nc()
    nc = _NC_CACHE["nc"]
    res = run_bass_kernel_spmd(nc, maps, core_ids=list(range(len(maps))))
    return np.stack([np.asarray(r["out"], dtype=np.float32) for r in res.results], axis=0)
```

```python
import numpy as np
from contextlib import ExitStack
import concourse.bass as bass
import concourse.mybir as mybir
from concourse.bass_utils import run_bass_kernel_spmd

F32 = mybir.dt.float32
BF16 = mybir.dt.bfloat16
AF = mybir.ActivationFunctionType
ALU = mybir.AluOpType
AX = mybir.AxisListType

T = 4096
D = 1024
NT = T // 128
DFF = 2816
DEXP = 3584
NEXP = 8
EPS = 1e-6
COMPUTE = ("pe", "act", "dve", "pool")
EPOCH = 20000
A_CONFIGS = ((128, 1), (512, 4), (2048, 16))


class _Op:
    __slots__ = ("eng", "reads", "writes", "dma", "semkey", "waits", "sig", "idx", "fence")

    def __init__(self, eng, reads, writes, dma, semkey):
        self.eng = eng
        self.reads = reads
        self.writes = writes
        self.dma = dma
        self.semkey = semkey
        self.waits = None
        self.sig = None
        self.fence = False


class Prog:
    def __init__(self):
        self.meta = []
        self.mode = "record"
        self.count = 0
        self.cur = []
        self.sems = None
        self.phase_map = {}

    def add(self, eng, fn, reads=(), writes=(), dma=False, semkey=None):
        if self.mode == "record":
            if dma:
                pm = self.phase_map.setdefault(eng, {})
                semkey = ("dq", eng, pm.setdefault(semkey, len(pm)))
            op = _Op(eng, tuple(reads), tuple(writes), dma, semkey)
            op.idx = len(self.meta)
            self.meta.append(op)
        else:
            op = self.meta[self.count]
            assert op.eng == eng and op.dma == dma, (op.idx, op.eng, eng)
            self.cur.append((op, fn))
        self.count += 1

    def pe(self, fn, reads=(), writes=()):
        self.add("pe", fn, reads, writes)

    def act(self, fn, reads=(), writes=()):
        self.add("act", fn, reads, writes)

    def dve(self, fn, reads=(), writes=()):
        self.add("dve", fn, reads, writes)

    def pool(self, fn, reads=(), writes=()):
        self.add("pool", fn, reads, writes)

    def dma(self, q, fn, reads=(), writes=(), semkey=None):
        assert semkey is not None
        self.add(q, fn, reads, writes, dma=True, semkey=semkey)

    def fence(self):
        if self.mode == "record":
            op = _Op(None, (), (), False, None)
            op.fence = True
            op.idx = len(self.meta)
            self.meta.append(op)
            self.phase_map = {}
        self.count += 1

    def analyze(self):
        ops = self.meta
        last_w, readers = {}, {}
        last_dma_on_sem, dma_count = {}, {}
        last_on_eng = {}
        pending_fence = {}
        needs_sig = [False] * len(ops)
        deps_list = [None] * len(ops)
        for op in ops:
            i = op.idx
            if op.fence:
                fd = set(last_on_eng.values()) | set(last_dma_on_sem.values())
                for e in COMPUTE + ("sp",):
                    pending_fence[e] = set(fd)
                deps_list[i] = []
                continue
            raw, other = set(), set()
            xr = tuple(r for r in op.reads if isinstance(r, tuple) and r[0] == "PS" and r not in op.writes)
            for r in op.reads:
                w = last_w.get(r)
                if w is not None:
                    raw.add(w)
            for w_ in op.writes + xr:
                w = last_w.get(w_)
                if w is not None:
                    other.add(w)
                for rd in readers.get(w_, ()):
                    other.add(rd)
            if op.eng in pending_fence:
                raw |= pending_fence.pop(op.eng)
            if op.dma:
                p = last_dma_on_sem.get(op.semkey)
                if p is not None:
                    raw.add(p)
                last_dma_on_sem[op.semkey] = i
                dma_count[op.semkey] = dma_count.get(op.semkey, 0) + 1
                op.sig = ("d", op.semkey, dma_count[op.semkey] * 16)
            else:
                last_on_eng[op.eng] = i
            deps = set()
            for d in raw | other:
                if d == i:
                    continue
                dop = ops[d]
                if dop.dma:
                    deps.add(d)
                    continue
                if (not op.dma) and dop.eng == op.eng:
                    if op.eng != "pe":
                        deps.add(d)
                    continue
                deps.add(d)
            best, final = {}, []
            for d in deps:
                dop = ops[d]
                if dop.dma:
                    final.append(d)
                elif dop.eng not in best or best[dop.eng] < d:
                    best[dop.eng] = d
            final.extend(best.values())
            for d in final:
                needs_sig[d] = True
            deps_list[i] = final
            for r in op.reads:
                readers.setdefault(r, []).append(i)
            for w_ in op.writes + xr:
                last_w[w_] = i
                readers[w_] = []
        seq = {e: 0 for e in COMPUTE + ("sp",)}
        for op in ops:
            if op.fence or op.dma:
                continue
            if needs_sig[op.idx]:
                seq[op.eng] += 1
                s = seq[op.eng]
                op.sig = ("e", op.eng, (s - 1) // EPOCH, s - ((s - 1) // EPOCH) * EPOCH)
        known = {}
        for op in ops:
            if op.fence:
                continue
            ws = []
            kn = known.setdefault(op.eng, {})
            for d in deps_list[op.idx]:
                sg = ops[d].sig
                if sg[0] == "d":
                    key, val = ("d", sg[1]), sg[2]
                else:
                    key, val = ("e", sg[1], sg[2]), sg[3]
                if kn.get(key, 0) >= val:
                    continue
                kn[key] = val
                ws.append((key, val))
            op.waits = ws
        self.n_epochs = {e: (seq[e] + EPOCH - 1) // EPOCH for e in seq}
        self.semkeys = list(dma_count.keys())

    def alloc_sems(self, nc, es):
        sems = {}
        for e, n in self.n_epochs.items():
            for k in range(n):
                sems[("e", e, k)] = es.enter_context(nc.semaphore(f"s_{e}_{k}"))
        for j, sk in enumerate(self.semkeys):
            sems[("d", sk)] = es.enter_context(nc.semaphore(f"d_{j}"))
        self.sems = sems

    def flush(self, nc):
        if self.mode != "emit" or not self.cur:
            self.cur = []
            return
        per = {}
        for op, fn in self.cur:
            per.setdefault(op.eng, []).append((op, fn))
        self.cur = []
        sems = self.sems

        def run(engobj, lst):
            for op, fn in lst:
                for key, val in op.waits:
                    engobj.wait_ge(sems[key], val)
                ins = fn(engobj)
                sg = op.sig
                if sg is not None:
                    if sg[0] == "d":
                        ins.then_inc(sems[("d", sg[1])], 16)
                    else:
                        ins.then_inc(sems[("e", sg[1], sg[2])], 1)

        with nc.Block() as block:
            if "pe" in per:
                @block.tensor
                def _(t):
                    run(t, per["pe"])
            if "act" in per:
                @block.scalar
                def _(a):
                    run(a, per["act"])
            if "dve" in per:
                @block.vector
                def _(v):
                    run(v, per["dve"])
            if "pool" in per:
                @block.gpsimd
                def _(g):
                    run(g, per["pool"])
            if "sp" in per:
                @block.sync
                def _(s):
                    run(s, per["sp"])


_UID = [0]


def _sbt(nc, name, shape, dt):
    _UID[0] += 1
    return nc.sbuf_tensor(f"{name}_u{_UID[0]}", shape, dt)


def _pst(nc, name, shape, dt):
    _UID[0] += 1
    return nc.psum_tensor(f"{name}_u{_UID[0]}", shape, dt)


def tokview(ap2d, dil):
    if dil == 1:
        return ap2d.unsqueeze(1)
    return ap2d.rearrange("p (m d) -> p d m", d=dil)


class Ctx:
    pass


def declare_dram(nc):
    g = Ctx()
    ei = lambda n, s: nc.dram_tensor(n, s, F32, kind="ExternalInput").ap()
    g.x = ei("x", [T, D])
    g.pT = ei("pT", [2, 256, T])
    g.wqkv = ei("wqkv", [D, 9216])
    g.wo0 = ei("wo0", [D, D])
    g.kvw = ei("kvw", [D, 256])
    g.wq1 = ei("wq1", [D, D])
    g.wo1 = ei("wo1", [D, D])
    g.w1d = ei("w1d", [D, DFF])
    g.w3d = ei("w3d", [D, DFF])
    g.w2d = ei("w2d", [DFF, D])
    g.wr = ei("wr", [D, NEXP])
    g.w1m = ei("w1m", [NEXP, D, DEXP])
    g.w3m = ei("w3m", [NEXP, D, DEXP])
    g.w2m = ei("w2m", [NEXP, DEXP, D])
    g.wg = ei("wg", [2, D, D])
    g.wp = ei("wp", [2, 256, D])
    g.gT = ei("gT", [128, 7 * 8])
    g.fgain = ei("fgain", [128, D])
    g.sinks = ei("sinks", [128, 16])
    g.ropeC = ei("ropeC", [128, T])
    g.ropeS = ei("ropeS", [128, T])
    g.masks = ei("masks", [2, 128, 256])
    g.ident = ei("ident", [128, 128])
    g.pswap = ei("pswap", [128, 128])
    g.out = nc.dram_tensor("out", [T, D], F32, kind="ExternalOutput").ap()
    g.xs = nc.dram_tensor("xs", [T, D], F32).ap()
    g.oT = nc.dram_tensor("oTs", [8, 128, T], BF16).ap()
    g.hsc = nc.dram_tensor("hsc", [128, 8, T], BF16).ap()
    g.hkv = nc.dram_tensor("hkv", [128, 8, T], BF16).ap()
    g.hat = nc.dram_tensor("hat", [128, 8, T], BF16).ap()
    return g


class NormUnit:
    def __init__(self, nc, es, P, tag, idb, gT, nslots=2):
        sb = lambda n, s, d: es.enter_context(_sbt(nc, f"{tag}_{n}", s, d))
        self.P = P
        self.tag = tag
        self.idb = idb
        self.gT = gT
        self.ns = nslots
        self.junk = sb("junk", [128, D], BF16)
        self.ss = sb("ss", [128, nslots], F32)
        self.rr = sb("rr", [128, nslots], F32)
        self.eps = sb("eps", [128, 1], F32)
        self.xn = [sb(f"xn{i}", [128, D], BF16) for i in range(nslots)]
        self.pt = [es.enter_context(_pst(nc, f"{tag}_pt{i}", [128, 8, 128], BF16)) for i in range(2)]
        self.n = 0
        self.pn = 0
        P.pool(lambda e: e.memset(self.eps[:], EPS), writes=[(tag, "eps")])

    def stats(self, xt, xkey):
        P, tag = self.P, self.tag
        s = self.n % self.ns
        self.n += 1
        P.act(lambda e: e.activation(out=self.junk[:], in_=xt, func=AF.Square, accum_out=self.ss[:, s:s + 1]),
              reads=[xkey], writes=[(tag, "junk"), (tag, "ss", s)])
        P.act(lambda e: e.activation(out=self.rr[:, s:s + 1], in_=self.ss[:, s:s + 1], func=AF.Sqrt,
                                     scale=1.0 / D, bias=self.eps[:]),
              reads=[(tag, "ss", s), (tag, "eps")], writes=[(tag, "rr", s)])
        P.dve(lambda e: e.reciprocal(self.rr[:, s:s + 1], self.rr[:, s:s + 1]),
              reads=[(tag, "rr", s)], writes=[(tag, "rr", s)])
        return s

    def run_a(self, xt, xkey):
        P, tag = self.P, self.tag
        s = self.stats(xt, xkey)
        xn = self.xn[s]
        P.dve(lambda e: e.tensor_scalar(xn[:], xt, self.rr[:, s:s + 1], None, ALU.mult),
              reads=[xkey, (tag, "rr", s)], writes=[(tag, "xn", s)])
        return s

    def run_b(self, s, outs):
        P, tag = self.P, self.tag
        xn = self.xn[s]
        ps = self.pn % len(self.pt)
        self.pn += 1
        pt = self.pt[ps]
        for c in range(8):
            P.pe(lambda e, c=c: e.transpose(pt[:, c, :], xn[:, c * 128:(c + 1) * 128], self.idb[:]),
                 reads=[(tag, "xn", s), "idb"], writes=[("PS", tag, "pt", ps)])
        for gi, oap, okey in outs:
            P.dve(lambda e, gi=gi, oap=oap: e.tensor_tensor(
                oap, pt[:], self.gT[:, gi * 8:(gi + 1) * 8].unsqueeze(2).to_broadcast([128, 8, 128]), ALU.mult),
                reads=[("PS", tag, "pt", ps), "gT"], writes=[okey])
        return ps

    def run(self, xt, xkey, outs):
        s = self.run_a(xt, xkey)
        self.run_b(s, outs)
        return s


def load_consts(nc, es, P, g):
    sb = lambda n, s, d: es.enter_context(_sbt(nc, n, s, d))
    c = Ctx()
    c.idb = sb("idb", [128, 128], BF16)
    c.psw = sb("pswb", [128, 128], BF16)
    c.gT = sb("gTs", [128, 56], F32)
    c.masks = sb("masksb", [128, 2, 256], BF16)
    c.gates = sb("gates", [128, NT, NEXP], F32)
    P.dma("pool", lambda e: e.dma_start(out=c.idb[:], in_=g.ident[:, :]), writes=["idb"], semkey="c_idb")
    P.dma("pool", lambda e: e.dma_start(out=c.psw[:], in_=g.pswap[:, :]), writes=["psw"], semkey="c_psw")
    P.dma("sp", lambda e: e.dma_start(out=c.gT[:], in_=g.gT[:, :]), writes=["gT"], semkey="c_gT")
    for l in range(2):
        P.dma("pool", lambda e, l=l: e.dma_start(out=c.masks[:, l, :], in_=g.masks[l, :, :]),
              writes=["masks"], semkey=("c_mask", l))
    return c


def rope_proj(P, nc, B, tag, nslab, mm_fn, mm_reads, Ctab, Stab, out_fn, out_key, psw, idb, col0=0, pre=None):
    L = 1

    def stage1(s):
        sl = s % 2
        cs = slice(col0 + s * 512, col0 + (s + 1) * 512)
        hx = pre(s) if pre is not None else None
        for c in range(8):
            if pre is not None:
                P.pe(lambda e, s=s, c=c, sl=sl, hx=hx: mm_fn(e, s, c, B.pq[sl][:, :], hx), reads=mm_reads(s, hx),
                     writes=[("PS", "bk", sl)])
            else:
                P.pe(lambda e, s=s, c=c, sl=sl: mm_fn(e, s, c, B.pq[sl][:, :]), reads=mm_reads(s), writes=[("PS", "bk", sl)])
        P.dve(lambda e, sl=sl, cs=cs: e.tensor_tensor(B.ua[sl][:], B.pq[sl][:, :], Ctab[:, cs], ALU.mult),
              reads=[("PS", "bk", sl), "ropeC"], writes=[("ua", sl)])
        P.dve(lambda e, sl=sl, cs=cs: e.tensor_tensor(B.ub[sl][:], B.pq[sl][:, :], Stab[:, cs], ALU.mult),
              reads=[("PS", "bk", sl), "ropeS"], writes=[("ub", sl)])

    def stage2(s):
        sl = s % 2
        P.pe(lambda e, sl=sl: e.matmul(B.psw[sl][:, :], lhsT=idb[:], rhs=B.ua[sl][:], start=True, stop=False),
             reads=[("ua", sl), "idb"], writes=[("PS", "bk", 2 + sl)])
        P.pe(lambda e, sl=sl: e.matmul(B.psw[sl][:, :], lhsT=psw[:], rhs=B.ub[sl][:], start=False, stop=True),
             reads=[("ub", sl), "psw"], writes=[("PS", "bk", 2 + sl)])
        for oap, rows in out_fn(s):
            P.act(lambda e, sl=sl, oap=oap, rows=rows: e.copy(oap, B.psw[sl][rows, :]), reads=[("PS", "bk", 2 + sl)],
                  writes=[out_key])

    for i in range(nslab + L):
        if i < nslab:
            stage1(i)
        if i >= L:
            stage2(i - L)


def attention(P, nc, B, tag, dil, kt_fn, qt_fn, v_fn, kv_reads, q_reads, mask, evac_fn, idb, LOOK=3):
    nsub = T // dil
    nb = nsub // 128
    blocks = [(r, n) for r in range(dil) for n in range(nb)]

    def s1(idx):
        r, n = blocks[idx]
        nq = 256 if n < nb - 1 else 128
        sl = idx % 4
        stv = B.st[sl][:, :].rearrange("p (h q) -> p h q", h=2)[:, :, 0:nq]
        ptv = B.pt[sl][:, :, 0:nq]
        if nq == 256:
            P.pe(lambda e: e.matmul(stv, lhsT=kt_fn(r, 128 * n, 128), rhs=qt_fn(r, 128 * n, nq),
                                    start=True, stop=True), reads=kv_reads + q_reads, writes=[("PS", "bk", sl)])
        else:
            for hh in range(2):
                P.pe(lambda e, hh=hh: e.matmul(stv[:, hh, :], lhsT=kt_fn(r, 128 * n, 128), rhs=qt_fn(r, 128 * n, nq)[:, hh, :],
                                               start=True, stop=True), reads=kv_reads + q_reads, writes=[("PS", "bk", sl)])
        P.act(lambda e: e.activation(out=ptv, in_=stv, func=AF.Exp, scale=0.125),
              reads=[("PS", "bk", sl)], writes=[("ptile", sl)])
        P.dve(lambda e: e.tensor_tensor(ptv, ptv, mask[:, 0:nq].unsqueeze(1).to_broadcast([128, 2, nq]), ALU.mult),
              reads=[("ptile", sl), "masks"], writes=[("ptile", sl)])

    def s2(idx):
        r, n = blocks[idx]
        b = r * nb + n
        sl = idx % 4
        bank, pos = (b // 4) % 2, b % 4
        for hh in range(2):
            kb = 4 + hh * 2 + bank
            P.pe(lambda e, hh=hh, kb=kb: e.matmul(B.bk[kb][:, pos * 128:(pos + 1) * 128], lhsT=v_fn(hh, b),
                                                  rhs=B.pt[sl][:, hh, 0:128], start=(n == 0), stop=True),
                 reads=kv_reads + [("ptile", sl)], writes=[("PS", "bk", kb)])
            if n < nb - 1:
                b2 = b + 1
                bank2, pos2 = (b2 // 4) % 2, b2 % 4
                kb2 = 4 + hh * 2 + bank2
                P.pe(lambda e, hh=hh, kb2=kb2, pos2=pos2: e.matmul(
                    B.bk[kb2][:, pos2 * 128:(pos2 + 1) * 128], lhsT=v_fn(hh, b), rhs=B.pt[sl][:, hh, 128:256],
                    start=True, stop=False), reads=kv_reads + [("ptile", sl)], writes=[("PS", "bk", kb2)])
            if pos == 3:
                evac_fn(hh, b // 4, B.bk[kb], ("PS", "bk", kb))

    nblk = len(blocks)
    for i in range(nblk + LOOK):
        if i < nblk:
            s1(i)
        if i >= LOOK:
            s2(i - LOOK)


def v_from_vt(P, B, dil, VTs, vkey):
    nb = T // dil // 128
    vv = tokview(VTs[:, :], dil)
    for b in range(32):
        r, n = b // nb, b % nb
        half, pos = (b // 4) % 2, b % 4
        P.pe(lambda e, r=r, n=n, half=half, pos=pos: e.transpose(
            B.vtr[half][:, pos, :], vv[:, r, 128 * n:128 * (n + 1)], B.idb[:]),
            reads=[vkey, "idb"], writes=[("PS", "bk", 2 + half)])
        if pos == 3:
            b0 = b - 3
            P.dve(lambda e, b0=b0, half=half: e.tensor_copy(
                B.V[:, b0:b0 + 4, :, 0:64], B.vtr[half].rearrange("p b (h d) -> p b h d", h=2)),
                reads=[("PS", "bk", 2 + half)], writes=["V"])


def attn_buffers(nc, es, tag):
    sb = lambda n, s, d: es.enter_context(_sbt(nc, f"{tag}_{n}", s, d))
    ps = lambda n, s, d: es.enter_context(_pst(nc, f"{tag}_{n}", s, d))
    B = Ctx()
    bk = [ps(f"bk{i}", [128, 512], F32) for i in range(8)]
    B.bk = bk
    B.pq = [bk[0], bk[1]]
    B.psw = [bk[2], bk[3]]
    B.st = [bk[i] for i in range(4)]
    B.vtr = [bk[2 + i][:, :].bitcast(BF16)[:, 0:512].rearrange("p (b d) -> p b d", b=4) for i in range(2)]
    B.ua = [sb(f"ua{i}", [128, 512], BF16) for i in range(2)]
    B.ub = [sb(f"ub{i}", [128, 512], BF16) for i in range(2)]
    B.pt = [sb(f"ptl{i}", [128, 2, 256], BF16) for i in range(4)]
    B.ropeC = sb("ropeC", [128, T], BF16)
    B.ropeS = sb("ropeS", [128, T], BF16)
    B.V = sb("V", [128, 32, 2, 128], BF16)
    return B


def load_rope(P, g, B):
    for h in range(2):
        cs = slice(h * 2048, (h + 1) * 2048)
        P.dma("pool", lambda e, cs=cs: e.dma_start(out=B.ropeC[:, cs], in_=g.ropeC[:, cs]), writes=["ropeC"],
              semkey=("ropeC", h))
        P.dma("pool", lambda e, cs=cs: e.dma_start(out=B.ropeS[:, cs], in_=g.ropeS[:, cs]), writes=["ropeS"],
              semkey=("ropeS", h))


def phase_A(P, nc, g, C):
    with ExitStack() as es:
        sb = lambda n, s, d: es.enter_context(_sbt(nc, n, s, d))
        hT0 = sb("A_hT0", [128, 8, T], BF16)
        with ExitStack() as es2:
            sb2 = lambda n, s, d: es2.enter_context(_sbt(nc, n, s, d))
            xt = [sb2(f"A_xt{i}", [128, D], F32) for i in range(3)]
            NU = NormUnit(nc, es2, P, "An", C.idb, C.gT)
            for t in range(NT):
                s = t % 3
                P.dma("sp", lambda e, t=t, s=s: e.dma_start(out=xt[s][:], in_=g.x[t * 128:(t + 1) * 128, :]),
                      writes=[("A_xt", s)], semkey=("A_xt", s))
                NU.run(xt[s][:], ("A_xt", s), [(0, hT0[:, :, t * 128:(t + 1) * 128], "hT0")])
            P.fence()
            P.flush(nc)
        B = attn_buffers(nc, es, "A")
        B.idb = C.idb
        load_rope(P, g, B)
        wq = [sb(f"A_wq{i}", [128, 8, 384], BF16) for i in range(2)]
        QTd = sb("A_QTd", [128, 2, T], BF16)
        KT = sb("A_KT", [128, T], BF16)
        VTs = sb("A_VTs", [128, T], BF16)
        P.pool(lambda e: e.memset(QTd[:], 0.0), writes=["QT"])
        acc = [sb(f"A_acc{i}", [128, T], F32) for i in range(2)]
        rec = sb("A_rec", [128, 512], F32)
        lnd = sb("A_lnd", [128, 512], F32)
        obf = sb("A_obf", [128, T], BF16)
        P.pool(lambda e: e.memset(B.V[:, :, :, 64:128], 1.0), writes=["V"])
        wv_ = g.wqkv.rearrange("(c p) n -> p c n", p=128)
        it = 0
        for pair in range(STOP.get("a_pairs", 8)):
            for gi, (win, dil) in enumerate(A_CONFIGS):
                ws = it % 2
                it += 1
                if it > STOP.get("a_iters", 99):
                    continue
                for k in range(3):
                    c0 = gi * 3072 + k * 1024 + pair * 128
                    P.dma("pool", lambda e, ws=ws, k=k, c0=c0: e.dma_start(
                        out=wq[ws][:, :, k * 128:(k + 1) * 128], in_=wv_[:, :, c0:c0 + 128]),
                        writes=[("wq", ws)], semkey=("wq", ws, k))
                nb = T // dil // 128
                rope_proj(P, nc, B, "A", 8,
                          lambda e, s, c, o, ws=ws: e.matmul(o, lhsT=wq[ws][:, c, 0:128], rhs=hT0[:, c, s * 512:(s + 1) * 512],
                                                             start=(c == 0), stop=(c == 7)),
                          lambda s, ws=ws: [("wq", ws), "hT0"], B.ropeC, B.ropeS,
                          lambda s: [(QTd[0:64, 0, s * 512:(s + 1) * 512], slice(0, 64)),
                                     (QTd[64:128, 1, s * 512:(s + 1) * 512], slice(64, 128))], "QT", C.psw, C.idb)
                rope_proj(P, nc, B, "A", 8,
                          lambda e, s, c, o, ws=ws: e.matmul(o, lhsT=wq[ws][:, c, 128:256], rhs=hT0[:, c, s * 512:(s + 1) * 512],
                                                             start=(c == 0), stop=(c == 7)),
                          lambda s, ws=ws: [("wq", ws), "hT0"], B.ropeC, B.ropeS,
                          lambda s: [(KT[:, s * 512:(s + 1) * 512], slice(0, 128))], "KT", C.psw, C.idb)
                if STOP.get("a_part", 9) < 1:
                    continue
                for sv in range(8):
                    sl = sv % 2
                    for c in range(8):
                        P.pe(lambda e, c=c, sv=sv, sl=sl, ws=ws: e.matmul(
                            B.pq[sl][:, :], lhsT=wq[ws][:, c, 256:384], rhs=hT0[:, c, sv * 512:(sv + 1) * 512],
                            start=(c == 0), stop=(c == 7)), reads=[("wq", ws), "hT0"], writes=[("PS", "bk", sl)])
                    P.act(lambda e, sv=sv, sl=sl: e.copy(VTs[:, sv * 512:(sv + 1) * 512], B.pq[sl][:, :]),
                          reads=[("PS", "bk", sl)], writes=["VTs"])
                v_from_vt(P, B, dil, VTs, "VTs")
                if STOP.get("a_part", 9) < 2:
                    continue
                def evac(hh, k, pvb, pvkey, gi=gi, dil=dil, nb=nb):
                    accv = tokview(acc[hh][:, :], dil)
                    if nb >= 4:
                        r, n0 = (4 * k) // nb, (4 * k) % nb
                        dst = accv[:, r, 128 * n0:128 * n0 + 512]
                        src = pvb[:, :]
                    else:
                        dst = accv[:, 2 * k:2 * k + 2, 0:256]
                        src = pvb[:, :].rearrange("p (a b) -> p a b", a=2)
                    if gi == 0:
                        P.dve(lambda e: e.tensor_copy(dst, src), reads=[pvkey], writes=[("acc", hh)])
                    else:
                        P.dve(lambda e: e.tensor_tensor(dst, src, dst, ALU.add), reads=[pvkey, ("acc", hh)],
                              writes=[("acc", hh)])

                if dil == 1:
                    qtf = lambda r, n0, cnt: QTd[:, :, n0:n0 + cnt]
                else:
                    qtf = lambda r, n0, cnt, dil=dil: QTd[:, :, :].rearrange("p h (m d) -> p h d m", d=dil)[:, :, r, n0:n0 + cnt]
                attention(P, nc, B, "A", dil,
                          lambda r, n0, cnt, dil=dil: tokview(KT[:, :], dil)[:, r, n0:n0 + cnt],
                          qtf, lambda hh, b: B.V[:, b, hh, :],
                          ["KT", "V"], ["QT"], C.masks[:, 0, :], evac, C.idb)
            for hh in range(2):
                for s in range(8):
                    cs = slice(s * 512, (s + 1) * 512)
                    P.act(lambda e, hh=hh, cs=cs: e.activation(out=lnd[64:128, :], in_=acc[hh][64:128, cs], func=AF.Ln),
                          reads=[("acc", hh)], writes=["lnd"])
                    P.act(lambda e: e.activation(out=rec[0:64, :], in_=lnd[64:128, :], func=AF.Exp, scale=-1.0),
                          reads=["lnd"], writes=["rec"])
                    P.dve(lambda e, hh=hh, cs=cs: e.tensor_tensor(obf[hh * 64:(hh + 1) * 64, cs], acc[hh][0:64, cs],
                                                                   rec[0:64, :], ALU.mult),
                           reads=[("acc", hh), "rec"], writes=["obf"])
            P.dma("sp", lambda e, pair=pair: e.dma_start(out=g.oT[pair, :, :], in_=obf[:]), reads=["obf"],
                  writes=[("oT", pair)], semkey="obf_st")
        P.fence()
        P.flush(nc)


def phase_B(P, nc, g, C, wo_dram, x_src, gain_idx, h_dst, router=False):
    tag = "B%d" % gain_idx
    with ExitStack() as es:
        sb = lambda n, s, d: es.enter_context(_sbt(nc, f"{tag}_{n}", s, d))
        ps = lambda n, s, d: es.enter_context(_pst(nc, f"{tag}_{n}", s, d))
        wo = sb("wo", [128, 8, D], BF16)
        og = [sb(f"og{i}", [128, 8, 1024], BF16) for i in range(2)]
        xt = [sb(f"xt{i}", [128, D], F32) for i in range(3)]
        x1 = [sb(f"x1{i}", [128, D], F32) for i in range(4)]
        hg = [sb(f"hg{i}", [128, 8, 1024], BF16) for i in range(2)]
        py = [ps(f"py{i}", [128, D], F32) for i in range(2)]
        NU = NormUnit(nc, es, P, tag + "n", C.idb, C.gT, nslots=3)
        wov = wo_dram.rearrange("(c p) n -> p c n", p=128)
        for h in range(2):
            P.dma("pool", lambda e, h=h: e.dma_start(out=wo[:, :, h * 512:(h + 1) * 512], in_=wov[:, :, h * 512:(h + 1) * 512]),
                  writes=[(tag, "wo")], semkey=(tag, "wo", h))
        if router:
            wr32 = sb("wr32", [128, 8, NEXP], F32)
            wrg = sb("wrg", [128, 8, NEXP], F32)
            wrh = sb("wrh", [128, 8, NEXP], BF16)
            wrh32 = sb("wrh32", [128, 8, NEXP], F32)
            wrl = sb("wrl", [128, 8, NEXP], BF16)
            xlo = [sb(f"xlo{i}", [128, D], BF16) for i in range(2)]
            hiT = [sb(f"hiT{i}", [128, 8, 128], BF16) for i in range(2)]
            loT = [sb(f"loT{i}", [128, 8, 128], BF16) for i in range(2)]
            plo = ps("plo", [128, 8, 128], BF16)
            plg = ps("plg", [128, NEXP], F32)
            lg = sb("lg", [128, NEXP], F32)
            l2 = sb("l2", [128, NEXP], F32)
            eq1 = sb("eq1", [128, NEXP], F32)
            eq2 = sb("eq2", [128, NEXP], F32)
            sm = sb("sm", [128, 8], F32)
            P.dma("sp", lambda e: e.dma_start(out=wr32[:], in_=g.wr.rearrange("(c p) n -> p c n", p=128)),
                  writes=["wr32"], semkey="wr32")
            P.dve(lambda e: e.tensor_tensor(wrg[:], wr32[:], C.gT[:, gain_idx * 8:(gain_idx + 1) * 8].unsqueeze(2).to_broadcast([128, 8, NEXP]), ALU.mult),
                  reads=["wr32", "gT"], writes=["wrg"])
            P.dve(lambda e: e.tensor_copy(wrh[:], wrg[:]), reads=["wrg"], writes=["wrh"])
            P.dve(lambda e: e.tensor_copy(wrh32[:], wrh[:]), reads=["wrh"], writes=["wrh32"])
            P.dve(lambda e: e.tensor_tensor(wrl[:], wrg[:], wrh32[:], ALU.subtract), reads=["wrg", "wrh32"], writes=["wrl"])
        slots = {}

        def stage1(t):
            gq, tt = t // 8, t % 8
            gs = gq % 2
            if tt == 0:
                P.dma("sp", lambda e: e.dma_start(
                    out=og[gs][:], in_=g.oT[:, :, gq * 1024:(gq + 1) * 1024].rearrange("c p t -> p c t")),
                    reads=[("oT", c) for c in range(8)], writes=[(tag, "og", gs)], semkey=(tag, "og", gs))
            sx = t % 3
            s = t % 4
            P.dma("sp", lambda e: e.dma_start(out=xt[sx][:], in_=x_src[t * 128:(t + 1) * 128, :]),
                  reads=[("xs", t)], writes=[(tag, "xt", sx)], semkey=(tag, "xt", sx))
            pb = t % 2
            for half in range(2):
                for c in range(8):
                    P.pe(lambda e, c=c, half=half: e.matmul(
                        py[pb][:, half * 512:(half + 1) * 512], lhsT=og[gs][:, c, tt * 128:(tt + 1) * 128],
                        rhs=wo[:, c, half * 512:(half + 1) * 512], start=(c == 0), stop=(c == 7)),
                        reads=[(tag, "og", gs), (tag, "wo")], writes=[("PS", tag, "py", pb)])
            P.dve(lambda e: e.tensor_tensor(x1[s][:], py[pb][:, :], xt[sx][:], ALU.add),
                  reads=[("PS", tag, "py", pb), (tag, "xt", sx)], writes=[(tag, "x1", s)])
            P.dma("sp", lambda e: e.dma_start(out=g.xs[t * 128:(t + 1) * 128, :], in_=x1[s][:]),
                  reads=[(tag, "x1", s)], writes=[("xs", t)], semkey=(tag, "x1st", s))

        def stage1b(t):
            s = t % 4
            slots[t] = NU.run_a(x1[s][:], (tag, "x1", s))

        def stage2(t):
            gq, tt = t // 8, t % 8
            gs = gq % 2
            s = t % 4
            ns = slots[t]
            ps_ = NU.run_b(ns, [(gain_idx, hg[gs][:, :, tt * 128:(tt + 1) * 128], (tag, "hg", gs))])
            if router:
                rs = t % 2
                ntag = tag + "n"
                P.act(lambda e: e.copy(hiT[rs][:], NU.pt[ps_][:]), reads=[("PS", ntag, "pt", ps_)], writes=[("hiT", rs)])
                P.dve(lambda e: e.scalar_tensor_tensor(
                    out=xlo[rs][:], in0=x1[s][:], scalar=NU.rr[:, ns:ns + 1], in1=NU.xn[ns][:], op0=ALU.mult, op1=ALU.subtract),
                    reads=[(tag, "x1", s), (ntag, "rr", ns), (ntag, "xn", ns)], writes=[("xlo", rs)])
                for c in range(8):
                    P.pe(lambda e, c=c: e.transpose(plo[:, c, :], xlo[rs][:, c * 128:(c + 1) * 128], C.idb[:]),
                         reads=[("xlo", rs), "idb"], writes=[("PS", "plo")])
                P.act(lambda e: e.copy(loT[rs][:], plo[:]), reads=[("PS", "plo")], writes=[("loT", rs)])
                k = 0
                for (aT, akey, wmat, wkey) in ((hiT, "hiT", wrh, "wrh"), (loT, "loT", wrh, "wrh"), (hiT, "hiT", wrl, "wrl")):
                    for c in range(8):
                        P.pe(lambda e, c=c, aT=aT, wmat=wmat, k=k: e.matmul(
                            plg[:, :], lhsT=aT[rs][:, c, :], rhs=wmat[:, c, :], start=(k == 0), stop=(k == 23)),
                            reads=[(akey, rs), wkey], writes=[("PS", "plg")])
                        k += 1
                P.dve(lambda e: e.tensor_copy(lg[:], plg[:, :]), reads=[("PS", "plg")], writes=["lg"])
                P.dve(lambda e: e.reduce_max(sm[:, 0:1], lg[:], axis=AX.X), reads=["lg"], writes=["sm0"])
                P.dve(lambda e: e.tensor_scalar(eq1[:], lg[:], sm[:, 0:1], None, ALU.is_equal), reads=["lg", "sm0"], writes=["eq1"])
                P.dve(lambda e: e.scalar_tensor_tensor(out=l2[:], in0=eq1[:], scalar=-1e30, in1=lg[:], op0=ALU.mult, op1=ALU.add),
                      reads=["eq1", "lg"], writes=["l2"])
                P.dve(lambda e: e.reduce_max(sm[:, 1:2], l2[:], axis=AX.X), reads=["l2"], writes=["sm1"])
                P.dve(lambda e: e.tensor_scalar(eq2[:], l2[:], sm[:, 1:2], None, ALU.is_equal), reads=["l2", "sm1"], writes=["eq2"])
                P.dve(lambda e: e.tensor_tensor(sm[:, 2:3], sm[:, 1:2], sm[:, 0:1], ALU.subtract), reads=["sm0", "sm1"], writes=["sm2"])
                P.act(lambda e: e.activation(out=sm[:, 3:4], in_=sm[:, 2:3], func=AF.Exp), reads=["sm2"], writes=["sm3"])
                P.dve(lambda e: e.tensor_scalar(sm[:, 4:5], sm[:, 3:4], 1.0, None, ALU.add), reads=["sm3"], writes=["sm4"])
                P.dve(lambda e: e.reciprocal(sm[:, 5:6], sm[:, 4:5]), reads=["sm4"], writes=["sm5"])
                P.dve(lambda e: e.tensor_tensor(sm[:, 6:7], sm[:, 3:4], sm[:, 5:6], ALU.mult), reads=["sm3", "sm5"], writes=["sm6"])
                P.dve(lambda e: e.tensor_scalar(eq1[:], eq1[:], sm[:, 5:6], None, ALU.mult), reads=["eq1", "sm5"], writes=["eq1"])
                P.dve(lambda e: e.scalar_tensor_tensor(out=C.gates[:, t, :], in0=eq2[:], scalar=sm[:, 6:7], in1=eq1[:],
                                                       op0=ALU.mult, op1=ALU.add),
                      reads=["eq2", "sm6", "eq1"], writes=["gates"])
            if tt == 7:
                P.dma("sp", lambda e: e.dma_start(out=h_dst[:, :, gq * 1024:(gq + 1) * 1024], in_=hg[gs][:]),
                      reads=[(tag, "hg", gs)], writes=[("hsc", gq)], semkey=(tag, "hgst", gs))

        for i in range(NT + 2):
            if i < NT:
                stage1(i)
            if 0 <= i - 1 < NT:
                stage1b(i - 1)
            if 0 <= i - 2 < NT:
                stage2(i - 2)
        P.fence()
        P.flush(nc)


def ffn_stage(P, nc, tag, hTg, hkey, nchunk, wsrc1, wsrc3, wb1, wb3, pa, pb, sa, actT, wit):
    nblk = (nchunk + 3) // 4
    cnt = 0
    for fb in range(nblk):
        ncols = min(512, nchunk * 128 - fb * 512)
        ws = wit["w"] % 2
        wit["w"] += 1
        P.dma("pool", lambda e, fb=fb, ncols=ncols, ws=ws: e.dma_start(out=wb1[ws][:, :, 0:ncols], in_=wsrc1(fb * 512, ncols)),
              writes=[(tag, "wb1", ws)], semkey=(tag, "wb1", ws))
        P.dma("pool", lambda e, fb=fb, ncols=ncols, ws=ws: e.dma_start(out=wb3[ws][:, :, 0:ncols], in_=wsrc3(fb * 512, ncols)),
              writes=[(tag, "wb3", ws)], semkey=(tag, "wb3", ws))
        for jj in range(ncols // 128):
            j = fb * 4 + jj
            for th in range(2):
                sl = wit["p"] % 2
                wit["p"] += 1
                ts_ = slice(th * 512, (th + 1) * 512)
                for c in range(8):
                    P.pe(lambda e, c=c, jj=jj, ws=ws, sl=sl, ts_=ts_: e.matmul(
                        pa[sl][:, :], lhsT=wb1[ws][:, c, jj * 128:(jj + 1) * 128], rhs=hTg[:, c, ts_],
                        start=(c == 0), stop=(c == 7)), reads=[(tag, "wb1", ws), hkey], writes=[("PS", tag, "pa", sl)])
                for c in range(8):
                    P.pe(lambda e, c=c, jj=jj, ws=ws, sl=sl, ts_=ts_: e.matmul(
                        pb[sl][:, :], lhsT=wb3[ws][:, c, jj * 128:(jj + 1) * 128], rhs=hTg[:, c, ts_],
                        start=(c == 0), stop=(c == 7)), reads=[(tag, "wb3", ws), hkey], writes=[("PS", tag, "pb", sl)])
                P.act(lambda e, sl=sl: e.activation(out=sa[sl][:], in_=pa[sl][:, :], func=AF.Silu),
                      reads=[("PS", tag, "pa", sl)], writes=[(tag, "sa", sl)])
                P.dve(lambda e, sl=sl, j=j, ts_=ts_: e.tensor_tensor(actT[:, j, ts_], sa[sl][:], pb[sl][:, :], ALU.mult),
                      reads=[(tag, "sa", sl), ("PS", tag, "pb", sl)], writes=[(tag, "actT")])


def phase_C1(P, nc, g, C):
    tag = "C1"
    NCH = DFF // 128
    with ExitStack() as es:
        sb = lambda n, s, d: es.enter_context(_sbt(nc, f"{tag}_{n}", s, d))
        ps = lambda n, s, d: es.enter_context(_pst(nc, f"{tag}_{n}", s, d))
        w2 = sb("w2", [128, NCH, D], BF16)
        hTg = [sb(f"hTg{i}", [128, 8, 1024], BF16) for i in range(2)]
        actT = sb("actT", [128, NCH, 1024], BF16)
        wb1 = [sb(f"wb1{i}", [128, 8, 512], BF16) for i in range(2)]
        wb3 = [sb(f"wb3{i}", [128, 8, 512], BF16) for i in range(2)]
        sa = [sb(f"sa{i}", [128, 512], BF16) for i in range(2)]
        xt = [sb(f"xt{i}", [128, D], F32) for i in range(3)]
        pa = [ps(f"pa{i}", [128, 512], F32) for i in range(2)]
        pb = [ps(f"pb{i}", [128, 512], F32) for i in range(2)]
        py = [ps(f"py{i}", [128, D], F32) for i in range(2)]
        w2v = g.w2d.rearrange("(j p) n -> p j n", p=128)
        for q in range(0, NCH, 4):
            n = min(4, NCH - q)
            P.dma("pool", lambda e, q=q, n=n: e.dma_start(out=w2[:, q:q + n, :], in_=w2v[:, q:q + n, :]),
                  writes=[(tag, "w2")], semkey=(tag, "w2", q))
        w1v = g.w1d.rearrange("(c p) n -> p c n", p=128)
        w3v = g.w3d.rearrange("(c p) n -> p c n", p=128)
        wit = {"w": 0, "p": 0}
        for gq in range(4):
            gs = gq % 2
            P.dma("sp", lambda e, gq=gq, gs=gs: e.dma_start(out=hTg[gs][:], in_=g.hsc[:, :, gq * 1024:(gq + 1) * 1024]),
                  reads=[("hsc", gq)], writes=[(tag, "hTg", gs)], semkey=(tag, "hTg", gs))
            ffn_stage(P, nc, tag, hTg[gs], (tag, "hTg", gs), NCH,
                      lambda c0, n: w1v[:, :, c0:c0 + n], lambda c0, n: w3v[:, :, c0:c0 + n],
                      wb1, wb3, pa, pb, sa, actT, wit)
            for tt in range(8):
                t = gq * 8 + tt
                s = t % 3
                pbk = t % 2
                P.dma("sp", lambda e, t=t, s=s: e.dma_start(out=xt[s][:], in_=g.xs[t * 128:(t + 1) * 128, :]),
                      reads=[("xs", t)], writes=[(tag, "xt", s)], semkey=(tag, "xt", s))
                for half in range(2):
                    for j in range(NCH):
                        P.pe(lambda e, j=j, half=half, tt=tt, pbk=pbk: e.matmul(
                            py[pbk][:, half * 512:(half + 1) * 512], lhsT=actT[:, j, tt * 128:(tt + 1) * 128],
                            rhs=w2[:, j, half * 512:(half + 1) * 512], start=(j == 0), stop=(j == NCH - 1)),
                            reads=[(tag, "actT"), (tag, "w2")], writes=[("PS", tag, "py", pbk)])
                P.dve(lambda e, s=s, pbk=pbk: e.tensor_tensor(xt[s][:], py[pbk][:, :], xt[s][:], ALU.add),
                      reads=[("PS", tag, "py", pbk), (tag, "xt", s)], writes=[(tag, "xt", s)])
                P.dma("sp", lambda e, t=t, s=s: e.dma_start(out=g.xs[t * 128:(t + 1) * 128, :], in_=xt[s][:]),
                      reads=[(tag, "xt", s)], writes=[("xs", t)], semkey=(tag, "xst", s))
        P.fence()
        P.flush(nc)


def phase_PLE(P, nc, g, C, layer, gain_idx, final):
    tag = "E%d" % layer
    with ExitStack() as es:
        sb = lambda n, s, d: es.enter_context(_sbt(nc, f"{tag}_{n}", s, d))
        ps = lambda n, s, d: es.enter_context(_pst(nc, f"{tag}_{n}", s, d))
        wg = sb("wg", [128, 8, D], BF16)
        wp = sb("wp", [128, 2, D], BF16)
        pTg = [sb(f"pTg{i}", [128, 2, 1024], BF16) for i in range(2)]
        NXT = 5
        xt = [sb(f"xt{i}", [128, D], F32) for i in range(NXT)]
        gTt = [sb(f"gTt{i}", [128, 8, 128], BF16) for i in range(3)]
        sg = [sb(f"sg{i}", [128, 512], F32) for i in range(2)]
        pg = [ps(f"pg{i}", [128, 512], F32) for i in range(2)]
        pp = [ps(f"pp{i}", [128, 512], F32) for i in range(2)]
        NU = NormUnit(nc, es, P, tag + "n", C.idb, C.gT, nslots=3)
        NU2 = NormUnit(nc, es, P, tag + "m", C.idb, C.gT, nslots=3)
        if final:
            fg = sb("fg", [128, D], F32)
            ot = [sb(f"ot{i}", [128, D], F32) for i in range(2)]
            P.dma("sp", lambda e: e.dma_start(out=fg[:], in_=g.fgain[:, :]), writes=[(tag, "fg")], semkey=(tag, "fg"))
        else:
            hk = [sb(f"hk{i}", [128, 8, 1024], BF16) for i in range(2)]
            ha = [sb(f"ha{i}", [128, 8, 1024], BF16) for i in range(2)]
        wgv = g.wg[layer].rearrange("(c p) n -> p c n", p=128)
        for h in range(2):
            P.dma("pool", lambda e, h=h: e.dma_start(out=wg[:, :, h * 512:(h + 1) * 512], in_=wgv[:, :, h * 512:(h + 1) * 512]),
                  writes=[(tag, "wg")], semkey=(tag, "wg", h))
        P.dma("pool", lambda e: e.dma_start(out=wp[:], in_=g.wp[layer].rearrange("(c p) n -> p c n", p=128)),
              writes=[(tag, "wp")], semkey=(tag, "wp"))
        sl1, sl2 = {}, {}
        hcnt = {"n": 0}

        def s1(t):
            gq, tt = t // 8, t % 8
            gs = gq % 2
            if tt == 0:
                P.dma("pool", lambda e: e.dma_start(
                    out=pTg[gs][:], in_=g.pT[layer, :, gq * 1024:(gq + 1) * 1024].rearrange("(c p) t -> p c t", p=128)),
                    writes=[(tag, "pTg", gs)], semkey=(tag, "pTg", gs))
            s = t % NXT
            P.dma("sp", lambda e: e.dma_start(out=xt[s][:], in_=g.xs[t * 128:(t + 1) * 128, :]),
                  reads=[("xs", t)], writes=[(tag, "xt", s)], semkey=(tag, "xt", s))
            sl1[t] = NU.run_a(xt[s][:], (tag, "xt", s))

        def s2(t):
            s3_ = t % 3
            NU.run_b(sl1[t], [(gain_idx, gTt[s3_][:], (tag, "gTt", s3_))])

        def s3(t):
            gq, tt = t // 8, t % 8
            gs = gq % 2
            s = t % NXT
            s3_ = t % 3
            for half in range(2):
                hs_ = slice(half * 512, (half + 1) * 512)
                k = hcnt["n"] % 2
                hcnt["n"] += 1
                for c in range(8):
                    P.pe(lambda e, c=c, hs_=hs_, k=k: e.matmul(pg[k][:, :], lhsT=gTt[s3_][:, c, :], rhs=wg[:, c, hs_],
                                                              start=(c == 0), stop=(c == 7)),
                         reads=[(tag, "gTt", s3_), (tag, "wg")], writes=[("PS", tag, "pg", k)])
                for c in range(2):
                    P.pe(lambda e, c=c, hs_=hs_, k=k: e.matmul(pp[k][:, :], lhsT=pTg[gs][:, c, tt * 128:(tt + 1) * 128],
                                                              rhs=wp[:, c, hs_], start=(c == 0), stop=(c == 1)),
                         reads=[(tag, "pTg", gs), (tag, "wp")], writes=[("PS", tag, "pp", k)])
                P.act(lambda e, k=k: e.activation(out=sg[k][:], in_=pg[k][:, :], func=AF.Sigmoid),
                      reads=[("PS", tag, "pg", k)], writes=[(tag, "sg", k)])
                P.dve(lambda e, k=k: e.tensor_tensor(sg[k][:], sg[k][:], pp[k][:, :], ALU.mult),
                      reads=[(tag, "sg", k), ("PS", tag, "pp", k)], writes=[(tag, "sg", k)])
                P.dve(lambda e, k=k, hs_=hs_: e.tensor_tensor(xt[s][:, hs_], xt[s][:, hs_], sg[k][:], ALU.add),
                      reads=[(tag, "xt", s), (tag, "sg", k)], writes=[(tag, "xt", s)])
            xkey = (tag, "xt", s)
            if not final:
                P.dma("sp", lambda e: e.dma_start(out=g.xs[t * 128:(t + 1) * 128, :], in_=xt[s][:]),
                      reads=[xkey], writes=[("xs", t)], semkey=(tag, "xst", s))

        def s3b(t):
            s = t % NXT
            xkey = (tag, "xt", s)
            if final:
                s2_ = t % 2
                ns = NU2.stats(xt[s][:], xkey)
                P.dve(lambda e: e.scalar_tensor_tensor(out=ot[s2_][:], in0=xt[s][:], scalar=NU2.rr[:, ns:ns + 1], in1=fg[:],
                                                       op0=ALU.mult, op1=ALU.mult),
                      reads=[xkey, (tag + "m", "rr", ns), (tag, "fg")], writes=[(tag, "ot", s2_)])
                P.dma("sp", lambda e: e.dma_start(out=g.out[t * 128:(t + 1) * 128, :], in_=ot[s2_][:]),
                      reads=[(tag, "ot", s2_)], writes=[("out", t)], semkey=(tag, "ost", s2_))
            else:
                sl2[t] = NU2.run_a(xt[s][:], xkey)

        def s4(t):
            if final:
                return
            gq, tt = t // 8, t % 8
            gs = gq % 2
            NU2.run_b(sl2[t], [(3, hk[gs][:, :, tt * 128:(tt + 1) * 128], (tag, "hk", gs)),
                               (4, ha[gs][:, :, tt * 128:(tt + 1) * 128], (tag, "ha", gs))])
            if tt == 7:
                cs = slice(gq * 1024, (gq + 1) * 1024)
                P.dma("sp", lambda e: e.dma_start(out=g.hkv[:, :, cs], in_=hk[gs][:]), reads=[(tag, "hk", gs)],
                      writes=[("hkv", gq)], semkey=(tag, "hkst", gs))
                P.dma("sp", lambda e: e.dma_start(out=g.hat[:, :, cs], in_=ha[gs][:]), reads=[(tag, "ha", gs)],
                      writes=[("hat", gq)], semkey=(tag, "hast", gs))

        for i in range(NT + 4):
            if i < NT:
                s1(i)
            if 0 <= i - 1 < NT:
                s2(i - 1)
            if 0 <= i - 2 < NT:
                s3(i - 2)
            if 0 <= i - 3 < NT:
                s3b(i - 3)
            if 0 <= i - 4 < NT:
                s4(i - 4)
        P.fence()
        P.flush(nc)


def phase_D(P, nc, g, C):
    tag = "D"
    with ExitStack() as es:
        sb = lambda n, s, d: es.enter_context(_sbt(nc, f"{tag}_{n}", s, d))
        B = attn_buffers(nc, es, "D")
        B.idb = C.idb
        load_rope(P, g, B)
        wk2 = sb("wk2", [128, 8, 2, 128], BF16)
        wv = sb("wv", [128, 8, 128], BF16)
        wq = sb("wq", [128, 8, D], BF16)
        KT2 = [sb(f"KT{i}", [128, T], BF16) for i in range(2)]
        QTd = [sb(f"QTd{i}", [128, 2, T], BF16) for i in range(2)]
        VTs = sb("VTs", [128, T], BF16)
        for i in range(2):
            P.pool(lambda e, i=i: e.memset(QTd[i][:], 0.0), writes=[("QT", i)])
        obf = [sb(f"obf{i}", [128, T], BF16) for i in range(2)]
        hs = [sb(f"hs{i}", [128, 8, 512], BF16) for i in range(3)]
        esk = sb("esk", [128, 16], F32)
        tmp = sb("tmp", [128, 512], F32)
        rec = sb("rec", [128, 512], F32)
        kvv = g.kvw.rearrange("(c p) n -> p c n", p=128)
        for kvh in range(2):
            for dup in range(2):
                P.dma("pool", lambda e, kvh=kvh, dup=dup: e.dma_start(
                    out=wk2[:, :, kvh, dup * 64:(dup + 1) * 64], in_=kvv[:, :, kvh * 64:(kvh + 1) * 64]),
                    writes=["wk2"], semkey=("wk2", kvh, dup))
        P.dma("pool", lambda e: e.dma_start(out=wv[:], in_=kvv[:, :, 128:256]), writes=["wv"], semkey="wv")
        wqv = g.wq1.rearrange("(c p) n -> p c n", p=128)
        for h in range(2):
            P.dma("pool", lambda e, h=h: e.dma_start(out=wq[:, :, h * 512:(h + 1) * 512], in_=wqv[:, :, h * 512:(h + 1) * 512]),
                  writes=["wq1"], semkey=("wq1", h))
        P.dma("sp", lambda e: e.dma_start(out=esk[:], in_=g.sinks[:, :]), writes=["esk"], semkey="esk")
        P.act(lambda e: e.activation(out=esk[:], in_=esk[:], func=AF.Exp), reads=["esk"], writes=["esk"])
        P.pool(lambda e: e.memset(B.V[:, :, :, 64:128], 1.0), writes=["V"])
        hst = {"cnt": 0, "slot": {}}

        def mk_pre(src, srckey):
            def pre(s_):
                sl = hst["cnt"] % 3
                hst["cnt"] += 1
                hst["slot"][s_] = sl
                P.dma("sp", lambda e: e.dma_start(out=hs[sl][:], in_=src[:, :, s_ * 512:(s_ + 1) * 512]),
                      reads=[(srckey, s_ // 2)], writes=[("hs", sl)], semkey=("hs", sl))
                return sl
            return pre

        for kvh in range(2):
            rope_proj(P, nc, B, "D", 8,
                      lambda e, s_, c, o, hx, kvh=kvh: e.matmul(o, lhsT=wk2[:, c, kvh, :], rhs=hs[hx][:, c, :],
                                                                start=(c == 0), stop=(c == 7)),
                      lambda s_, hx: ["wk2", ("hs", hx)], B.ropeC, B.ropeS,
                      lambda s_, kvh=kvh: [(KT2[kvh][:, s_ * 512:(s_ + 1) * 512], slice(0, 128))], ("KT", kvh), C.psw, C.idb,
                      pre=mk_pre(g.hkv, "hkv"))
        prev = mk_pre(g.hkv, "hkv")
        for sv in range(8):
            hx = prev(sv)
            sl = sv % 2
            for c in range(8):
                P.pe(lambda e, c=c, sl=sl, hx=hx: e.matmul(B.pq[sl][:, :], lhsT=wv[:, c, :], rhs=hs[hx][:, c, :],
                                                          start=(c == 0), stop=(c == 7)),
                     reads=[("hs", hx), "wv"], writes=[("PS", "bk", sl)])
            P.act(lambda e, sv=sv, sl=sl: e.copy(VTs[:, sv * 512:(sv + 1) * 512], B.pq[sl][:, :]),
                  reads=[("PS", "bk", sl)], writes=["VTs"])
        v_from_vt(P, B, 1, VTs, "VTs")
        for pair in range(8):
            qs = pair % 2
            rope_proj(P, nc, B, "D", 8,
                      lambda e, s_, c, o, hx, pair=pair: e.matmul(o, lhsT=wq[:, c, pair * 128:(pair + 1) * 128],
                                                                  rhs=hs[hx][:, c, :], start=(c == 0), stop=(c == 7)),
                      lambda s_, hx: ["wq1", ("hs", hx)], B.ropeC, B.ropeS,
                      lambda s_, qs=qs: [(QTd[qs][0:64, 0, s_ * 512:(s_ + 1) * 512], slice(0, 64)),
                                         (QTd[qs][64:128, 1, s_ * 512:(s_ + 1) * 512], slice(64, 128))], ("QT", qs), C.psw, C.idb,
                      pre=mk_pre(g.hat, "hat"))
            kvh = pair // 4

            def evac(hh, k, pvb, pvkey, pair=pair, qs=qs):
                head = pair * 2 + hh
                cs = slice(k * 512, (k + 1) * 512)
                P.act(lambda e: e.activation(out=tmp[64:128, :], in_=pvb[64:128, :], func=AF.Ln, bias=esk[64:128, head:head + 1]),
                      reads=[pvkey, "esk"], writes=["tmp"])
                P.act(lambda e: e.activation(out=rec[0:64, :], in_=tmp[64:128, :], func=AF.Exp, scale=-1.0),
                      reads=["tmp"], writes=["rec"])
                P.dve(lambda e: e.tensor_tensor(obf[qs][hh * 64:(hh + 1) * 64, cs], pvb[0:64, :], rec[0:64, :], ALU.mult),
                      reads=[pvkey, "rec"], writes=[("obf", qs)])

            attention(P, nc, B, "D", 1,
                      lambda r, n0, cnt, kvh=kvh: KT2[kvh][:, n0:n0 + cnt],
                      lambda r, n0, cnt, qs=qs: QTd[qs][:, :, n0:n0 + cnt],
                      lambda hh, b, kvh=kvh: B.V[:, b, kvh, :],
                      [("KT", kvh), "V"], [("QT", qs)], C.masks[:, 1, :], evac, C.idb)
            P.dma("sp", lambda e, pair=pair, qs=qs: e.dma_start(out=g.oT[pair, :, :], in_=obf[qs][:]), reads=[("obf", qs)],
                  writes=[("oT", pair)], semkey=("obf_st1", qs))
        P.fence()
        P.flush(nc)


def phase_F1(P, nc, g, C):
    tag = "F1"
    NH = 14
    with ExitStack() as es:
        sb = lambda n, s, d: es.enter_context(_sbt(nc, f"{tag}_{n}", s, d))
        ps = lambda n, s, d: es.enter_context(_pst(nc, f"{tag}_{n}", s, d))
        xg = sb("xg", [128, 8, D], F32)
        hTg = [sb(f"hTg{i}", [128, 8, 1024], BF16) for i in range(2)]
        actT = sb("actT", [128, NH, 1024], BF16)
        w2h = [sb(f"w2h{i}", [128, NH, D], BF16) for i in range(2)]
        wb1 = [sb(f"wb1{i}", [128, 8, 512], BF16) for i in range(2)]
        wb3 = [sb(f"wb3{i}", [128, 8, 512], BF16) for i in range(2)]
        sa = [sb(f"sa{i}", [128, 512], BF16) for i in range(2)]
        pa = [ps(f"pa{i}", [128, 512], F32) for i in range(2)]
        pb = [ps(f"pb{i}", [128, 512], F32) for i in range(2)]
        py = [ps(f"py{i}", [128, D], F32) for i in range(2)]
        wit = {"w": 0, "p": 0}
        it = 0
        pyc = 0
        for gq in range(4):
            gs = gq % 2
            P.dma("sp", lambda e, gq=gq, gs=gs: e.dma_start(out=hTg[gs][:], in_=g.hsc[:, :, gq * 1024:(gq + 1) * 1024]),
                  reads=[("hsc", gq)], writes=[(tag, "hTg", gs)], semkey=(tag, "hTg", gs))
            for q in range(2):
                P.dma("sp", lambda e, gq=gq, q=q: e.dma_start(
                    out=xg[:, q * 4:(q + 1) * 4, :],
                    in_=g.xs[gq * 1024 + q * 512:gq * 1024 + (q + 1) * 512, :].rearrange("(t p) n -> p t n", p=128)),
                    reads=[("xs", gq * 8 + q * 4 + i) for i in range(4)], writes=[(tag, "xg")], semkey=(tag, "xg", q))
            for ex in range(NEXP):
                for hf in range(2):
                    w2s = it % 2
                    it += 1
                    f0 = hf * NH * 128
                    w2v = g.w2m[ex].rearrange("(j p) n -> p j n", p=128)
                    for q in range(0, NH, 7):
                        P.dma("pool", lambda e, q=q, w2s=w2s, w2v=w2v, hf=hf: e.dma_start(
                            out=w2h[w2s][:, q:q + 7, :], in_=w2v[:, hf * NH + q:hf * NH + q + 7, :]),
                            writes=[(tag, "w2h", w2s)], semkey=(tag, "w2h", w2s, q))
                    w1v = g.w1m[ex].rearrange("(c p) n -> p c n", p=128)
                    w3v = g.w3m[ex].rearrange("(c p) n -> p c n", p=128)
                    ffn_stage(P, nc, tag, hTg[gs], (tag, "hTg", gs), NH,
                              lambda c0, n, w1v=w1v, f0=f0: w1v[:, :, f0 + c0:f0 + c0 + n],
                              lambda c0, n, w3v=w3v, f0=f0: w3v[:, :, f0 + c0:f0 + c0 + n],
                              wb1, wb3, pa, pb, sa, actT, wit)
                    for tt in range(8):
                        t = gq * 8 + tt
                        pbk = pyc % 2
                        pyc += 1
                        for half in range(2):
                            for j in range(NH):
                                P.pe(lambda e, j=j, half=half, tt=tt, pbk=pbk, w2s=w2s: e.matmul(
                                    py[pbk][:, half * 512:(half + 1) * 512], lhsT=actT[:, j, tt * 128:(tt + 1) * 128],
                                    rhs=w2h[w2s][:, j, half * 512:(half + 1) * 512], start=(j == 0), stop=(j == NH - 1)),
                                    reads=[(tag, "actT"), (tag, "w2h", w2s)], writes=[("PS", tag, "py", pbk)])
                        P.dve(lambda e, tt=tt, t=t, pbk=pbk, ex=ex: e.scalar_tensor_tensor(
                            out=xg[:, tt, :], in0=py[pbk][:, :], scalar=C.gates[:, t, ex:ex + 1], in1=xg[:, tt, :],
                            op0=ALU.mult, op1=ALU.add), reads=[("PS", tag, "py", pbk), "gates", (tag, "xg")], writes=[(tag, "xg")])
            for q in range(2):
                P.dma("sp", lambda e, gq=gq, q=q: e.dma_start(
                    out=g.xs[gq * 1024 + q * 512:gq * 1024 + (q + 1) * 512, :].rearrange("(t p) n -> p t n", p=128),
                    in_=xg[:, q * 4:(q + 1) * 4, :]),
                    reads=[(tag, "xg")], writes=[("xs", gq * 8 + q * 4 + i) for i in range(4)], semkey=(tag, "xgst", q))
        P.fence()
        P.flush(nc)


STOP = {"n": 99}
import os, json
if os.environ.get("KSTOP"):
    STOP.update(json.loads(os.environ["KSTOP"]))


def program(P, nc, g):
    with ExitStack() as es:
        C = load_consts(nc, es, P, g)
        stages = [
            lambda: phase_A(P, nc, g, C),
            lambda: phase_B(P, nc, g, C, g.wo0, g.x, 1, g.hsc),
            lambda: phase_C1(P, nc, g, C),
            lambda: phase_PLE(P, nc, g, C, 0, 2, False),
            lambda: phase_D(P, nc, g, C),
            lambda: phase_B(P, nc, g, C, g.wo1, g.xs, 5, g.hsc, router=True),
            lambda: phase_F1(P, nc, g, C),
            lambda: phase_PLE(P, nc, g, C, 1, 6, True),
        ]
        nst = min(STOP["n"], len(stages))
        for st in stages[:nst]:
            st()
        if nst < len(stages):
            for q in range(4):
                P.dma("sp", lambda e, q=q: e.dma_start(out=g.out[q * 1024:(q + 1) * 1024, :], in_=g.xs[q * 1024:(q + 1) * 1024, :]),
                      reads=[("xs", q * 8 + i) for i in range(8)], writes=[("out", q * 8 + i) for i in range(8)],
                      semkey=("dbg", q))
        P.add("sp", lambda e: e.nop(), reads=[("out", t) for t in range(NT)])
        P.flush(nc)


def build_nc():
    nc = bass.Bass("TRN2", target_bir_lowering=False)
    g = declare_dram(nc)
    P = Prog()
    program(P, nc, g)
    P.analyze()
    P.mode = "emit"
    P.count = 0
    with ExitStack() as es:
        P.alloc_sems(nc, es)
        program(P, nc, g)
    return nc


def _consts():
    half = 32
    inv = (1.0 / (np.float32(10000.0) ** (np.arange(half, dtype=np.float32) / np.float32(half)))).astype(np.float32)
    pos = np.arange(T, dtype=np.float32)
    ang = (pos[:, None] * inv[None, :]).astype(np.float32)
    cos = np.cos(ang).astype(np.float32).T
    sin = np.sin(ang).astype(np.float32).T
    Ct = np.ascontiguousarray(np.tile(cos, (4, 1)))
    sign = np.where((np.arange(128) % 64) < 32, -1.0, 1.0).astype(np.float32)[:, None]
    St = np.ascontiguousarray(np.tile(sin, (4, 1)) * (-sign))
    k = np.arange(128)[:, None]
    q = np.arange(128)[None, :]
    m_a = (q >= k)
    masks = np.stack([np.concatenate([m_a, q <= k], 1), np.concatenate([m_a, q < k], 1)]).astype(np.float32)
    ident = np.eye(128, dtype=np.float32)
    m = np.arange(128)
    sw = np.where((m % 64) < 32, m + 32, m - 32)
    pswap = np.zeros((128, 128), np.float32)
    pswap[sw, m] = 1.0
    return Ct, St, masks, ident, pswap


def make_in_maps(inp):
    f = lambda a: np.ascontiguousarray(np.asarray(a, dtype=np.float32))
    Ct, St, masks, ident, pswap = _consts()
    gl = [inp["attn_norm"][0], inp["ffn_norm"][0], inp["ple_norm"][0], inp["kv_norm"], inp["attn_norm"][1],
          inp["ffn_norm"][1], inp["ple_norm"][1]]
    gT = np.concatenate([f(v).reshape(8, 128).T for v in gl], axis=1)
    shared = dict(
        wqkv=f(inp["a_w_qkv"][0]), wo0=f(inp["a_w_o"][0]), kvw=f(inp["kv_w"]), wq1=f(inp["b_w_q"][0]),
        wo1=f(inp["b_w_o"][0]), w1d=f(inp["dense_w1"][0]), w3d=f(inp["dense_w3"][0]), w2d=f(inp["dense_w2"][0]),
        wr=f(inp["moe_router"][0]), w1m=f(inp["moe_w1"][0]), w3m=f(inp["moe_w3"][0]), w2m=f(inp["moe_w2"][0]),
        wg=f(inp["ple_w_gate"]), wp=f(inp["ple_w_proj"]), gT=f(gT),
        fgain=f(np.broadcast_to(f(inp["final_norm"])[None, :], (128, D))),
        sinks=f(np.broadcast_to(f(inp["b_sinks"][0])[None, :], (128, 16))),
        ropeC=Ct, ropeS=St, masks=masks, ident=ident, pswap=pswap)
    x = np.asarray(inp["x"], dtype=np.float32)
    p = np.asarray(inp["p"], dtype=np.float32)
    maps = []
    for b in range(x.shape[0]):
        m = dict(shared)
        m["x"] = f(x[b])
        m["pT"] = f(np.transpose(p[:, b], (0, 2, 1)))
        maps.append(m)
    return maps


_NC_CACHE = {}


def kernel(**inputs):
    maps = make_in_maps(inputs)
    if "nc" not in _NC_CACHE:
        _NC_CACHE["nc"] = build_nc()
    nc = _NC_CACHE["nc"]
    res = run_bass_kernel_spmd(nc, maps, core_ids=list(range(len(maps))))
    return np.stack([np.asarray(r["out"], dtype=np.float32) for r in res.results], axis=0)
```

```python
import numpy as np
from contextlib import ExitStack
import concourse.bass as bass
import concourse.mybir as mybir
from concourse.bass_utils import run_bass_kernel_spmd

F32 = mybir.dt.float32
BF16 = mybir.dt.bfloat16
AF = mybir.ActivationFunctionType
ALU = mybir.AluOpType
AX = mybir.AxisListType

T = 4096
D = 1024
NT = T // 128
DFF = 2816
DEXP = 3584
NEXP = 8
EPS = 1e-6
COMPUTE = ("pe", "act", "dve", "pool")
EPOCH = 20000
A_CONFIGS = ((128, 1), (512, 4), (2048, 16))


class _Op:
    __slots__ = ("eng", "reads", "writes", "dma", "semkey", "waits", "sig", "idx", "fence")

    def __init__(self, eng, reads, writes, dma, semkey):
        self.eng = eng
        self.reads = reads
        self.writes = writes
        self.dma = dma
        self.semkey = semkey
        self.waits = None
        self.sig = None
        self.fence = False


class Prog:
    def __init__(self):
        self.meta = []
        self.mode = "record"
        self.count = 0
        self.cur = []
        self.sems = None
        self.phase_map = {}

    def add(self, eng, fn, reads=(), writes=(), dma=False, semkey=None):
        if self.mode == "record":
            if dma:
                pm = self.phase_map.setdefault(eng, {})
                semkey = ("dq", eng, pm.setdefault(semkey, len(pm)))
            op = _Op(eng, tuple(reads), tuple(writes), dma, semkey)
            op.idx = len(self.meta)
            self.meta.append(op)
        else:
            op = self.meta[self.count]
            assert op.eng == eng and op.dma == dma, (op.idx, op.eng, eng)
            self.cur.append((op, fn))
        self.count += 1

    def pe(self, fn, reads=(), writes=()):
        self.add("pe", fn, reads, writes)

    def act(self, fn, reads=(), writes=()):
        self.add("act", fn, reads, writes)

    def dve(self, fn, reads=(), writes=()):
        self.add("dve", fn, reads, writes)

    def pool(self, fn, reads=(), writes=()):
        self.add("pool", fn, reads, writes)

    def dma(self, q, fn, reads=(), writes=(), semkey=None):
        assert semkey is not None
        self.add(q, fn, reads, writes, dma=True, semkey=semkey)

    def fence(self):
        if self.mode == "record":
            op = _Op(None, (), (), False, None)
            op.fence = True
            op.idx = len(self.meta)
            self.meta.append(op)
            self.phase_map = {}
        self.count += 1

    def analyze(self):
        ops = self.meta
        last_w, readers = {}, {}
        last_dma_on_sem, dma_count = {}, {}
        last_on_eng = {}
        pending_fence = {}
        needs_sig = [False] * len(ops)
        deps_list = [None] * len(ops)
        for op in ops:
            i = op.idx
            if op.fence:
                fd = set(last_on_eng.values()) | set(last_dma_on_sem.values())
                for e in COMPUTE + ("sp",):
                    pending_fence[e] = set(fd)
                deps_list[i] = []
                continue
            raw, other = set(), set()
            xr = tuple(r for r in op.reads if isinstance(r, tuple) and r[0] == "PS" and r not in op.writes)
            for r in op.reads:
                w = last_w.get(r)
                if w is not None:
                    raw.add(w)
            for w_ in op.writes + xr:
                w = last_w.get(w_)
                if w is not None:
                    other.add(w)
                for rd in readers.get(w_, ()):
                    other.add(rd)
            if op.eng in pending_fence:
                raw |= pending_fence.pop(op.eng)
            if op.dma:
                p = last_dma_on_sem.get(op.semkey)
                if p is not None:
                    raw.add(p)
                last_dma_on_sem[op.semkey] = i
                dma_count[op.semkey] = dma_count.get(op.semkey, 0) + 1
                op.sig = ("d", op.semkey, dma_count[op.semkey] * 16)
            else:
                last_on_eng[op.eng] = i
            deps = set()
            for d in raw | other:
                if d == i:
                    continue
                dop = ops[d]
                if dop.dma:
                    deps.add(d)
                    continue
                if (not op.dma) and dop.eng == op.eng:
                    if op.eng != "pe":
                        deps.add(d)
                    continue
                deps.add(d)
            best, final = {}, []
            for d in deps:
                dop = ops[d]
                if dop.dma:
                    final.append(d)
                elif dop.eng not in best or best[dop.eng] < d:
                    best[dop.eng] = d
            final.extend(best.values())
            for d in final:
                needs_sig[d] = True
            deps_list[i] = final
            for r in op.reads:
                readers.setdefault(r, []).append(i)
            for w_ in op.writes + xr:
                last_w[w_] = i
                readers[w_] = []
        seq = {e: 0 for e in COMPUTE + ("sp",)}
        for op in ops:
            if op.fence or op.dma:
                continue
            if needs_sig[op.idx]:
                seq[op.eng] += 1
                s = seq[op.eng]
                op.sig = ("e", op.eng, (s - 1) // EPOCH, s - ((s - 1) // EPOCH) * EPOCH)
        known = {}
        for op in ops:
            if op.fence:
                continue
            ws = []
            kn = known.setdefault(op.eng, {})
            for d in deps_list[op.idx]:
                sg = ops[d].sig
                if sg[0] == "d":
                    key, val = ("d", sg[1]), sg[2]
                else:
                    key, val = ("e", sg[1], sg[2]), sg[3]
                if kn.get(key, 0) >= val:
                    continue
                kn[key] = val
                ws.append((key, val))
            op.waits = ws
        self.n_epochs = {e: (seq[e] + EPOCH - 1) // EPOCH for e in seq}
        self.semkeys = list(dma_count.keys())

    def alloc_sems(self, nc, es):
        sems = {}
        for e, n in self.n_epochs.items():
            for k in range(n):
                sems[("e", e, k)] = es.enter_context(nc.semaphore(f"s_{e}_{k}"))
        for j, sk in enumerate(self.semkeys):
            sems[("d", sk)] = es.enter_context(nc.semaphore(f"d_{j}"))
        self.sems = sems

    def flush(self, nc):
        if self.mode != "emit" or not self.cur:
            self.cur = []
            return
        per = {}
        for op, fn in self.cur:
            per.setdefault(op.eng, []).append((op, fn))
        self.cur = []
        sems = self.sems

        def run(engobj, lst):
            for op, fn in lst:
                for key, val in op.waits:
                    engobj.wait_ge(sems[key], val)
                ins = fn(engobj)
                sg = op.sig
                if sg is not None:
                    if sg[0] == "d":
                        ins.then_inc(sems[("d", sg[1])], 16)
                    else:
                        ins.then_inc(sems[("e", sg[1], sg[2])], 1)

        with nc.Block() as block:
            if "pe" in per:
                @block.tensor
                def _(t):
                    run(t, per["pe"])
            if "act" in per:
                @block.scalar
                def _(a):
                    run(a, per["act"])
            if "dve" in per:
                @block.vector
                def _(v):
                    run(v, per["dve"])
            if "pool" in per:
                @block.gpsimd
                def _(g):
                    run(g, per["pool"])
            if "sp" in per:
                @block.sync
                def _(s):
                    run(s, per["sp"])


_UID = [0]


def _sbt(nc, name, shape, dt):
    _UID[0] += 1
    return nc.sbuf_tensor(f"{name}_u{_UID[0]}", shape, dt)


def _pst(nc, name, shape, dt):
    _UID[0] += 1
    return nc.psum_tensor(f"{name}_u{_UID[0]}", shape, dt)


def tokview(ap2d, dil):
    if dil == 1:
        return ap2d.unsqueeze(1)
    return ap2d.rearrange("p (m d) -> p d m", d=dil)


class Ctx:
    pass


def declare_dram(nc):
    g = Ctx()
    ei = lambda n, s: nc.dram_tensor(n, s, F32, kind="ExternalInput").ap()
    g.x = ei("x", [T, D])
    g.pT = ei("pT", [2, 256, T])
    g.wqkv = ei("wqkv", [D, 9216])
    g.wo0 = ei("wo0", [D, D])
    g.kvw = ei("kvw", [D, 256])
    g.wq1 = ei("wq1", [D, D])
    g.wo1 = ei("wo1", [D, D])
    g.w1d = ei("w1d", [D, DFF])
    g.w3d = ei("w3d", [D, DFF])
    g.w2d = ei("w2d", [DFF, D])
    g.wr = ei("wr", [D, NEXP])
    g.w1m = ei("w1m", [NEXP, D, DEXP])
    g.w3m = ei("w3m", [NEXP, D, DEXP])
    g.w2m = ei("w2m", [NEXP, DEXP, D])
    g.wg = ei("wg", [2, D, D])
    g.wp = ei("wp", [2, 256, D])
    g.gT = ei("gT", [128, 7 * 8])
    g.fgain = ei("fgain", [128, D])
    g.sinks = ei("sinks", [128, 16])
    g.ropeC = ei("ropeC", [128, T])
    g.ropeS = ei("ropeS", [128, T])
    g.masks = ei("masks", [2, 128, 256])
    g.ident = ei("ident", [128, 128])
    g.pswap = ei("pswap", [128, 128])
    g.out = nc.dram_tensor("out", [T, D], F32, kind="ExternalOutput").ap()
    g.xs = nc.dram_tensor("xs", [T, D], F32).ap()
    g.oT = nc.dram_tensor("oTs", [8, 128, T], BF16).ap()
    g.hsc = nc.dram_tensor("hsc", [128, 8, T], BF16).ap()
    g.hkv = nc.dram_tensor("hkv", [128, 8, T], BF16).ap()
    g.hat = nc.dram_tensor("hat", [128, 8, T], BF16).ap()
    return g


class NormUnit:
    def __init__(self, nc, es, P, tag, idb, gT, nslots=2):
        sb = lambda n, s, d: es.enter_context(_sbt(nc, f"{tag}_{n}", s, d))
        self.P = P
        self.tag = tag
        self.idb = idb
        self.gT = gT
        self.ns = nslots
        self.junk = sb("junk", [128, D], BF16)
        self.ss = sb("ss", [128, nslots], F32)
        self.rr = sb("rr", [128, nslots], F32)
        self.eps = sb("eps", [128, 1], F32)
        self.xn = [sb(f"xn{i}", [128, D], BF16) for i in range(nslots)]
        self.pt = [es.enter_context(_pst(nc, f"{tag}_pt{i}", [128, 8, 128], BF16)) for i in range(2)]
        self.n = 0
        self.pn = 0
        P.pool(lambda e: e.memset(self.eps[:], EPS), writes=[(tag, "eps")])

    def stats(self, xt, xkey):
        P, tag = self.P, self.tag
        s = self.n % self.ns
        self.n += 1
        P.act(lambda e: e.activation(out=self.junk[:], in_=xt, func=AF.Square, accum_out=self.ss[:, s:s + 1]),
              reads=[xkey], writes=[(tag, "junk"), (tag, "ss", s)])
        P.act(lambda e: e.activation(out=self.rr[:, s:s + 1], in_=self.ss[:, s:s + 1], func=AF.Sqrt,
                                     scale=1.0 / D, bias=self.eps[:]),
              reads=[(tag, "ss", s), (tag, "eps")], writes=[(tag, "rr", s)])
        P.dve(lambda e: e.reciprocal(self.rr[:, s:s + 1], self.rr[:, s:s + 1]),
              reads=[(tag, "rr", s)], writes=[(tag, "rr", s)])
        return s

    def run_a(self, xt, xkey):
        P, tag = self.P, self.tag
        s = self.stats(xt, xkey)
        xn = self.xn[s]
        P.dve(lambda e: e.tensor_scalar(xn[:], xt, self.rr[:, s:s + 1], None, ALU.mult),
              reads=[xkey, (tag, "rr", s)], writes=[(tag, "xn", s)])
        return s

    def run_b(self, s, outs):
        P, tag = self.P, self.tag
        xn = self.xn[s]
        ps = self.pn % len(self.pt)
        self.pn += 1
        pt = self.pt[ps]
        for c in range(8):
            P.pe(lambda e, c=c: e.transpose(pt[:, c, :], xn[:, c * 128:(c + 1) * 128], self.idb[:]),
                 reads=[(tag, "xn", s), "idb"], writes=[("PS", tag, "pt", ps)])
        for gi, oap, okey in outs:
            P.dve(lambda e, gi=gi, oap=oap: e.tensor_tensor(
                oap, pt[:], self.gT[:, gi * 8:(gi + 1) * 8].unsqueeze(2).to_broadcast([128, 8, 128]), ALU.mult),
                reads=[("PS", tag, "pt", ps), "gT"], writes=[okey])
        return ps

    def run(self, xt, xkey, outs):
        s = self.run_a(xt, xkey)
        self.run_b(s, outs)
        return s


def load_consts(nc, es, P, g):
    sb = lambda n, s, d: es.enter_context(_sbt(nc, n, s, d))
    c = Ctx()
    c.idb = sb("idb", [128, 128], BF16)
    c.psw = sb("pswb", [128, 128], BF16)
    c.gT = sb("gTs", [128, 56], F32)
    c.masks = sb("masksb", [128, 2, 256], BF16)
    c.gates = sb("gates", [128, NT, NEXP], F32)
    P.dma("pool", lambda e: e.dma_start(out=c.idb[:], in_=g.ident[:, :]), writes=["idb"], semkey="c_idb")
    P.dma("pool", lambda e: e.dma_start(out=c.psw[:], in_=g.pswap[:, :]), writes=["psw"], semkey="c_psw")
    P.dma("sp", lambda e: e.dma_start(out=c.gT[:], in_=g.gT[:, :]), writes=["gT"], semkey="c_gT")
    for l in range(2):
        P.dma("pool", lambda e, l=l: e.dma_start(out=c.masks[:, l, :], in_=g.masks[l, :, :]),
              writes=["masks"], semkey=("c_mask", l))
    return c


def rope_proj(P, nc, B, tag, nslab, mm_fn, mm_reads, Ctab, Stab, out_fn, out_key, psw, idb, col0=0, pre=None):
    L = 1

    def stage1(s):
        sl = s % 2
        cs = slice(col0 + s * 512, col0 + (s + 1) * 512)
        hx = pre(s) if pre is not None else None
        for c in range(8):
            if pre is not None:
                P.pe(lambda e, s=s, c=c, sl=sl, hx=hx: mm_fn(e, s, c, B.pq[sl][:, :], hx), reads=mm_reads(s, hx),
                     writes=[("PS", "bk", sl)])
            else:
                P.pe(lambda e, s=s, c=c, sl=sl: mm_fn(e, s, c, B.pq[sl][:, :]), reads=mm_reads(s), writes=[("PS", "bk", sl)])
        P.dve(lambda e, sl=sl, cs=cs: e.tensor_tensor(B.ua[sl][:], B.pq[sl][:, :], Ctab[:, cs], ALU.mult),
              reads=[("PS", "bk", sl), "ropeC"], writes=[("ua", sl)])
        P.dve(lambda e, sl=sl, cs=cs: e.tensor_tensor(B.ub[sl][:], B.pq[sl][:, :], Stab[:, cs], ALU.mult),
              reads=[("PS", "bk", sl), "ropeS"], writes=[("ub", sl)])

    def stage2(s):
        sl = s % 2
        P.pe(lambda e, sl=sl: e.matmul(B.psw[sl][:, :], lhsT=idb[:], rhs=B.ua[sl][:], start=True, stop=False),
             reads=[("ua", sl), "idb"], writes=[("PS", "bk", 2 + sl)])
        P.pe(lambda e, sl=sl: e.matmul(B.psw[sl][:, :], lhsT=psw[:], rhs=B.ub[sl][:], start=False, stop=True),
             reads=[("ub", sl), "psw"], writes=[("PS", "bk", 2 + sl)])
        for oap, rows in out_fn(s):
            P.act(lambda e, sl=sl, oap=oap, rows=rows: e.copy(oap, B.psw[sl][rows, :]), reads=[("PS", "bk", 2 + sl)],
                  writes=[out_key])

    for i in range(nslab + L):
        if i < nslab:
            stage1(i)
        if i >= L:
            stage2(i - L)


def attention(P, nc, B, tag, dil, kt_fn, qt_fn, v_fn, kv_reads, q_reads, mask, evac_fn, idb, LOOK=3):
    nsub = T // dil
    nb = nsub // 128
    blocks = [(r, n) for r in range(dil) for n in range(nb)]

    def s1(idx):
        r, n = blocks[idx]
        nq = 256 if n < nb - 1 else 128
        sl = idx % 4
        stv = B.st[sl][:, :].rearrange("p (h q) -> p h q", h=2)[:, :, 0:nq]
        ptv = B.pt[sl][:, :, 0:nq]
        if nq == 256:
            P.pe(lambda e: e.matmul(stv, lhsT=kt_fn(r, 128 * n, 128), rhs=qt_fn(r, 128 * n, nq),
                                    start=True, stop=True), reads=kv_reads + q_reads, writes=[("PS", "bk", sl)])
        else:
            for hh in range(2):
                P.pe(lambda e, hh=hh: e.matmul(stv[:, hh, :], lhsT=kt_fn(r, 128 * n, 128), rhs=qt_fn(r, 128 * n, nq)[:, hh, :],
                                               start=True, stop=True), reads=kv_reads + q_reads, writes=[("PS", "bk", sl)])
        P.act(lambda e: e.activation(out=ptv, in_=stv, func=AF.Exp, scale=0.125),
              reads=[("PS", "bk", sl)], writes=[("ptile", sl)])
        P.dve(lambda e: e.tensor_tensor(ptv, ptv, mask[:, 0:nq].unsqueeze(1).to_broadcast([128, 2, nq]), ALU.mult),
              reads=[("ptile", sl), "masks"], writes=[("ptile", sl)])

    def s2(idx):
        r, n = blocks[idx]
        b = r * nb + n
        sl = idx % 4
        bank, pos = (b // 4) % 2, b % 4
        for hh in range(2):
            kb = 4 + hh * 2 + bank
            P.pe(lambda e, hh=hh, kb=kb: e.matmul(B.bk[kb][:, pos * 128:(pos + 1) * 128], lhsT=v_fn(hh, b),
                                                  rhs=B.pt[sl][:, hh, 0:128], start=(n == 0), stop=True),
                 reads=kv_reads + [("ptile", sl)], writes=[("PS", "bk", kb)])
            if n < nb - 1:
                b2 = b + 1
                bank2, pos2 = (b2 // 4) % 2, b2 % 4
                kb2 = 4 + hh * 2 + bank2
                P.pe(lambda e, hh=hh, kb2=kb2, pos2=pos2: e.matmul(
                    B.bk[kb2][:, pos2 * 128:(pos2 + 1) * 128], lhsT=v_fn(hh, b), rhs=B.pt[sl][:, hh, 128:256],
                    start=True, stop=False), reads=kv_reads + [("ptile", sl)], writes=[("PS", "bk", kb2)])
            if pos == 3:
                evac_fn(hh, b // 4, B.bk[kb], ("PS", "bk", kb))

    nblk = len(blocks)
    for i in range(nblk + LOOK):
        if i < nblk:
            s1(i)
        if i >= LOOK:
            s2(i - LOOK)


def v_from_vt(P, B, dil, VTs, vkey):
    nb = T // dil // 128
    vv = tokview(VTs[:, :], dil)
    for b in range(32):
        r, n = b // nb, b % nb
        half, pos = (b // 4) % 2, b % 4
        P.pe(lambda e, r=r, n=n, half=half, pos=pos: e.transpose(
            B.vtr[half][:, pos, :], vv[:, r, 128 * n:128 * (n + 1)], B.idb[:]),
            reads=[vkey, "idb"], writes=[("PS", "bk", 2 + half)])
        if pos == 3:
            b0 = b - 3
            P.dve(lambda e, b0=b0, half=half: e.tensor_copy(
                B.V[:, b0:b0 + 4, :, 0:64], B.vtr[half].rearrange("p b (h d) -> p b h d", h=2)),
                reads=[("PS", "bk", 2 + half)], writes=["V"])


def attn_buffers(nc, es, tag):
    sb = lambda n, s, d: es.enter_context(_sbt(nc, f"{tag}_{n}", s, d))
    ps = lambda n, s, d: es.enter_context(_pst(nc, f"{tag}_{n}", s, d))
    B = Ctx()
    bk = [ps(f"bk{i}", [128, 512], F32) for i in range(8)]
    B.bk = bk
    B.pq = [bk[0], bk[1]]
    B.psw = [bk[2], bk[3]]
    B.st = [bk[i] for i in range(4)]
    B.vtr = [bk[2 + i][:, :].bitcast(BF16)[:, 0:512].rearrange("p (b d) -> p b d", b=4) for i in range(2)]
    B.ua = [sb(f"ua{i}", [128, 512], BF16) for i in range(2)]
    B.ub = [sb(f"ub{i}", [128, 512], BF16) for i in range(2)]
    B.pt = [sb(f"ptl{i}", [128, 2, 256], BF16) for i in range(4)]
    B.ropeC = sb("ropeC", [128, T], BF16)
    B.ropeS = sb("ropeS", [128, T], BF16)
    B.V = sb("V", [128, 32, 2, 128], BF16)
    return B


def load_rope(P, g, B):
    for h in range(2):
        cs = slice(h * 2048, (h + 1) * 2048)
        P.dma("pool", lambda e, cs=cs: e.dma_start(out=B.ropeC[:, cs], in_=g.ropeC[:, cs]), writes=["ropeC"],
              semkey=("ropeC", h))
        P.dma("pool", lambda e, cs=cs: e.dma_start(out=B.ropeS[:, cs], in_=g.ropeS[:, cs]), writes=["ropeS"],
              semkey=("ropeS", h))


def phase_A(P, nc, g, C):
    with ExitStack() as es:
        sb = lambda n, s, d: es.enter_context(_sbt(nc, n, s, d))
        hT0 = sb("A_hT0", [128, 8, T], BF16)
        with ExitStack() as es2:
            sb2 = lambda n, s, d: es2.enter_context(_sbt(nc, n, s, d))
            xt = [sb2(f"A_xt{i}", [128, D], F32) for i in range(3)]
            NU = NormUnit(nc, es2, P, "An", C.idb, C.gT)
            for t in range(NT):
                s = t % 3
                P.dma("sp", lambda e, t=t, s=s: e.dma_start(out=xt[s][:], in_=g.x[t * 128:(t + 1) * 128, :]),
                      writes=[("A_xt", s)], semkey=("A_xt", s))
                NU.run(xt[s][:], ("A_xt", s), [(0, hT0[:, :, t * 128:(t + 1) * 128], "hT0")])
            P.fence()
            P.flush(nc)
        B = attn_buffers(nc, es, "A")
        B.idb = C.idb
        load_rope(P, g, B)
        wq = [sb(f"A_wq{i}", [128, 8, 384], BF16) for i in range(2)]
        QTd = sb("A_QTd", [128, 2, T], BF16)
        KT = sb("A_KT", [128, T], BF16)
        VTs = sb("A_VTs", [128, T], BF16)
        P.pool(lambda e: e.memset(QTd[:], 0.0), writes=["QT"])
        acc = [sb(f"A_acc{i}", [128, T], F32) for i in range(2)]
        rec = sb("A_rec", [128, 512], F32)
        lnd = sb("A_lnd", [128, 512], F32)
        obf = sb("A_obf", [128, T], BF16)
        P.pool(lambda e: e.memset(B.V[:, :, :, 64:128], 1.0), writes=["V"])
        wv_ = g.wqkv.rearrange("(c p) n -> p c n", p=128)
        it = 0
        for pair in range(STOP.get("a_pairs", 8)):
            for gi, (win, dil) in enumerate(A_CONFIGS):
                ws = it % 2
                it += 1
                if it > STOP.get("a_iters", 99):
                    continue
                for k in range(3):
                    c0 = gi * 3072 + k * 1024 + pair * 128
                    P.dma("pool", lambda e, ws=ws, k=k, c0=c0: e.dma_start(
                        out=wq[ws][:, :, k * 128:(k + 1) * 128], in_=wv_[:, :, c0:c0 + 128]),
                        writes=[("wq", ws)], semkey=("wq", ws, k))
                nb = T // dil // 128
                rope_proj(P, nc, B, "A", 8,
                          lambda e, s, c, o, ws=ws: e.matmul(o, lhsT=wq[ws][:, c, 0:128], rhs=hT0[:, c, s * 512:(s + 1) * 512],
                                                             start=(c == 0), stop=(c == 7)),
                          lambda s, ws=ws: [("wq", ws), "hT0"], B.ropeC, B.ropeS,
                          lambda s: [(QTd[0:64, 0, s * 512:(s + 1) * 512], slice(0, 64)),
                                     (QTd[64:128, 1, s * 512:(s + 1) * 512], slice(64, 128))], "QT", C.psw, C.idb)
                rope_proj(P, nc, B, "A", 8,
                          lambda e, s, c, o, ws=ws: e.matmul(o, lhsT=wq[ws][:, c, 128:256], rhs=hT0[:, c, s * 512:(s + 1) * 512],
                                                             start=(c == 0), stop=(c == 7)),
                          lambda s, ws=ws: [("wq", ws), "hT0"], B.ropeC, B.ropeS,
                          lambda s: [(KT[:, s * 512:(s + 1) * 512], slice(0, 128))], "KT", C.psw, C.idb)
                if STOP.get("a_part", 9) < 1:
                    continue
                for sv in range(8):
                    sl = sv % 2
                    for c in range(8):
                        P.pe(lambda e, c=c, sv=sv, sl=sl, ws=ws: e.matmul(
                            B.pq[sl][:, :], lhsT=wq[ws][:, c, 256:384], rhs=hT0[:, c, sv * 512:(sv + 1) * 512],
                            start=(c == 0), stop=(c == 7)), reads=[("wq", ws), "hT0"], writes=[("PS", "bk", sl)])
                    P.act(lambda e, sv=sv, sl=sl: e.copy(VTs[:, sv * 512:(sv + 1) * 512], B.pq[sl][:, :]),
                          reads=[("PS", "bk", sl)], writes=["VTs"])
                v_from_vt(P, B, dil, VTs, "VTs")
                if STOP.get("a_part", 9) < 2:
                    continue
                def evac(hh, k, pvb, pvkey, gi=gi, dil=dil, nb=nb):
                    accv = tokview(acc[hh][:, :], dil)
                    if nb >= 4:
                        r, n0 = (4 * k) // nb, (4 * k) % nb
                        dst = accv[:, r, 128 * n0:128 * n0 + 512]
                        src = pvb[:, :]
                    else:
                        dst = accv[:, 2 * k:2 * k + 2, 0:256]
                        src = pvb[:, :].rearrange("p (a b) -> p a b", a=2)
                    if gi == 0:
                        P.dve(lambda e: e.tensor_copy(dst, src), reads=[pvkey], writes=[("acc", hh)])
                    else:
                        P.dve(lambda e: e.tensor_tensor(dst, src, dst, ALU.add), reads=[pvkey, ("acc", hh)],
                              writes=[("acc", hh)])

                if dil == 1:
                    qtf = lambda r, n0, cnt: QTd[:, :, n0:n0 + cnt]
                else:
                    qtf = lambda r, n0, cnt, dil=dil: QTd[:, :, :].rearrange("p h (m d) -> p h d m", d=dil)[:, :, r, n0:n0 + cnt]
                attention(P, nc, B, "A", dil,
                          lambda r, n0, cnt, dil=dil: tokview(KT[:, :], dil)[:, r, n0:n0 + cnt],
                          qtf, lambda hh, b: B.V[:, b, hh, :],
                          ["KT", "V"], ["QT"], C.masks[:, 0, :], evac, C.idb)
            for hh in range(2):
                for s in range(8):
                    cs = slice(s * 512, (s + 1) * 512)
                    P.act(lambda e, hh=hh, cs=cs: e.activation(out=lnd[64:128, :], in_=acc[hh][64:128, cs], func=AF.Ln),
                          reads=[("acc", hh)], writes=["lnd"])
                    P.act(lambda e: e.activation(out=rec[0:64, :], in_=lnd[64:128, :], func=AF.Exp, scale=-1.0),
                          reads=["lnd"], writes=["rec"])
                    P.dve(lambda e, hh=hh, cs=cs: e.tensor_tensor(obf[hh * 64:(hh + 1) * 64, cs], acc[hh][0:64, cs],
                                                                   rec[0:64, :], ALU.mult),
                           reads=[("acc", hh), "rec"], writes=["obf"])
            P.dma("sp", lambda e, pair=pair: e.dma_start(out=g.oT[pair, :, :], in_=obf[:]), reads=["obf"],
                  writes=[("oT", pair)], semkey="obf_st")
        P.fence()
        P.flush(nc)


def phase_B(P, nc, g, C, wo_dram, x_src, gain_idx, h_dst, router=False):
    tag = "B%d" % gain_idx
    with ExitStack() as es:
        sb = lambda n, s, d: es.enter_context(_sbt(nc, f"{tag}_{n}", s, d))
        ps = lambda n, s, d: es.enter_context(_pst(nc, f"{tag}_{n}", s, d))
        wo = sb("wo", [128, 8, D], BF16)
        og = [sb(f"og{i}", [128, 8, 1024], BF16) for i in range(2)]
        xt = [sb(f"xt{i}", [128, D], F32) for i in range(3)]
        x1 = [sb(f"x1{i}", [128, D], F32) for i in range(4)]
        hg = [sb(f"hg{i}", [128, 8, 1024], BF16) for i in range(2)]
        py = [ps(f"py{i}", [128, D], F32) for i in range(2)]
        NU = NormUnit(nc, es, P, tag + "n", C.idb, C.gT, nslots=3)
        wov = wo_dram.rearrange("(c p) n -> p c n", p=128)
        for h in range(2):
            P.dma("pool", lambda e, h=h: e.dma_start(out=wo[:, :, h * 512:(h + 1) * 512], in_=wov[:, :, h * 512:(h + 1) * 512]),
                  writes=[(tag, "wo")], semkey=(tag, "wo", h))
        if router:
            wr32 = sb("wr32", [128, 8, NEXP], F32)
            wrg = sb("wrg", [128, 8, NEXP], F32)
            wrh = sb("wrh", [128, 8, NEXP], BF16)
            wrh32 = sb("wrh32", [128, 8, NEXP], F32)
            wrl = sb("wrl", [128, 8, NEXP], BF16)
            xlo = [sb(f"xlo{i}", [128, D], BF16) for i in range(2)]
            hiT = [sb(f"hiT{i}", [128, 8, 128], BF16) for i in range(2)]
            loT = [sb(f"loT{i}", [128, 8, 128], BF16) for i in range(2)]
            plo = ps("plo", [128, 8, 128], BF16)
            plg = ps("plg", [128, NEXP], F32)
            lg = sb("lg", [128, NEXP], F32)
            l2 = sb("l2", [128, NEXP], F32)
            eq1 = sb("eq1", [128, NEXP], F32)
            eq2 = sb("eq2", [128, NEXP], F32)
            sm = sb("sm", [128, 8], F32)
            P.dma("sp", lambda e: e.dma_start(out=wr32[:], in_=g.wr.rearrange("(c p) n -> p c n", p=128)),
                  writes=["wr32"], semkey="wr32")
            P.dve(lambda e: e.tensor_tensor(wrg[:], wr32[:], C.gT[:, gain_idx * 8:(gain_idx + 1) * 8].unsqueeze(2).to_broadcast([128, 8, NEXP]), ALU.mult),
                  reads=["wr32", "gT"], writes=["wrg"])
            P.dve(lambda e: e.tensor_copy(wrh[:], wrg[:]), reads=["wrg"], writes=["wrh"])
            P.dve(lambda e: e.tensor_copy(wrh32[:], wrh[:]), reads=["wrh"], writes=["wrh32"])
            P.dve(lambda e: e.tensor_tensor(wrl[:], wrg[:], wrh32[:], ALU.subtract), reads=["wrg", "wrh32"], writes=["wrl"])
        slots = {}

        def stage1(t):
            gq, tt = t // 8, t % 8
            gs = gq % 2
            if tt == 0:
                P.dma("sp", lambda e: e.dma_start(
                    out=og[gs][:], in_=g.oT[:, :, gq * 1024:(gq + 1) * 1024].rearrange("c p t -> p c t")),
                    reads=[("oT", c) for c in range(8)], writes=[(tag, "og", gs)], semkey=(tag, "og", gs))
            sx = t % 3
            s = t % 4
            P.dma("sp", lambda e: e.dma_start(out=xt[sx][:], in_=x_src[t * 128:(t + 1) * 128, :]),
                  reads=[("xs", t)], writes=[(tag, "xt", sx)], semkey=(tag, "xt", sx))
            pb = t % 2
            for half in range(2):
                for c in range(8):
                    P.pe(lambda e, c=c, half=half: e.matmul(
                        py[pb][:, half * 512:(half + 1) * 512], lhsT=og[gs][:, c, tt * 128:(tt + 1) * 128],
                        rhs=wo[:, c, half * 512:(half + 1) * 512], start=(c == 0), stop=(c == 7)),
                        reads=[(tag, "og", gs), (tag, "wo")], writes=[("PS", tag, "py", pb)])
            P.dve(lambda e: e.tensor_tensor(x1[s][:], py[pb][:, :], xt[sx][:], ALU.add),
                  reads=[("PS", tag, "py", pb), (tag, "xt", sx)], writes=[(tag, "x1", s)])
            P.dma("pool", lambda e: e.dma_start(out=g.xs[t * 128:(t + 1) * 128, :], in_=x1[s][:]),
                  reads=[(tag, "x1", s)], writes=[("xs", t)], semkey=(tag, "x1st", s))

        def stage1b(t):
            s = t % 4
            slots[t] = NU.run_a(x1[s][:], (tag, "x1", s))

        def stage2(t):
            gq, tt = t // 8, t % 8
            gs = gq % 2
            s = t % 4
            ns = slots[t]
            ps_ = NU.run_b(ns, [(gain_idx, hg[gs][:, :, tt * 128:(tt + 1) * 128], (tag, "hg", gs))])
            if router:
                rs = t % 2
                ntag = tag + "n"
                P.act(lambda e: e.copy(hiT[rs][:], NU.pt[ps_][:]), reads=[("PS", ntag, "pt", ps_)], writes=[("hiT", rs)])
                P.dve(lambda e: e.scalar_tensor_tensor(
                    out=xlo[rs][:], in0=x1[s][:], scalar=NU.rr[:, ns:ns + 1], in1=NU.xn[ns][:], op0=ALU.mult, op1=ALU.subtract),
                    reads=[(tag, "x1", s), (ntag, "rr", ns), (ntag, "xn", ns)], writes=[("xlo", rs)])
                for c in range(8):
                    P.pe(lambda e, c=c: e.transpose(plo[:, c, :], xlo[rs][:, c * 128:(c + 1) * 128], C.idb[:]),
                         reads=[("xlo", rs), "idb"], writes=[("PS", "plo")])
                P.act(lambda e: e.copy(loT[rs][:], plo[:]), reads=[("PS", "plo")], writes=[("loT", rs)])
                k = 0
                for (aT, akey, wmat, wkey) in ((hiT, "hiT", wrh, "wrh"), (loT, "loT", wrh, "wrh"), (hiT, "hiT", wrl, "wrl")):
                    for c in range(8):
                        P.pe(lambda e, c=c, aT=aT, wmat=wmat, k=k: e.matmul(
                            plg[:, :], lhsT=aT[rs][:, c, :], rhs=wmat[:, c, :], start=(k == 0), stop=(k == 23)),
                            reads=[(akey, rs), wkey], writes=[("PS", "plg")])
                        k += 1
                P.dve(lambda e: e.tensor_copy(lg[:], plg[:, :]), reads=[("PS", "plg")], writes=["lg"])
                P.dve(lambda e: e.reduce_max(sm[:, 0:1], lg[:], axis=AX.X), reads=["lg"], writes=["sm0"])
                P.dve(lambda e: e.tensor_scalar(eq1[:], lg[:], sm[:, 0:1], None, ALU.is_equal), reads=["lg", "sm0"], writes=["eq1"])
                P.dve(lambda e: e.scalar_tensor_tensor(out=l2[:], in0=eq1[:], scalar=-1e30, in1=lg[:], op0=ALU.mult, op1=ALU.add),
                      reads=["eq1", "lg"], writes=["l2"])
                P.dve(lambda e: e.reduce_max(sm[:, 1:2], l2[:], axis=AX.X), reads=["l2"], writes=["sm1"])
                P.dve(lambda e: e.tensor_scalar(eq2[:], l2[:], sm[:, 1:2], None, ALU.is_equal), reads=["l2", "sm1"], writes=["eq2"])
                P.dve(lambda e: e.tensor_tensor(sm[:, 2:3], sm[:, 1:2], sm[:, 0:1], ALU.subtract), reads=["sm0", "sm1"], writes=["sm2"])
                P.act(lambda e: e.activation(out=sm[:, 3:4], in_=sm[:, 2:3], func=AF.Exp), reads=["sm2"], writes=["sm3"])
                P.dve(lambda e: e.tensor_scalar(sm[:, 4:5], sm[:, 3:4], 1.0, None, ALU.add), reads=["sm3"], writes=["sm4"])
                P.dve(lambda e: e.reciprocal(sm[:, 5:6], sm[:, 4:5]), reads=["sm4"], writes=["sm5"])
                P.dve(lambda e: e.tensor_tensor(sm[:, 6:7], sm[:, 3:4], sm[:, 5:6], ALU.mult), reads=["sm3", "sm5"], writes=["sm6"])
                P.dve(lambda e: e.tensor_scalar(eq1[:], eq1[:], sm[:, 5:6], None, ALU.mult), reads=["eq1", "sm5"], writes=["eq1"])
                P.dve(lambda e: e.scalar_tensor_tensor(out=C.gates[:, t, :], in0=eq2[:], scalar=sm[:, 6:7], in1=eq1[:],
                                                       op0=ALU.mult, op1=ALU.add),
                      reads=["eq2", "sm6", "eq1"], writes=["gates"])
            if tt == 7:
                P.dma("pool", lambda e: e.dma_start(out=h_dst[:, :, gq * 1024:(gq + 1) * 1024], in_=hg[gs][:]),
                      reads=[(tag, "hg", gs)], writes=[("hsc", gq)], semkey=(tag, "hgst", gs))

        for i in range(NT + 2):
            if i < NT:
                stage1(i)
            if 0 <= i - 1 < NT:
                stage1b(i - 1)
            if 0 <= i - 2 < NT:
                stage2(i - 2)
        P.fence()
        P.flush(nc)


def ffn_stage(P, nc, tag, hTg, hkey, nchunk, wsrc1, wsrc3, wb1, wb3, pa, pb, sa, actT, wit):
    nblk = (nchunk + 3) // 4
    cnt = 0
    for fb in range(nblk):
        ncols = min(512, nchunk * 128 - fb * 512)
        ws = wit["w"] % 2
        wit["w"] += 1
        P.dma("pool", lambda e, fb=fb, ncols=ncols, ws=ws: e.dma_start(out=wb1[ws][:, :, 0:ncols], in_=wsrc1(fb * 512, ncols)),
              writes=[(tag, "wb1", ws)], semkey=(tag, "wb1", ws))
        P.dma("pool", lambda e, fb=fb, ncols=ncols, ws=ws: e.dma_start(out=wb3[ws][:, :, 0:ncols], in_=wsrc3(fb * 512, ncols)),
              writes=[(tag, "wb3", ws)], semkey=(tag, "wb3", ws))
        for jj in range(ncols // 128):
            j = fb * 4 + jj
            for th in range(2):
                sl = wit["p"] % 2
                wit["p"] += 1
                ts_ = slice(th * 512, (th + 1) * 512)
                for c in range(8):
                    P.pe(lambda e, c=c, jj=jj, ws=ws, sl=sl, ts_=ts_: e.matmul(
                        pa[sl][:, :], lhsT=wb1[ws][:, c, jj * 128:(jj + 1) * 128], rhs=hTg[:, c, ts_],
                        start=(c == 0), stop=(c == 7)), reads=[(tag, "wb1", ws), hkey], writes=[("PS", tag, "pa", sl)])
                for c in range(8):
                    P.pe(lambda e, c=c, jj=jj, ws=ws, sl=sl, ts_=ts_: e.matmul(
                        pb[sl][:, :], lhsT=wb3[ws][:, c, jj * 128:(jj + 1) * 128], rhs=hTg[:, c, ts_],
                        start=(c == 0), stop=(c == 7)), reads=[(tag, "wb3", ws), hkey], writes=[("PS", tag, "pb", sl)])
                P.act(lambda e, sl=sl: e.activation(out=sa[sl][:], in_=pa[sl][:, :], func=AF.Silu),
                      reads=[("PS", tag, "pa", sl)], writes=[(tag, "sa", sl)])
                P.dve(lambda e, sl=sl, j=j, ts_=ts_: e.tensor_tensor(actT[:, j, ts_], sa[sl][:], pb[sl][:, :], ALU.mult),
                      reads=[(tag, "sa", sl), ("PS", tag, "pb", sl)], writes=[(tag, "actT")])


def phase_C1(P, nc, g, C):
    tag = "C1"
    NCH = DFF // 128
    with ExitStack() as es:
        sb = lambda n, s, d: es.enter_context(_sbt(nc, f"{tag}_{n}", s, d))
        ps = lambda n, s, d: es.enter_context(_pst(nc, f"{tag}_{n}", s, d))
        w2 = sb("w2", [128, NCH, D], BF16)
        hTg = [sb(f"hTg{i}", [128, 8, 1024], BF16) for i in range(2)]
        actT = sb("actT", [128, NCH, 1024], BF16)
        wb1 = [sb(f"wb1{i}", [128, 8, 512], BF16) for i in range(2)]
        wb3 = [sb(f"wb3{i}", [128, 8, 512], BF16) for i in range(2)]
        sa = [sb(f"sa{i}", [128, 512], BF16) for i in range(2)]
        xt = [sb(f"xt{i}", [128, D], F32) for i in range(3)]
        pa = [ps(f"pa{i}", [128, 512], F32) for i in range(2)]
        pb = [ps(f"pb{i}", [128, 512], F32) for i in range(2)]
        py = [ps(f"py{i}", [128, D], F32) for i in range(2)]
        w2v = g.w2d.rearrange("(j p) n -> p j n", p=128)
        for q in range(0, NCH, 4):
            n = min(4, NCH - q)
            P.dma("pool", lambda e, q=q, n=n: e.dma_start(out=w2[:, q:q + n, :], in_=w2v[:, q:q + n, :]),
                  writes=[(tag, "w2")], semkey=(tag, "w2", q))
        w1v = g.w1d.rearrange("(c p) n -> p c n", p=128)
        w3v = g.w3d.rearrange("(c p) n -> p c n", p=128)
        wit = {"w": 0, "p": 0}
        for gq in range(4):
            gs = gq % 2
            P.dma("sp", lambda e, gq=gq, gs=gs: e.dma_start(out=hTg[gs][:], in_=g.hsc[:, :, gq * 1024:(gq + 1) * 1024]),
                  reads=[("hsc", gq)], writes=[(tag, "hTg", gs)], semkey=(tag, "hTg", gs))
            ffn_stage(P, nc, tag, hTg[gs], (tag, "hTg", gs), NCH,
                      lambda c0, n: w1v[:, :, c0:c0 + n], lambda c0, n: w3v[:, :, c0:c0 + n],
                      wb1, wb3, pa, pb, sa, actT, wit)
            for tt in range(8):
                t = gq * 8 + tt
                s = t % 3
                pbk = t % 2
                P.dma("sp", lambda e, t=t, s=s: e.dma_start(out=xt[s][:], in_=g.xs[t * 128:(t + 1) * 128, :]),
                      reads=[("xs", t)], writes=[(tag, "xt", s)], semkey=(tag, "xt", s))
                for half in range(2):
                    for j in range(NCH):
                        P.pe(lambda e, j=j, half=half, tt=tt, pbk=pbk: e.matmul(
                            py[pbk][:, half * 512:(half + 1) * 512], lhsT=actT[:, j, tt * 128:(tt + 1) * 128],
                            rhs=w2[:, j, half * 512:(half + 1) * 512], start=(j == 0), stop=(j == NCH - 1)),
                            reads=[(tag, "actT"), (tag, "w2")], writes=[("PS", tag, "py", pbk)])
                P.dve(lambda e, s=s, pbk=pbk: e.tensor_tensor(xt[s][:], py[pbk][:, :], xt[s][:], ALU.add),
                      reads=[("PS", tag, "py", pbk), (tag, "xt", s)], writes=[(tag, "xt", s)])
                P.dma("sp", lambda e, t=t, s=s: e.dma_start(out=g.xs[t * 128:(t + 1) * 128, :], in_=xt[s][:]),
                      reads=[(tag, "xt", s)], writes=[("xs", t)], semkey=(tag, "xst", s))
        P.fence()
        P.flush(nc)


def phase_PLE(P, nc, g, C, layer, gain_idx, final):
    tag = "E%d" % layer
    with ExitStack() as es:
        sb = lambda n, s, d: es.enter_context(_sbt(nc, f"{tag}_{n}", s, d))
        ps = lambda n, s, d: es.enter_context(_pst(nc, f"{tag}_{n}", s, d))
        wg = sb("wg", [128, 8, D], BF16)
        wp = sb("wp", [128, 2, D], BF16)
        pTg = [sb(f"pTg{i}", [128, 2, 1024], BF16) for i in range(2)]
        NXT = 5
        xt = [sb(f"xt{i}", [128, D], F32) for i in range(NXT)]
        gTt = [sb(f"gTt{i}", [128, 8, 128], BF16) for i in range(3)]
        sg = [sb(f"sg{i}", [128, 512], F32) for i in range(2)]
        pg = [ps(f"pg{i}", [128, 512], F32) for i in range(2)]
        pp = [ps(f"pp{i}", [128, 512], F32) for i in range(2)]
        NU = NormUnit(nc, es, P, tag + "n", C.idb, C.gT, nslots=3)
        NU2 = NormUnit(nc, es, P, tag + "m", C.idb, C.gT, nslots=3)
        if final:
            fg = sb("fg", [128, D], F32)
            ot = [sb(f"ot{i}", [128, D], F32) for i in range(2)]
            P.dma("sp", lambda e: e.dma_start(out=fg[:], in_=g.fgain[:, :]), writes=[(tag, "fg")], semkey=(tag, "fg"))
        else:
            hk = [sb(f"hk{i}", [128, 8, 1024], BF16) for i in range(2)]
            ha = [sb(f"ha{i}", [128, 8, 1024], BF16) for i in range(2)]
        wgv = g.wg[layer].rearrange("(c p) n -> p c n", p=128)
        for h in range(2):
            P.dma("pool", lambda e, h=h: e.dma_start(out=wg[:, :, h * 512:(h + 1) * 512], in_=wgv[:, :, h * 512:(h + 1) * 512]),
                  writes=[(tag, "wg")], semkey=(tag, "wg", h))
        P.dma("pool", lambda e: e.dma_start(out=wp[:], in_=g.wp[layer].rearrange("(c p) n -> p c n", p=128)),
              writes=[(tag, "wp")], semkey=(tag, "wp"))
        sl1, sl2 = {}, {}
        hcnt = {"n": 0}

        def s1(t):
            gq, tt = t // 8, t % 8
            gs = gq % 2
            if tt == 0:
                P.dma("pool", lambda e: e.dma_start(
                    out=pTg[gs][:], in_=g.pT[layer, :, gq * 1024:(gq + 1) * 1024].rearrange("(c p) t -> p c t", p=128)),
                    writes=[(tag, "pTg", gs)], semkey=(tag, "pTg", gs))
            s = t % NXT
            P.dma("sp", lambda e: e.dma_start(out=xt[s][:], in_=g.xs[t * 128:(t + 1) * 128, :]),
                  reads=[("xs", t)], writes=[(tag, "xt", s)], semkey=(tag, "xt", s))
            sl1[t] = NU.run_a(xt[s][:], (tag, "xt", s))

        def s2(t):
            s3_ = t % 3
            NU.run_b(sl1[t], [(gain_idx, gTt[s3_][:], (tag, "gTt", s3_))])

        def s3(t):
            gq, tt = t // 8, t % 8
            gs = gq % 2
            s = t % NXT
            s3_ = t % 3
            for half in range(2):
                hs_ = slice(half * 512, (half + 1) * 512)
                k = hcnt["n"] % 2
                hcnt["n"] += 1
                for c in range(8):
                    P.pe(lambda e, c=c, hs_=hs_, k=k: e.matmul(pg[k][:, :], lhsT=gTt[s3_][:, c, :], rhs=wg[:, c, hs_],
                                                              start=(c == 0), stop=(c == 7)),
                         reads=[(tag, "gTt", s3_), (tag, "wg")], writes=[("PS", tag, "pg", k)])
                for c in range(2):
                    P.pe(lambda e, c=c, hs_=hs_, k=k: e.matmul(pp[k][:, :], lhsT=pTg[gs][:, c, tt * 128:(tt + 1) * 128],
                                                              rhs=wp[:, c, hs_], start=(c == 0), stop=(c == 1)),
                         reads=[(tag, "pTg", gs), (tag, "wp")], writes=[("PS", tag, "pp", k)])
                P.act(lambda e, k=k: e.activation(out=sg[k][:], in_=pg[k][:, :], func=AF.Sigmoid),
                      reads=[("PS", tag, "pg", k)], writes=[(tag, "sg", k)])
                P.dve(lambda e, k=k: e.tensor_tensor(sg[k][:], sg[k][:], pp[k][:, :], ALU.mult),
                      reads=[(tag, "sg", k), ("PS", tag, "pp", k)], writes=[(tag, "sg", k)])
                P.dve(lambda e, k=k, hs_=hs_: e.tensor_tensor(xt[s][:, hs_], xt[s][:, hs_], sg[k][:], ALU.add),
                      reads=[(tag, "xt", s), (tag, "sg", k)], writes=[(tag, "xt", s)])
            xkey = (tag, "xt", s)
            if not final:
                P.dma("pool", lambda e: e.dma_start(out=g.xs[t * 128:(t + 1) * 128, :], in_=xt[s][:]),
                      reads=[xkey], writes=[("xs", t)], semkey=(tag, "xst", s))

        def s3b(t):
            s = t % NXT
            xkey = (tag, "xt", s)
            if final:
                s2_ = t % 2
                ns = NU2.stats(xt[s][:], xkey)
                P.dve(lambda e: e.scalar_tensor_tensor(out=ot[s2_][:], in0=xt[s][:], scalar=NU2.rr[:, ns:ns + 1], in1=fg[:],
                                                       op0=ALU.mult, op1=ALU.mult),
                      reads=[xkey, (tag + "m", "rr", ns), (tag, "fg")], writes=[(tag, "ot", s2_)])
                P.dma("pool", lambda e: e.dma_start(out=g.out[t * 128:(t + 1) * 128, :], in_=ot[s2_][:]),
                      reads=[(tag, "ot", s2_)], writes=[("out", t)], semkey=(tag, "ost", s2_))
            else:
                sl2[t] = NU2.run_a(xt[s][:], xkey)

        def s4(t):
            if final:
                return
            gq, tt = t // 8, t % 8
            gs = gq % 2
            NU2.run_b(sl2[t], [(3, hk[gs][:, :, tt * 128:(tt + 1) * 128], (tag, "hk", gs)),
                               (4, ha[gs][:, :, tt * 128:(tt + 1) * 128], (tag, "ha", gs))])
            if tt == 7:
                cs = slice(gq * 1024, (gq + 1) * 1024)
                P.dma("pool", lambda e: e.dma_start(out=g.hkv[:, :, cs], in_=hk[gs][:]), reads=[(tag, "hk", gs)],
                      writes=[("hkv", gq)], semkey=(tag, "hkst", gs))
                P.dma("pool", lambda e: e.dma_start(out=g.hat[:, :, cs], in_=ha[gs][:]), reads=[(tag, "ha", gs)],
                      writes=[("hat", gq)], semkey=(tag, "hast", gs))

        for i in range(NT + 4):
            if i < NT:
                s1(i)
            if 0 <= i - 1 < NT:
                s2(i - 1)
            if 0 <= i - 2 < NT:
                s3(i - 2)
            if 0 <= i - 3 < NT:
                s3b(i - 3)
            if 0 <= i - 4 < NT:
                s4(i - 4)
        P.fence()
        P.flush(nc)


def phase_D(P, nc, g, C):
    tag = "D"
    with ExitStack() as es:
        sb = lambda n, s, d: es.enter_context(_sbt(nc, f"{tag}_{n}", s, d))
        B = attn_buffers(nc, es, "D")
        B.idb = C.idb
        load_rope(P, g, B)
        wk2 = sb("wk2", [128, 8, 2, 128], BF16)
        wv = sb("wv", [128, 8, 128], BF16)
        wq = sb("wq", [128, 8, D], BF16)
        KT2 = [sb(f"KT{i}", [128, T], BF16) for i in range(2)]
        QTd = [sb(f"QTd{i}", [128, 2, T], BF16) for i in range(2)]
        VTs = sb("VTs", [128, T], BF16)
        for i in range(2):
            P.pool(lambda e, i=i: e.memset(QTd[i][:], 0.0), writes=[("QT", i)])
        obf = [sb(f"obf{i}", [128, T], BF16) for i in range(2)]
        hs = [sb(f"hs{i}", [128, 8, 512], BF16) for i in range(3)]
        esk = sb("esk", [128, 16], F32)
        tmp = sb("tmp", [128, 512], F32)
        rec = sb("rec", [128, 512], F32)
        kvv = g.kvw.rearrange("(c p) n -> p c n", p=128)
        for kvh in range(2):
            for dup in range(2):
                P.dma("pool", lambda e, kvh=kvh, dup=dup: e.dma_start(
                    out=wk2[:, :, kvh, dup * 64:(dup + 1) * 64], in_=kvv[:, :, kvh * 64:(kvh + 1) * 64]),
                    writes=["wk2"], semkey=("wk2", kvh, dup))
        P.dma("pool", lambda e: e.dma_start(out=wv[:], in_=kvv[:, :, 128:256]), writes=["wv"], semkey="wv")
        wqv = g.wq1.rearrange("(c p) n -> p c n", p=128)
        for h in range(2):
            P.dma("pool", lambda e, h=h: e.dma_start(out=wq[:, :, h * 512:(h + 1) * 512], in_=wqv[:, :, h * 512:(h + 1) * 512]),
                  writes=["wq1"], semkey=("wq1", h))
        P.dma("sp", lambda e: e.dma_start(out=esk[:], in_=g.sinks[:, :]), writes=["esk"], semkey="esk")
        P.act(lambda e: e.activation(out=esk[:], in_=esk[:], func=AF.Exp), reads=["esk"], writes=["esk"])
        P.pool(lambda e: e.memset(B.V[:, :, :, 64:128], 1.0), writes=["V"])
        hst = {"cnt": 0, "slot": {}}

        def mk_pre(src, srckey):
            def pre(s_):
                sl = hst["cnt"] % 3
                hst["cnt"] += 1
                hst["slot"][s_] = sl
                P.dma("sp", lambda e: e.dma_start(out=hs[sl][:], in_=src[:, :, s_ * 512:(s_ + 1) * 512]),
                      reads=[(srckey, s_ // 2)], writes=[("hs", sl)], semkey=("hs", sl))
                return sl
            return pre

        for kvh in range(2):
            rope_proj(P, nc, B, "D", 8,
                      lambda e, s_, c, o, hx, kvh=kvh: e.matmul(o, lhsT=wk2[:, c, kvh, :], rhs=hs[hx][:, c, :],
                                                                start=(c == 0), stop=(c == 7)),
                      lambda s_, hx: ["wk2", ("hs", hx)], B.ropeC, B.ropeS,
                      lambda s_, kvh=kvh: [(KT2[kvh][:, s_ * 512:(s_ + 1) * 512], slice(0, 128))], ("KT", kvh), C.psw, C.idb,
                      pre=mk_pre(g.hkv, "hkv"))
        prev = mk_pre(g.hkv, "hkv")
        for sv in range(8):
            hx = prev(sv)
            sl = sv % 2
            for c in range(8):
                P.pe(lambda e, c=c, sl=sl, hx=hx: e.matmul(B.pq[sl][:, :], lhsT=wv[:, c, :], rhs=hs[hx][:, c, :],
                                                          start=(c == 0), stop=(c == 7)),
                     reads=[("hs", hx), "wv"], writes=[("PS", "bk", sl)])
            P.act(lambda e, sv=sv, sl=sl: e.copy(VTs[:, sv * 512:(sv + 1) * 512], B.pq[sl][:, :]),
                  reads=[("PS", "bk", sl)], writes=["VTs"])
        v_from_vt(P, B, 1, VTs, "VTs")
        for pair in range(8):
            qs = pair % 2
            rope_proj(P, nc, B, "D", 8,
                      lambda e, s_, c, o, hx, pair=pair: e.matmul(o, lhsT=wq[:, c, pair * 128:(pair + 1) * 128],
                                                                  rhs=hs[hx][:, c, :], start=(c == 0), stop=(c == 7)),
                      lambda s_, hx: ["wq1", ("hs", hx)], B.ropeC, B.ropeS,
                      lambda s_, qs=qs: [(QTd[qs][0:64, 0, s_ * 512:(s_ + 1) * 512], slice(0, 64)),
                                         (QTd[qs][64:128, 1, s_ * 512:(s_ + 1) * 512], slice(64, 128))], ("QT", qs), C.psw, C.idb,
                      pre=mk_pre(g.hat, "hat"))
            kvh = pair // 4

            def evac(hh, k, pvb, pvkey, pair=pair, qs=qs):
                head = pair * 2 + hh
                cs = slice(k * 512, (k + 1) * 512)
                P.act(lambda e: e.activation(out=tmp[64:128, :], in_=pvb[64:128, :], func=AF.Ln, bias=esk[64:128, head:head + 1]),
                      reads=[pvkey, "esk"], writes=["tmp"])
                P.act(lambda e: e.activation(out=rec[0:64, :], in_=tmp[64:128, :], func=AF.Exp, scale=-1.0),
                      reads=["tmp"], writes=["rec"])
                P.dve(lambda e: e.tensor_tensor(obf[qs][hh * 64:(hh + 1) * 64, cs], pvb[0:64, :], rec[0:64, :], ALU.mult),
                      reads=[pvkey, "rec"], writes=[("obf", qs)])

            attention(P, nc, B, "D", 1,
                      lambda r, n0, cnt, kvh=kvh: KT2[kvh][:, n0:n0 + cnt],
                      lambda r, n0, cnt, qs=qs: QTd[qs][:, :, n0:n0 + cnt],
                      lambda hh, b, kvh=kvh: B.V[:, b, kvh, :],
                      [("KT", kvh), "V"], [("QT", qs)], C.masks[:, 1, :], evac, C.idb)
            P.dma("sp", lambda e, pair=pair, qs=qs: e.dma_start(out=g.oT[pair, :, :], in_=obf[qs][:]), reads=[("obf", qs)],
                  writes=[("oT", pair)], semkey=("obf_st1", qs))
        P.fence()
        P.flush(nc)


def phase_F1(P, nc, g, C):
    tag = "F1"
    NH = 14
    with ExitStack() as es:
        sb = lambda n, s, d: es.enter_context(_sbt(nc, f"{tag}_{n}", s, d))
        ps = lambda n, s, d: es.enter_context(_pst(nc, f"{tag}_{n}", s, d))
        xg = sb("xg", [128, 8, D], F32)
        hTg = [sb(f"hTg{i}", [128, 8, 1024], BF16) for i in range(2)]
        actT = sb("actT", [128, NH, 1024], BF16)
        w2h = [sb(f"w2h{i}", [128, NH, D], BF16) for i in range(2)]
        wb1 = [sb(f"wb1{i}", [128, 8, 512], BF16) for i in range(2)]
        wb3 = [sb(f"wb3{i}", [128, 8, 512], BF16) for i in range(2)]
        sa = [sb(f"sa{i}", [128, 512], BF16) for i in range(2)]
        pa = [ps(f"pa{i}", [128, 512], F32) for i in range(2)]
        pb = [ps(f"pb{i}", [128, 512], F32) for i in range(2)]
        py = [ps(f"py{i}", [128, D], F32) for i in range(2)]
        wit = {"w": 0, "p": 0}
        it = 0
        pyc = 0
        for gq in range(4):
            gs = gq % 2
            P.dma("sp", lambda e, gq=gq, gs=gs: e.dma_start(out=hTg[gs][:], in_=g.hsc[:, :, gq * 1024:(gq + 1) * 1024]),
                  reads=[("hsc", gq)], writes=[(tag, "hTg", gs)], semkey=(tag, "hTg", gs))
            for q in range(2):
                P.dma("sp", lambda e, gq=gq, q=q: e.dma_start(
                    out=xg[:, q * 4:(q + 1) * 4, :],
                    in_=g.xs[gq * 1024 + q * 512:gq * 1024 + (q + 1) * 512, :].rearrange("(t p) n -> p t n", p=128)),
                    reads=[("xs", gq * 8 + q * 4 + i) for i in range(4)], writes=[(tag, "xg")], semkey=(tag, "xg", q))
            for ex in range(NEXP):
                for hf in range(2):
                    w2s = it % 2
                    it += 1
                    f0 = hf * NH * 128
                    w2v = g.w2m[ex].rearrange("(j p) n -> p j n", p=128)
                    for q in range(0, NH, 7):
                        P.dma("pool", lambda e, q=q, w2s=w2s, w2v=w2v, hf=hf: e.dma_start(
                            out=w2h[w2s][:, q:q + 7, :], in_=w2v[:, hf * NH + q:hf * NH + q + 7, :]),
                            writes=[(tag, "w2h", w2s)], semkey=(tag, "w2h", w2s, q))
                    w1v = g.w1m[ex].rearrange("(c p) n -> p c n", p=128)
                    w3v = g.w3m[ex].rearrange("(c p) n -> p c n", p=128)
                    ffn_stage(P, nc, tag, hTg[gs], (tag, "hTg", gs), NH,
                              lambda c0, n, w1v=w1v, f0=f0: w1v[:, :, f0 + c0:f0 + c0 + n],
                              lambda c0, n, w3v=w3v, f0=f0: w3v[:, :, f0 + c0:f0 + c0 + n],
                              wb1, wb3, pa, pb, sa, actT, wit)
                    for tt in range(8):
                        t = gq * 8 + tt
                        pbk = pyc % 2
                        pyc += 1
                        for half in range(2):
                            for j in range(NH):
                                P.pe(lambda e, j=j, half=half, tt=tt, pbk=pbk, w2s=w2s: e.matmul(
                                    py[pbk][:, half * 512:(half + 1) * 512], lhsT=actT[:, j, tt * 128:(tt + 1) * 128],
                                    rhs=w2h[w2s][:, j, half * 512:(half + 1) * 512], start=(j == 0), stop=(j == NH - 1)),
                                    reads=[(tag, "actT"), (tag, "w2h", w2s)], writes=[("PS", tag, "py", pbk)])
                        P.dve(lambda e, tt=tt, t=t, pbk=pbk, ex=ex: e.scalar_tensor_tensor(
                            out=xg[:, tt, :], in0=py[pbk][:, :], scalar=C.gates[:, t, ex:ex + 1], in1=xg[:, tt, :],
                            op0=ALU.mult, op1=ALU.add), reads=[("PS", tag, "py", pbk), "gates", (tag, "xg")], writes=[(tag, "xg")])
            for q in range(2):
                P.dma("sp", lambda e, gq=gq, q=q: e.dma_start(
                    out=g.xs[gq * 1024 + q * 512:gq * 1024 + (q + 1) * 512, :].rearrange("(t p) n -> p t n", p=128),
                    in_=xg[:, q * 4:(q + 1) * 4, :]),
                    reads=[(tag, "xg")], writes=[("xs", gq * 8 + q * 4 + i) for i in range(4)], semkey=(tag, "xgst", q))
        P.fence()
        P.flush(nc)


STOP = {"n": 99}
import os, json
if os.environ.get("KSTOP"):
    STOP.update(json.loads(os.environ["KSTOP"]))


def program(P, nc, g):
    with ExitStack() as es:
        C = load_consts(nc, es, P, g)
        stages = [
            lambda: phase_A(P, nc, g, C),
            lambda: phase_B(P, nc, g, C, g.wo0, g.x, 1, g.hsc),
            lambda: phase_C1(P, nc, g, C),
            lambda: phase_PLE(P, nc, g, C, 0, 2, False),
            lambda: phase_D(P, nc, g, C),
            lambda: phase_B(P, nc, g, C, g.wo1, g.xs, 5, g.hsc, router=True),
            lambda: phase_F1(P, nc, g, C),
            lambda: phase_PLE(P, nc, g, C, 1, 6, True),
        ]
        nst = min(STOP["n"], len(stages))
        for st in stages[:nst]:
            st()
        if nst < len(stages):
            for q in range(4):
                P.dma("sp", lambda e, q=q: e.dma_start(out=g.out[q * 1024:(q + 1) * 1024, :], in_=g.xs[q * 1024:(q + 1) * 1024, :]),
                      reads=[("xs", q * 8 + i) for i in range(8)], writes=[("out", q * 8 + i) for i in range(8)],
                      semkey=("dbg", q))
        P.add("sp", lambda e: e.nop(), reads=[("out", t) for t in range(NT)])
        P.flush(nc)


def build_nc():
    nc = bass.Bass("TRN2", target_bir_lowering=False)
    g = declare_dram(nc)
    P = Prog()
    program(P, nc, g)
    P.analyze()
    P.mode = "emit"
    P.count = 0
    with ExitStack() as es:
        P.alloc_sems(nc, es)
        program(P, nc, g)
    return nc


def _consts():
    half = 32
    inv = (1.0 / (np.float32(10000.0) ** (np.arange(half, dtype=np.float32) / np.float32(half)))).astype(np.float32)
    pos = np.arange(T, dtype=np.float32)
    ang = (pos[:, None] * inv[None, :]).astype(np.float32)
    cos = np.cos(ang).astype(np.float32).T
    sin = np.sin(ang).astype(np.float32).T
    Ct = np.ascontiguousarray(np.tile(cos, (4, 1)))
    sign = np.where((np.arange(128) % 64) < 32, -1.0, 1.0).astype(np.float32)[:, None]
    St = np.ascontiguousarray(np.tile(sin, (4, 1)) * (-sign))
    k = np.arange(128)[:, None]
    q = np.arange(128)[None, :]
    m_a = (q >= k)
    masks = np.stack([np.concatenate([m_a, q <= k], 1), np.concatenate([m_a, q < k], 1)]).astype(np.float32)
    ident = np.eye(128, dtype=np.float32)
    m = np.arange(128)
    sw = np.where((m % 64) < 32, m + 32, m - 32)
    pswap = np.zeros((128, 128), np.float32)
    pswap[sw, m] = 1.0
    return Ct, St, masks, ident, pswap


def make_in_maps(inp):
    f = lambda a: np.ascontiguousarray(np.asarray(a, dtype=np.float32))
    Ct, St, masks, ident, pswap = _consts()
    gl = [inp["attn_norm"][0], inp["ffn_norm"][0], inp["ple_norm"][0], inp["kv_norm"], inp["attn_norm"][1],
          inp["ffn_norm"][1], inp["ple_norm"][1]]
    gT = np.concatenate([f(v).reshape(8, 128).T for v in gl], axis=1)
    shared = dict(
        wqkv=f(inp["a_w_qkv"][0]), wo0=f(inp["a_w_o"][0]), kvw=f(inp["kv_w"]), wq1=f(inp["b_w_q"][0]),
        wo1=f(inp["b_w_o"][0]), w1d=f(inp["dense_w1"][0]), w3d=f(inp["dense_w3"][0]), w2d=f(inp["dense_w2"][0]),
        wr=f(inp["moe_router"][0]), w1m=f(inp["moe_w1"][0]), w3m=f(inp["moe_w3"][0]), w2m=f(inp["moe_w2"][0]),
        wg=f(inp["ple_w_gate"]), wp=f(inp["ple_w_proj"]), gT=f(gT),
        fgain=f(np.broadcast_to(f(inp["final_norm"])[None, :], (128, D))),
        sinks=f(np.broadcast_to(f(inp["b_sinks"][0])[None, :], (128, 16))),
        ropeC=Ct, ropeS=St, masks=masks, ident=ident, pswap=pswap)
    x = np.asarray(inp["x"], dtype=np.float32)
    p = np.asarray(inp["p"], dtype=np.float32)
    maps = []
    for b in range(x.shape[0]):
        m = dict(shared)
        m["x"] = f(x[b])
        m["pT"] = f(np.transpose(p[:, b], (0, 2, 1)))
        maps.append(m)
    return maps


_NC_CACHE = {}


def kernel(**inputs):
    maps = make_in_maps(inputs)
    if "nc" not in _NC_CACHE:
        _NC_CACHE["nc"] = build_nc()
    nc = _NC_CACHE["nc"]
    res = run_bass_kernel_spmd(nc, maps, core_ids=list(range(len(maps))))
    return np.stack([np.asarray(r["out"], dtype=np.float32) for r in res.results], axis=0)
```

```python
import numpy as np
from contextlib import ExitStack
import concourse.bass as bass
import concourse.mybir as mybir
from concourse.bass_utils import run_bass_kernel_spmd

F32 = mybir.dt.float32
BF16 = mybir.dt.bfloat16
AF = mybir.ActivationFunctionType
ALU = mybir.AluOpType
AX = mybir.AxisListType

T = 4096
D = 1024
NT = T // 128
DFF = 2816
DEXP = 3584
NEXP = 8
EPS = 1e-6
COMPUTE = ("pe", "act", "dve", "pool")
EPOCH = 20000
A_CONFIGS = ((128, 1), (512, 4), (2048, 16))


class _Op:
    __slots__ = ("eng", "reads", "writes", "dma", "semkey", "waits", "sig", "idx", "fence")

    def __init__(self, eng, reads, writes, dma, semkey):
        self.eng = eng
        self.reads = reads
        self.writes = writes
        self.dma = dma
        self.semkey = semkey
        self.waits = None
        self.sig = None
        self.fence = False


class Prog:
    def __init__(self):
        self.meta = []
        self.mode = "record"
        self.count = 0
        self.cur = []
        self.sems = None
        self.phase_map = {}

    def add(self, eng, fn, reads=(), writes=(), dma=False, semkey=None):
        if self.mode == "record":
            if dma:
                pm = self.phase_map.setdefault(eng, {})
                semkey = ("dq", eng, pm.setdefault(semkey, len(pm)))
            op = _Op(eng, tuple(reads), tuple(writes), dma, semkey)
            op.idx = len(self.meta)
            self.meta.append(op)
        else:
            op = self.meta[self.count]
            assert op.eng == eng and op.dma == dma, (op.idx, op.eng, eng)
            self.cur.append((op, fn))
        self.count += 1

    def pe(self, fn, reads=(), writes=()):
        self.add("pe", fn, reads, writes)

    def act(self, fn, reads=(), writes=()):
        self.add("act", fn, reads, writes)

    def dve(self, fn, reads=(), writes=()):
        self.add("dve", fn, reads, writes)

    def pool(self, fn, reads=(), writes=()):
        self.add("pool", fn, reads, writes)

    def dma(self, q, fn, reads=(), writes=(), semkey=None):
        assert semkey is not None
        self.add(q, fn, reads, writes, dma=True, semkey=semkey)

    def fence(self):
        if self.mode == "record":
            op = _Op(None, (), (), False, None)
            op.fence = True
            op.idx = len(self.meta)
            self.meta.append(op)
            self.phase_map = {}
        self.count += 1

    def analyze(self):
        ops = self.meta
        last_w, readers = {}, {}
        last_dma_on_sem, dma_count = {}, {}
        last_on_eng = {}
        pending_fence = {}
        needs_sig = [False] * len(ops)
        deps_list = [None] * len(ops)
        for op in ops:
            i = op.idx
            if op.fence:
                fd = set(last_on_eng.values()) | set(last_dma_on_sem.values())
                for e in COMPUTE + ("sp",):
                    pending_fence[e] = set(fd)
                deps_list[i] = []
                continue
            raw, other = set(), set()
            xr = tuple(r for r in op.reads if isinstance(r, tuple) and r[0] == "PS" and r not in op.writes)
            for r in op.reads:
                w = last_w.get(r)
                if w is not None:
                    raw.add(w)
            for w_ in op.writes + xr:
                w = last_w.get(w_)
                if w is not None:
                    other.add(w)
                for rd in readers.get(w_, ()):
                    other.add(rd)
            if op.eng in pending_fence:
                raw |= pending_fence.pop(op.eng)
            if op.dma:
                p = last_dma_on_sem.get(op.semkey)
                if p is not None:
                    raw.add(p)
                last_dma_on_sem[op.semkey] = i
                dma_count[op.semkey] = dma_count.get(op.semkey, 0) + 1
                op.sig = ("d", op.semkey, dma_count[op.semkey] * 16)
            else:
                last_on_eng[op.eng] = i
            deps = set()
            for d in raw | other:
                if d == i:
                    continue
                dop = ops[d]
                if dop.dma:
                    deps.add(d)
                    continue
                if (not op.dma) and dop.eng == op.eng:
                    if op.eng != "pe":
                        deps.add(d)
                    continue
                deps.add(d)
            best, final = {}, []
            for d in deps:
                dop = ops[d]
                if dop.dma:
                    final.append(d)
                elif dop.eng not in best or best[dop.eng] < d:
                    best[dop.eng] = d
            final.extend(best.values())
            for d in final:
                needs_sig[d] = True
            deps_list[i] = final
            for r in op.reads:
                readers.setdefault(r, []).append(i)
            for w_ in op.writes + xr:
                last_w[w_] = i
                readers[w_] = []
        seq = {e: 0 for e in COMPUTE + ("sp",)}
        for op in ops:
            if op.fence or op.dma:
                continue
            if needs_sig[op.idx]:
                seq[op.eng] += 1
                s = seq[op.eng]
                op.sig = ("e", op.eng, (s - 1) // EPOCH, s - ((s - 1) // EPOCH) * EPOCH)
        known = {}
        for op in ops:
            if op.fence:
                continue
            ws = []
            kn = known.setdefault(op.eng, {})
            for d in deps_list[op.idx]:
                sg = ops[d].sig
                if sg[0] == "d":
                    key, val = ("d", sg[1]), sg[2]
                else:
                    key, val = ("e", sg[1], sg[2]), sg[3]
                if kn.get(key, 0) >= val:
                    continue
                kn[key] = val
                ws.append((key, val))
            op.waits = ws
        self.n_epochs = {e: (seq[e] + EPOCH - 1) // EPOCH for e in seq}
        self.semkeys = list(dma_count.keys())

    def alloc_sems(self, nc, es):
        sems = {}
        for e, n in self.n_epochs.items():
            for k in range(n):
                sems[("e", e, k)] = es.enter_context(nc.semaphore(f"s_{e}_{k}"))
        for j, sk in enumerate(self.semkeys):
            sems[("d", sk)] = es.enter_context(nc.semaphore(f"d_{j}"))
        self.sems = sems

    def flush(self, nc):
        if self.mode != "emit" or not self.cur:
            self.cur = []
            return
        per = {}
        for op, fn in self.cur:
            per.setdefault(op.eng, []).append((op, fn))
        self.cur = []
        sems = self.sems

        def run(engobj, lst):
            for op, fn in lst:
                for key, val in op.waits:
                    engobj.wait_ge(sems[key], val)
                ins = fn(engobj)
                sg = op.sig
                if sg is not None:
                    if sg[0] == "d":
                        ins.then_inc(sems[("d", sg[1])], 16)
                    else:
                        ins.then_inc(sems[("e", sg[1], sg[2])], 1)

        with nc.Block() as block:
            if "pe" in per:
                @block.tensor
                def _(t):
                    run(t, per["pe"])
            if "act" in per:
                @block.scalar
                def _(a):
                    run(a, per["act"])
            if "dve" in per:
                @block.vector
                def _(v):
                    run(v, per["dve"])
            if "pool" in per:
                @block.gpsimd
                def _(g):
                    run(g, per["pool"])
            if "sp" in per:
                @block.sync
                def _(s):
                    run(s, per["sp"])


_UID = [0]


def _sbt(nc, name, shape, dt):
    _UID[0] += 1
    return nc.sbuf_tensor(f"{name}_u{_UID[0]}", shape, dt)


def _pst(nc, name, shape, dt):
    _UID[0] += 1
    return nc.psum_tensor(f"{name}_u{_UID[0]}", shape, dt)


def tokview(ap2d, dil):
    if dil == 1:
        return ap2d.unsqueeze(1)
    return ap2d.rearrange("p (m d) -> p d m", d=dil)


class Ctx:
    pass


def declare_dram(nc):
    g = Ctx()
    ei = lambda n, s: nc.dram_tensor(n, s, F32, kind="ExternalInput").ap()
    g.x = ei("x", [T, D])
    g.pT = ei("pT", [2, 256, T])
    g.wqkv = ei("wqkv", [D, 9216])
    g.wo0 = ei("wo0", [D, D])
    g.kvw = ei("kvw", [D, 256])
    g.wq1 = ei("wq1", [D, D])
    g.wo1 = ei("wo1", [D, D])
    g.w1d = ei("w1d", [D, DFF])
    g.w3d = ei("w3d", [D, DFF])
    g.w2d = ei("w2d", [DFF, D])
    g.wr = ei("wr", [D, NEXP])
    g.w1m = ei("w1m", [NEXP, D, DEXP])
    g.w3m = ei("w3m", [NEXP, D, DEXP])
    g.w2m = ei("w2m", [NEXP, DEXP, D])
    g.wg = ei("wg", [2, D, D])
    g.wp = ei("wp", [2, 256, D])
    g.gT = ei("gT", [128, 7 * 8])
    g.fgain = ei("fgain", [128, D])
    g.sinks = ei("sinks", [128, 16])
    g.ropeC = ei("ropeC", [128, T])
    g.ropeS = ei("ropeS", [128, T])
    g.masks = ei("masks", [2, 128, 256])
    g.ident = ei("ident", [128, 128])
    g.pswap = ei("pswap", [128, 128])
    g.out = nc.dram_tensor("out", [T, D], F32, kind="ExternalOutput").ap()
    g.xs = nc.dram_tensor("xs", [T, D], F32).ap()
    g.oT = nc.dram_tensor("oTs", [8, 128, T], BF16).ap()
    g.hsc = nc.dram_tensor("hsc", [128, 8, T], BF16).ap()
    g.hkv = nc.dram_tensor("hkv", [128, 8, T], BF16).ap()
    g.hat = nc.dram_tensor("hat", [128, 8, T], BF16).ap()
    return g


class NormUnit:
    def __init__(self, nc, es, P, tag, idb, gT, nslots=2):
        sb = lambda n, s, d: es.enter_context(_sbt(nc, f"{tag}_{n}", s, d))
        self.P = P
        self.tag = tag
        self.idb = idb
        self.gT = gT
        self.ns = nslots
        self.junk = sb("junk", [128, D], BF16)
        self.ss = sb("ss", [128, nslots], F32)
        self.rr = sb("rr", [128, nslots], F32)
        self.eps = sb("eps", [128, 1], F32)
        self.xn = [sb(f"xn{i}", [128, D], BF16) for i in range(nslots)]
        self.pt = [es.enter_context(_pst(nc, f"{tag}_pt{i}", [128, 8, 128], BF16)) for i in range(2)]
        self.n = 0
        self.pn = 0
        P.pool(lambda e: e.memset(self.eps[:], EPS), writes=[(tag, "eps")])

    def stats(self, xt, xkey):
        P, tag = self.P, self.tag
        s = self.n % self.ns
        self.n += 1
        P.act(lambda e: e.activation(out=self.junk[:], in_=xt, func=AF.Square, accum_out=self.ss[:, s:s + 1]),
              reads=[xkey], writes=[(tag, "junk"), (tag, "ss", s)])
        P.act(lambda e: e.activation(out=self.rr[:, s:s + 1], in_=self.ss[:, s:s + 1], func=AF.Sqrt,
                                     scale=1.0 / D, bias=self.eps[:]),
              reads=[(tag, "ss", s), (tag, "eps")], writes=[(tag, "rr", s)])
        P.dve(lambda e: e.reciprocal(self.rr[:, s:s + 1], self.rr[:, s:s + 1]),
              reads=[(tag, "rr", s)], writes=[(tag, "rr", s)])
        return s

    def run_a(self, xt, xkey):
        P, tag = self.P, self.tag
        s = self.stats(xt, xkey)
        xn = self.xn[s]
        P.dve(lambda e: e.tensor_scalar(xn[:], xt, self.rr[:, s:s + 1], None, ALU.mult),
              reads=[xkey, (tag, "rr", s)], writes=[(tag, "xn", s)])
        return s

    def run_b(self, s, outs):
        P, tag = self.P, self.tag
        xn = self.xn[s]
        ps = self.pn % len(self.pt)
        self.pn += 1
        pt = self.pt[ps]
        for c in range(8):
            P.pe(lambda e, c=c: e.transpose(pt[:, c, :], xn[:, c * 128:(c + 1) * 128], self.idb[:]),
                 reads=[(tag, "xn", s), "idb"], writes=[("PS", tag, "pt", ps)])
        for gi, oap, okey in outs:
            P.dve(lambda e, gi=gi, oap=oap: e.tensor_tensor(
                oap, pt[:], self.gT[:, gi * 8:(gi + 1) * 8].unsqueeze(2).to_broadcast([128, 8, 128]), ALU.mult),
                reads=[("PS", tag, "pt", ps), "gT"], writes=[okey])
        return ps

    def run(self, xt, xkey, outs):
        s = self.run_a(xt, xkey)
        self.run_b(s, outs)
        return s


def load_consts(nc, es, P, g):
    sb = lambda n, s, d: es.enter_context(_sbt(nc, n, s, d))
    c = Ctx()
    c.idb = sb("idb", [128, 128], BF16)
    c.psw = sb("pswb", [128, 128], BF16)
    c.gT = sb("gTs", [128, 56], F32)
    c.masks = sb("masksb", [128, 2, 256], BF16)
    c.gates = sb("gates", [128, NT, NEXP], F32)
    P.dma("pool", lambda e: e.dma_start(out=c.idb[:], in_=g.ident[:, :]), writes=["idb"], semkey="c_idb")
    P.dma("pool", lambda e: e.dma_start(out=c.psw[:], in_=g.pswap[:, :]), writes=["psw"], semkey="c_psw")
    P.dma("sp", lambda e: e.dma_start(out=c.gT[:], in_=g.gT[:, :]), writes=["gT"], semkey="c_gT")
    for l in range(2):
        P.dma("pool", lambda e, l=l: e.dma_start(out=c.masks[:, l, :], in_=g.masks[l, :, :]),
              writes=["masks"], semkey=("c_mask", l))
    return c


def rope_proj(P, nc, B, tag, nslab, mm_fn, mm_reads, Ctab, Stab, out_fn, out_key, psw, idb, col0=0, pre=None):
    L = 1

    def stage1(s):
        sl = s % 2
        cs = slice(col0 + s * 512, col0 + (s + 1) * 512)
        hx = pre(s) if pre is not None else None
        for c in range(8):
            if pre is not None:
                P.pe(lambda e, s=s, c=c, sl=sl, hx=hx: mm_fn(e, s, c, B.pq[sl][:, :], hx), reads=mm_reads(s, hx),
                     writes=[("PS", "bk", sl)])
            else:
                P.pe(lambda e, s=s, c=c, sl=sl: mm_fn(e, s, c, B.pq[sl][:, :]), reads=mm_reads(s), writes=[("PS", "bk", sl)])
        P.dve(lambda e, sl=sl, cs=cs: e.tensor_tensor(B.ua[sl][:], B.pq[sl][:, :], Ctab[:, cs], ALU.mult),
              reads=[("PS", "bk", sl), "ropeC"], writes=[("ua", sl)])
        P.dve(lambda e, sl=sl, cs=cs: e.tensor_tensor(B.ub[sl][:], B.pq[sl][:, :], Stab[:, cs], ALU.mult),
              reads=[("PS", "bk", sl), "ropeS"], writes=[("ub", sl)])

    def stage2(s):
        sl = s % 2
        P.pe(lambda e, sl=sl: e.matmul(B.psw[sl][:, :], lhsT=idb[:], rhs=B.ua[sl][:], start=True, stop=False),
             reads=[("ua", sl), "idb"], writes=[("PS", "bk", 2 + sl)])
        P.pe(lambda e, sl=sl: e.matmul(B.psw[sl][:, :], lhsT=psw[:], rhs=B.ub[sl][:], start=False, stop=True),
             reads=[("ub", sl), "psw"], writes=[("PS", "bk", 2 + sl)])
        for oap, rows in out_fn(s):
            P.act(lambda e, sl=sl, oap=oap, rows=rows: e.copy(oap, B.psw[sl][rows, :]), reads=[("PS", "bk", 2 + sl)],
                  writes=[out_key])

    for i in range(nslab + L):
        if i < nslab:
            stage1(i)
        if i >= L:
            stage2(i - L)


def attention(P, nc, B, tag, dil, kt_fn, qt_fn, v_fn, kv_reads, q_reads, mask, evac_fn, idb, LOOK=3):
    nsub = T // dil
    nb = nsub // 128
    blocks = [(r, n) for r in range(dil) for n in range(nb)]

    def s1(idx):
        r, n = blocks[idx]
        nq = 256 if n < nb - 1 else 128
        sl = idx % 4
        stv = B.st[sl][:, :].rearrange("p (h q) -> p h q", h=2)[:, :, 0:nq]
        ptv = B.pt[sl][:, :, 0:nq]
        if nq == 256:
            P.pe(lambda e: e.matmul(stv, lhsT=kt_fn(r, 128 * n, 128), rhs=qt_fn(r, 128 * n, nq),
                                    start=True, stop=True), reads=kv_reads + q_reads, writes=[("PS", "bk", sl)])
        else:
            for hh in range(2):
                P.pe(lambda e, hh=hh: e.matmul(stv[:, hh, :], lhsT=kt_fn(r, 128 * n, 128), rhs=qt_fn(r, 128 * n, nq)[:, hh, :],
                                               start=True, stop=True), reads=kv_reads + q_reads, writes=[("PS", "bk", sl)])
        P.act(lambda e: e.activation(out=ptv, in_=stv, func=AF.Exp, scale=0.125),
              reads=[("PS", "bk", sl)], writes=[("ptile", sl)])
        P.dve(lambda e: e.tensor_tensor(ptv, ptv, mask[:, 0:nq].unsqueeze(1).to_broadcast([128, 2, nq]), ALU.mult),
              reads=[("ptile", sl), "masks"], writes=[("ptile", sl)])

    def s2(idx):
        r, n = blocks[idx]
        b = r * nb + n
        sl = idx % 4
        bank, pos = (b // 4) % 2, b % 4
        for hh in range(2):
            kb = 4 + hh * 2 + bank
            P.pe(lambda e, hh=hh, kb=kb: e.matmul(B.bk[kb][:, pos * 128:(pos + 1) * 128], lhsT=v_fn(hh, b),
                                                  rhs=B.pt[sl][:, hh, 0:128], start=(n == 0), stop=True),
                 reads=kv_reads + [("ptile", sl)], writes=[("PS", "bk", kb)])
            if n < nb - 1:
                b2 = b + 1
                bank2, pos2 = (b2 // 4) % 2, b2 % 4
                kb2 = 4 + hh * 2 + bank2
                P.pe(lambda e, hh=hh, kb2=kb2, pos2=pos2: e.matmul(
                    B.bk[kb2][:, pos2 * 128:(pos2 + 1) * 128], lhsT=v_fn(hh, b), rhs=B.pt[sl][:, hh, 128:256],
                    start=True, stop=False), reads=kv_reads + [("ptile", sl)], writes=[("PS", "bk", kb2)])
            if pos == 3:
                evac_fn(hh, b // 4, B.bk[kb], ("PS", "bk", kb))

    nblk = len(blocks)
    for i in range(nblk + LOOK):
        if i < nblk:
            s1(i)
        if i >= LOOK:
            s2(i - LOOK)


def v_from_vt(P, B, dil, VTs, vkey):
    nb = T // dil // 128
    vv = tokview(VTs[:, :], dil)
    for b in range(32):
        r, n = b // nb, b % nb
        half, pos = (b // 4) % 2, b % 4
        P.pe(lambda e, r=r, n=n, half=half, pos=pos: e.transpose(
            B.vtr[half][:, pos, :], vv[:, r, 128 * n:128 * (n + 1)], B.idb[:]),
            reads=[vkey, "idb"], writes=[("PS", "bk", 2 + half)])
        if pos == 3:
            b0 = b - 3
            P.dve(lambda e, b0=b0, half=half: e.tensor_copy(
                B.V[:, b0:b0 + 4, :, 0:64], B.vtr[half].rearrange("p b (h d) -> p b h d", h=2)),
                reads=[("PS", "bk", 2 + half)], writes=["V"])


def attn_buffers(nc, es, tag):
    sb = lambda n, s, d: es.enter_context(_sbt(nc, f"{tag}_{n}", s, d))
    ps = lambda n, s, d: es.enter_context(_pst(nc, f"{tag}_{n}", s, d))
    B = Ctx()
    bk = [ps(f"bk{i}", [128, 512], F32) for i in range(8)]
    B.bk = bk
    B.pq = [bk[0], bk[1]]
    B.psw = [bk[2], bk[3]]
    B.st = [bk[i] for i in range(4)]
    B.vtr = [bk[2 + i][:, :].bitcast(BF16)[:, 0:512].rearrange("p (b d) -> p b d", b=4) for i in range(2)]
    B.ua = [sb(f"ua{i}", [128, 512], BF16) for i in range(2)]
    B.ub = [sb(f"ub{i}", [128, 512], BF16) for i in range(2)]
    B.pt = [sb(f"ptl{i}", [128, 2, 256], BF16) for i in range(4)]
    B.ropeC = sb("ropeC", [128, T], BF16)
    B.ropeS = sb("ropeS", [128, T], BF16)
    B.V = sb("V", [128, 32, 2, 128], BF16)
    return B


def load_rope(P, g, B):
    for h in range(2):
        cs = slice(h * 2048, (h + 1) * 2048)
        P.dma("pool", lambda e, cs=cs: e.dma_start(out=B.ropeC[:, cs], in_=g.ropeC[:, cs]), writes=["ropeC"],
              semkey=("ropeC", h))
        P.dma("pool", lambda e, cs=cs: e.dma_start(out=B.ropeS[:, cs], in_=g.ropeS[:, cs]), writes=["ropeS"],
              semkey=("ropeS", h))


def phase_A(P, nc, g, C):
    with ExitStack() as es:
        sb = lambda n, s, d: es.enter_context(_sbt(nc, n, s, d))
        hT0 = sb("A_hT0", [128, 8, T], BF16)
        with ExitStack() as es2:
            sb2 = lambda n, s, d: es2.enter_context(_sbt(nc, n, s, d))
            xt = [sb2(f"A_xt{i}", [128, D], F32) for i in range(3)]
            NU = NormUnit(nc, es2, P, "An", C.idb, C.gT)
            for t in range(NT):
                s = t % 3
                P.dma("sp", lambda e, t=t, s=s: e.dma_start(out=xt[s][:], in_=g.x[t * 128:(t + 1) * 128, :]),
                      writes=[("A_xt", s)], semkey=("A_xt", s))
                NU.run(xt[s][:], ("A_xt", s), [(0, hT0[:, :, t * 128:(t + 1) * 128], "hT0")])
            P.fence()
            P.flush(nc)
        B = attn_buffers(nc, es, "A")
        B.idb = C.idb
        load_rope(P, g, B)
        wq = [sb(f"A_wq{i}", [128, 8, 384], BF16) for i in range(2)]
        QTd = sb("A_QTd", [128, 2, T], BF16)
        KT = sb("A_KT", [128, T], BF16)
        VTs = sb("A_VTs", [128, T], BF16)
        P.pool(lambda e: e.memset(QTd[:], 0.0), writes=["QT"])
        acc = [sb(f"A_acc{i}", [128, T], F32) for i in range(2)]
        rec = sb("A_rec", [128, 512], F32)
        lnd = sb("A_lnd", [128, 512], F32)
        obf = sb("A_obf", [128, T], BF16)
        P.pool(lambda e: e.memset(B.V[:, :, :, 64:128], 1.0), writes=["V"])
        wv_ = g.wqkv.rearrange("(c p) n -> p c n", p=128)
        it = 0
        for pair in range(STOP.get("a_pairs", 8)):
            for gi, (win, dil) in enumerate(A_CONFIGS):
                ws = it % 2
                it += 1
                if it > STOP.get("a_iters", 99):
                    continue
                for k in range(3):
                    c0 = gi * 3072 + k * 1024 + pair * 128
                    P.dma("pool", lambda e, ws=ws, k=k, c0=c0: e.dma_start(
                        out=wq[ws][:, :, k * 128:(k + 1) * 128], in_=wv_[:, :, c0:c0 + 128]),
                        writes=[("wq", ws)], semkey=("wq", ws, k))
                nb = T // dil // 128
                rope_proj(P, nc, B, "A", 8,
                          lambda e, s, c, o, ws=ws: e.matmul(o, lhsT=wq[ws][:, c, 0:128], rhs=hT0[:, c, s * 512:(s + 1) * 512],
                                                             start=(c == 0), stop=(c == 7)),
                          lambda s, ws=ws: [("wq", ws), "hT0"], B.ropeC, B.ropeS,
                          lambda s: [(QTd[0:64, 0, s * 512:(s + 1) * 512], slice(0, 64)),
                                     (QTd[64:128, 1, s * 512:(s + 1) * 512], slice(64, 128))], "QT", C.psw, C.idb)
                rope_proj(P, nc, B, "A", 8,
                          lambda e, s, c, o, ws=ws: e.matmul(o, lhsT=wq[ws][:, c, 128:256], rhs=hT0[:, c, s * 512:(s + 1) * 512],
                                                             start=(c == 0), stop=(c == 7)),
                          lambda s, ws=ws: [("wq", ws), "hT0"], B.ropeC, B.ropeS,
                          lambda s: [(KT[:, s * 512:(s + 1) * 512], slice(0, 128))], "KT", C.psw, C.idb)
                if STOP.get("a_part", 9) < 1:
                    continue
                for sv in range(8):
                    sl = sv % 2
                    for c in range(8):
                        P.pe(lambda e, c=c, sv=sv, sl=sl, ws=ws: e.matmul(
                            B.pq[sl][:, :], lhsT=wq[ws][:, c, 256:384], rhs=hT0[:, c, sv * 512:(sv + 1) * 512],
                            start=(c == 0), stop=(c == 7)), reads=[("wq", ws), "hT0"], writes=[("PS", "bk", sl)])
                    P.act(lambda e, sv=sv, sl=sl: e.copy(VTs[:, sv * 512:(sv + 1) * 512], B.pq[sl][:, :]),
                          reads=[("PS", "bk", sl)], writes=["VTs"])
                v_from_vt(P, B, dil, VTs, "VTs")
                if STOP.get("a_part", 9) < 2:
                    continue
                def evac(hh, k, pvb, pvkey, gi=gi, dil=dil, nb=nb):
                    accv = tokview(acc[hh][:, :], dil)
                    if nb >= 4:
                        r, n0 = (4 * k) // nb, (4 * k) % nb
                        dst = accv[:, r, 128 * n0:128 * n0 + 512]
                        src = pvb[:, :]
                    else:
                        dst = accv[:, 2 * k:2 * k + 2, 0:256]
                        src = pvb[:, :].rearrange("p (a b) -> p a b", a=2)
                    if gi == 0:
                        P.dve(lambda e: e.tensor_copy(dst, src), reads=[pvkey], writes=[("acc", hh)])
                    else:
                        P.dve(lambda e: e.tensor_tensor(dst, src, dst, ALU.add), reads=[pvkey, ("acc", hh)],
                              writes=[("acc", hh)])

                if dil == 1:
                    qtf = lambda r, n0, cnt: QTd[:, :, n0:n0 + cnt]
                else:
                    qtf = lambda r, n0, cnt, dil=dil: QTd[:, :, :].rearrange("p h (m d) -> p h d m", d=dil)[:, :, r, n0:n0 + cnt]
                attention(P, nc, B, "A", dil,
                          lambda r, n0, cnt, dil=dil: tokview(KT[:, :], dil)[:, r, n0:n0 + cnt],
                          qtf, lambda hh, b: B.V[:, b, hh, :],
                          ["KT", "V"], ["QT"], C.masks[:, 0, :], evac, C.idb)
            for hh in range(2):
                for s in range(8):
                    cs = slice(s * 512, (s + 1) * 512)
                    P.act(lambda e, hh=hh, cs=cs: e.activation(out=lnd[64:128, :], in_=acc[hh][64:128, cs], func=AF.Ln),
                          reads=[("acc", hh)], writes=["lnd"])
                    P.act(lambda e: e.activation(out=rec[0:64, :], in_=lnd[64:128, :], func=AF.Exp, scale=-1.0),
                          reads=["lnd"], writes=["rec"])
                    P.dve(lambda e, hh=hh, cs=cs: e.tensor_tensor(obf[hh * 64:(hh + 1) * 64, cs], acc[hh][0:64, cs],
                                                                   rec[0:64, :], ALU.mult),
                           reads=[("acc", hh), "rec"], writes=["obf"])
            P.dma("sp", lambda e, pair=pair: e.dma_start(out=g.oT[pair, :, :], in_=obf[:]), reads=["obf"],
                  writes=[("oT", pair)], semkey="obf_st")
        P.fence()
        P.flush(nc)


def phase_B(P, nc, g, C, wo_dram, x_src, gain_idx, h_dst, router=False):
    tag = "B%d" % gain_idx
    with ExitStack() as es:
        sb = lambda n, s, d: es.enter_context(_sbt(nc, f"{tag}_{n}", s, d))
        ps = lambda n, s, d: es.enter_context(_pst(nc, f"{tag}_{n}", s, d))
        wo = sb("wo", [128, 8, D], BF16)
        og = [sb(f"og{i}", [128, 8, 1024], BF16) for i in range(2)]
        xt = [sb(f"xt{i}", [128, D], F32) for i in range(3)]
        x1 = [sb(f"x1{i}", [128, D], F32) for i in range(4)]
        hg = [sb(f"hg{i}", [128, 8, 1024], BF16) for i in range(2)]
        py = [ps(f"py{i}", [128, D], F32) for i in range(2)]
        NU = NormUnit(nc, es, P, tag + "n", C.idb, C.gT, nslots=3)
        wov = wo_dram.rearrange("(c p) n -> p c n", p=128)
        for h in range(2):
            P.dma("pool", lambda e, h=h: e.dma_start(out=wo[:, :, h * 512:(h + 1) * 512], in_=wov[:, :, h * 512:(h + 1) * 512]),
                  writes=[(tag, "wo")], semkey=(tag, "wo", h))
        if router:
            wr32 = sb("wr32", [128, 8, NEXP], F32)
            wrg = sb("wrg", [128, 8, NEXP], F32)
            wrh = sb("wrh", [128, 8, NEXP], BF16)
            wrh32 = sb("wrh32", [128, 8, NEXP], F32)
            wrl = sb("wrl", [128, 8, NEXP], BF16)
            xlo = [sb(f"xlo{i}", [128, D], BF16) for i in range(2)]
            hiT = [sb(f"hiT{i}", [128, 8, 128], BF16) for i in range(2)]
            loT = [sb(f"loT{i}", [128, 8, 128], BF16) for i in range(2)]
            plo = ps("plo", [128, 8, 128], BF16)
            plg = ps("plg", [128, NEXP], F32)
            lg = sb("lg", [128, NEXP], F32)
            l2 = sb("l2", [128, NEXP], F32)
            eq1 = sb("eq1", [128, NEXP], F32)
            eq2 = sb("eq2", [128, NEXP], F32)
            sm = sb("sm", [128, 8], F32)
            P.dma("sp", lambda e: e.dma_start(out=wr32[:], in_=g.wr.rearrange("(c p) n -> p c n", p=128)),
                  writes=["wr32"], semkey="wr32")
            P.dve(lambda e: e.tensor_tensor(wrg[:], wr32[:], C.gT[:, gain_idx * 8:(gain_idx + 1) * 8].unsqueeze(2).to_broadcast([128, 8, NEXP]), ALU.mult),
                  reads=["wr32", "gT"], writes=["wrg"])
            P.dve(lambda e: e.tensor_copy(wrh[:], wrg[:]), reads=["wrg"], writes=["wrh"])
            P.dve(lambda e: e.tensor_copy(wrh32[:], wrh[:]), reads=["wrh"], writes=["wrh32"])
            P.dve(lambda e: e.tensor_tensor(wrl[:], wrg[:], wrh32[:], ALU.subtract), reads=["wrg", "wrh32"], writes=["wrl"])
        slots = {}

        def stage1(t):
            gq, tt = t // 8, t % 8
            gs = gq % 2
            if tt == 0:
                P.dma("sp", lambda e: e.dma_start(
                    out=og[gs][:], in_=g.oT[:, :, gq * 1024:(gq + 1) * 1024].rearrange("c p t -> p c t")),
                    reads=[("oT", c) for c in range(8)], writes=[(tag, "og", gs)], semkey=(tag, "og", gs))
            sx = t % 3
            s = t % 4
            P.dma("sp", lambda e: e.dma_start(out=xt[sx][:], in_=x_src[t * 128:(t + 1) * 128, :]),
                  reads=[("xs", t)], writes=[(tag, "xt", sx)], semkey=(tag, "xt", sx))
            pb = t % 2
            for half in range(2):
                for c in range(8):
                    P.pe(lambda e, c=c, half=half: e.matmul(
                        py[pb][:, half * 512:(half + 1) * 512], lhsT=og[gs][:, c, tt * 128:(tt + 1) * 128],
                        rhs=wo[:, c, half * 512:(half + 1) * 512], start=(c == 0), stop=(c == 7)),
                        reads=[(tag, "og", gs), (tag, "wo")], writes=[("PS", tag, "py", pb)])
            P.dve(lambda e: e.tensor_tensor(x1[s][:], py[pb][:, :], xt[sx][:], ALU.add),
                  reads=[("PS", tag, "py", pb), (tag, "xt", sx)], writes=[(tag, "x1", s)])
            P.dma("pool", lambda e: e.dma_start(out=g.xs[t * 128:(t + 1) * 128, :], in_=x1[s][:]),
                  reads=[(tag, "x1", s)], writes=[("xs", t)], semkey=(tag, "x1st", s))

        def stage1b(t):
            s = t % 4
            slots[t] = NU.run_a(x1[s][:], (tag, "x1", s))

        def stage2(t):
            gq, tt = t // 8, t % 8
            gs = gq % 2
            s = t % 4
            ns = slots[t]
            ps_ = NU.run_b(ns, [(gain_idx, hg[gs][:, :, tt * 128:(tt + 1) * 128], (tag, "hg", gs))])
            if router:
                rs = t % 2
                ntag = tag + "n"
                P.act(lambda e: e.copy(hiT[rs][:], NU.pt[ps_][:]), reads=[("PS", ntag, "pt", ps_)], writes=[("hiT", rs)])
                P.dve(lambda e: e.scalar_tensor_tensor(
                    out=xlo[rs][:], in0=x1[s][:], scalar=NU.rr[:, ns:ns + 1], in1=NU.xn[ns][:], op0=ALU.mult, op1=ALU.subtract),
                    reads=[(tag, "x1", s), (ntag, "rr", ns), (ntag, "xn", ns)], writes=[("xlo", rs)])
                for c in range(8):
                    P.pe(lambda e, c=c: e.transpose(plo[:, c, :], xlo[rs][:, c * 128:(c + 1) * 128], C.idb[:]),
                         reads=[("xlo", rs), "idb"], writes=[("PS", "plo")])
                P.act(lambda e: e.copy(loT[rs][:], plo[:]), reads=[("PS", "plo")], writes=[("loT", rs)])
                k = 0
                for (aT, akey, wmat, wkey) in ((hiT, "hiT", wrh, "wrh"), (loT, "loT", wrh, "wrh"), (hiT, "hiT", wrl, "wrl")):
                    for c in range(8):
                        P.pe(lambda e, c=c, aT=aT, wmat=wmat, k=k: e.matmul(
                            plg[:, :], lhsT=aT[rs][:, c, :], rhs=wmat[:, c, :], start=(k == 0), stop=(k == 23)),
                            reads=[(akey, rs), wkey], writes=[("PS", "plg")])
                        k += 1
                P.dve(lambda e: e.tensor_copy(lg[:], plg[:, :]), reads=[("PS", "plg")], writes=["lg"])
                P.dve(lambda e: e.reduce_max(sm[:, 0:1], lg[:], axis=AX.X), reads=["lg"], writes=["sm0"])
                P.dve(lambda e: e.tensor_scalar(eq1[:], lg[:], sm[:, 0:1], None, ALU.is_equal), reads=["lg", "sm0"], writes=["eq1"])
                P.dve(lambda e: e.scalar_tensor_tensor(out=l2[:], in0=eq1[:], scalar=-1e30, in1=lg[:], op0=ALU.mult, op1=ALU.add),
                      reads=["eq1", "lg"], writes=["l2"])
                P.dve(lambda e: e.reduce_max(sm[:, 1:2], l2[:], axis=AX.X), reads=["l2"], writes=["sm1"])
                P.dve(lambda e: e.tensor_scalar(eq2[:], l2[:], sm[:, 1:2], None, ALU.is_equal), reads=["l2", "sm1"], writes=["eq2"])
                P.dve(lambda e: e.tensor_tensor(sm[:, 2:3], sm[:, 1:2], sm[:, 0:1], ALU.subtract), reads=["sm0", "sm1"], writes=["sm2"])
                P.act(lambda e: e.activation(out=sm[:, 3:4], in_=sm[:, 2:3], func=AF.Exp), reads=["sm2"], writes=["sm3"])
                P.dve(lambda e: e.tensor_scalar(sm[:, 4:5], sm[:, 3:4], 1.0, None, ALU.add), reads=["sm3"], writes=["sm4"])
                P.dve(lambda e: e.reciprocal(sm[:, 5:6], sm[:, 4:5]), reads=["sm4"], writes=["sm5"])
                P.dve(lambda e: e.tensor_tensor(sm[:, 6:7], sm[:, 3:4], sm[:, 5:6], ALU.mult), reads=["sm3", "sm5"], writes=["sm6"])
                P.dve(lambda e: e.tensor_scalar(eq1[:], eq1[:], sm[:, 5:6], None, ALU.mult), reads=["eq1", "sm5"], writes=["eq1"])
                P.dve(lambda e: e.scalar_tensor_tensor(out=C.gates[:, t, :], in0=eq2[:], scalar=sm[:, 6:7], in1=eq1[:],
                                                       op0=ALU.mult, op1=ALU.add),
                      reads=["eq2", "sm6", "eq1"], writes=["gates"])
            if tt == 7:
                P.dma("pool", lambda e: e.dma_start(out=h_dst[:, :, gq * 1024:(gq + 1) * 1024], in_=hg[gs][:]),
                      reads=[(tag, "hg", gs)], writes=[("hsc", gq)], semkey=(tag, "hgst", gs))

        for i in range(NT + 2):
            if i < NT:
                stage1(i)
            if 0 <= i - 1 < NT:
                stage1b(i - 1)
            if 0 <= i - 2 < NT:
                stage2(i - 2)
        P.fence()
        P.flush(nc)


def ffn_stage(P, nc, tag, hTg, hkey, nchunk, wsrc1, wsrc3, wb1, wb3, pa, pb, sa, actT, wit):
    nblk = (nchunk + 3) // 4
    cnt = 0
    for fb in range(nblk):
        ncols = min(512, nchunk * 128 - fb * 512)
        ws = wit["w"] % 2
        wit["w"] += 1
        P.dma("pool", lambda e, fb=fb, ncols=ncols, ws=ws: e.dma_start(out=wb1[ws][:, :, 0:ncols], in_=wsrc1(fb * 512, ncols)),
              writes=[(tag, "wb1", ws)], semkey=(tag, "wb1", ws))
        P.dma("pool", lambda e, fb=fb, ncols=ncols, ws=ws: e.dma_start(out=wb3[ws][:, :, 0:ncols], in_=wsrc3(fb * 512, ncols)),
              writes=[(tag, "wb3", ws)], semkey=(tag, "wb3", ws))
        for jj in range(ncols // 128):
            j = fb * 4 + jj
            for th in range(2):
                sl = wit["p"] % 2
                wit["p"] += 1
                ts_ = slice(th * 512, (th + 1) * 512)
                for c in range(8):
                    P.pe(lambda e, c=c, jj=jj, ws=ws, sl=sl, ts_=ts_: e.matmul(
                        pa[sl][:, :], lhsT=wb1[ws][:, c, jj * 128:(jj + 1) * 128], rhs=hTg[:, c, ts_],
                        start=(c == 0), stop=(c == 7)), reads=[(tag, "wb1", ws), hkey], writes=[("PS", tag, "pa", sl)])
                for c in range(8):
                    P.pe(lambda e, c=c, jj=jj, ws=ws, sl=sl, ts_=ts_: e.matmul(
                        pb[sl][:, :], lhsT=wb3[ws][:, c, jj * 128:(jj + 1) * 128], rhs=hTg[:, c, ts_],
                        start=(c == 0), stop=(c == 7)), reads=[(tag, "wb3", ws), hkey], writes=[("PS", tag, "pb", sl)])
                P.act(lambda e, sl=sl: e.activation(out=sa[sl][:], in_=pa[sl][:, :], func=AF.Silu),
                      reads=[("PS", tag, "pa", sl)], writes=[(tag, "sa", sl)])
                P.dve(lambda e, sl=sl, j=j, ts_=ts_: e.tensor_tensor(actT[:, j, ts_], sa[sl][:], pb[sl][:, :], ALU.mult),
                      reads=[(tag, "sa", sl), ("PS", tag, "pb", sl)], writes=[(tag, "actT")])


def phase_C1(P, nc, g, C):
    tag = "C1"
    NCH = DFF // 128
    with ExitStack() as es:
        sb = lambda n, s, d: es.enter_context(_sbt(nc, f"{tag}_{n}", s, d))
        ps = lambda n, s, d: es.enter_context(_pst(nc, f"{tag}_{n}", s, d))
        w2 = sb("w2", [128, NCH, D], BF16)
        hTg = [sb(f"hTg{i}", [128, 8, 1024], BF16) for i in range(2)]
        actT = sb("actT", [128, NCH, 1024], BF16)
        wb1 = [sb(f"wb1{i}", [128, 8, 512], BF16) for i in range(2)]
        wb3 = [sb(f"wb3{i}", [128, 8, 512], BF16) for i in range(2)]
        sa = [sb(f"sa{i}", [128, 512], BF16) for i in range(2)]
        xt = [sb(f"xt{i}", [128, D], F32) for i in range(3)]
        pa = [ps(f"pa{i}", [128, 512], F32) for i in range(2)]
        pb = [ps(f"pb{i}", [128, 512], F32) for i in range(2)]
        py = [ps(f"py{i}", [128, D], F32) for i in range(2)]
        w2v = g.w2d.rearrange("(j p) n -> p j n", p=128)
        for q in range(0, NCH, 4):
            n = min(4, NCH - q)
            P.dma("pool", lambda e, q=q, n=n: e.dma_start(out=w2[:, q:q + n, :], in_=w2v[:, q:q + n, :]),
                  writes=[(tag, "w2")], semkey=(tag, "w2", q))
        w1v = g.w1d.rearrange("(c p) n -> p c n", p=128)
        w3v = g.w3d.rearrange("(c p) n -> p c n", p=128)
        wit = {"w": 0, "p": 0}
        for gq in range(4):
            gs = gq % 2
            P.dma("sp", lambda e, gq=gq, gs=gs: e.dma_start(out=hTg[gs][:], in_=g.hsc[:, :, gq * 1024:(gq + 1) * 1024]),
                  reads=[("hsc", gq)], writes=[(tag, "hTg", gs)], semkey=(tag, "hTg", gs))
            ffn_stage(P, nc, tag, hTg[gs], (tag, "hTg", gs), NCH,
                      lambda c0, n: w1v[:, :, c0:c0 + n], lambda c0, n: w3v[:, :, c0:c0 + n],
                      wb1, wb3, pa, pb, sa, actT, wit)
            for tt in range(8):
                t = gq * 8 + tt
                s = t % 3
                pbk = t % 2
                P.dma("sp", lambda e, t=t, s=s: e.dma_start(out=xt[s][:], in_=g.xs[t * 128:(t + 1) * 128, :]),
                      reads=[("xs", t)], writes=[(tag, "xt", s)], semkey=(tag, "xt", s))
                for half in range(2):
                    for j in range(NCH):
                        P.pe(lambda e, j=j, half=half, tt=tt, pbk=pbk: e.matmul(
                            py[pbk][:, half * 512:(half + 1) * 512], lhsT=actT[:, j, tt * 128:(tt + 1) * 128],
                            rhs=w2[:, j, half * 512:(half + 1) * 512], start=(j == 0), stop=(j == NCH - 1)),
                            reads=[(tag, "actT"), (tag, "w2")], writes=[("PS", tag, "py", pbk)])
                P.dve(lambda e, s=s, pbk=pbk: e.tensor_tensor(xt[s][:], py[pbk][:, :], xt[s][:], ALU.add),
                      reads=[("PS", tag, "py", pbk), (tag, "xt", s)], writes=[(tag, "xt", s)])
                P.dma("sp", lambda e, t=t, s=s: e.dma_start(out=g.xs[t * 128:(t + 1) * 128, :], in_=xt[s][:]),
                      reads=[(tag, "xt", s)], writes=[("xs", t)], semkey=(tag, "xst", s))
        P.fence()
        P.flush(nc)


def phase_PLE(P, nc, g, C, layer, gain_idx, final):
    tag = "E%d" % layer
    with ExitStack() as es:
        sb = lambda n, s, d: es.enter_context(_sbt(nc, f"{tag}_{n}", s, d))
        ps = lambda n, s, d: es.enter_context(_pst(nc, f"{tag}_{n}", s, d))
        wg = sb("wg", [128, 8, D], BF16)
        wp = sb("wp", [128, 2, D], BF16)
        pTg = [sb(f"pTg{i}", [128, 2, 1024], BF16) for i in range(2)]
        NXT = 5
        xt = [sb(f"xt{i}", [128, D], F32) for i in range(NXT)]
        gTt = [sb(f"gTt{i}", [128, 8, 128], BF16) for i in range(3)]
        sg = [sb(f"sg{i}", [128, 512], F32) for i in range(2)]
        pg = [ps(f"pg{i}", [128, 512], F32) for i in range(2)]
        pp = [ps(f"pp{i}", [128, 512], F32) for i in range(2)]
        NU = NormUnit(nc, es, P, tag + "n", C.idb, C.gT, nslots=3)
        NU2 = NormUnit(nc, es, P, tag + "m", C.idb, C.gT, nslots=3)
        if final:
            fg = sb("fg", [128, D], F32)
            ot = [sb(f"ot{i}", [128, D], F32) for i in range(2)]
            P.dma("sp", lambda e: e.dma_start(out=fg[:], in_=g.fgain[:, :]), writes=[(tag, "fg")], semkey=(tag, "fg"))
        else:
            hk = [sb(f"hk{i}", [128, 8, 1024], BF16) for i in range(2)]
            ha = [sb(f"ha{i}", [128, 8, 1024], BF16) for i in range(2)]
        wgv = g.wg[layer].rearrange("(c p) n -> p c n", p=128)
        for h in range(2):
            P.dma("pool", lambda e, h=h: e.dma_start(out=wg[:, :, h * 512:(h + 1) * 512], in_=wgv[:, :, h * 512:(h + 1) * 512]),
                  writes=[(tag, "wg")], semkey=(tag, "wg", h))
        P.dma("pool", lambda e: e.dma_start(out=wp[:], in_=g.wp[layer].rearrange("(c p) n -> p c n", p=128)),
              writes=[(tag, "wp")], semkey=(tag, "wp"))
        sl1, sl2 = {}, {}
        hcnt = {"n": 0}

        def s1(t):
            gq, tt = t // 8, t % 8
            gs = gq % 2
            if tt == 0:
                P.dma("pool", lambda e: e.dma_start(
                    out=pTg[gs][:], in_=g.pT[layer, :, gq * 1024:(gq + 1) * 1024].rearrange("(c p) t -> p c t", p=128)),
                    writes=[(tag, "pTg", gs)], semkey=(tag, "pTg", gs))
            s = t % NXT
            P.dma("sp", lambda e: e.dma_start(out=xt[s][:], in_=g.xs[t * 128:(t + 1) * 128, :]),
                  reads=[("xs", t)], writes=[(tag, "xt", s)], semkey=(tag, "xt", s))
            sl1[t] = NU.run_a(xt[s][:], (tag, "xt", s))

        def s2(t):
            s3_ = t % 3
            NU.run_b(sl1[t], [(gain_idx, gTt[s3_][:], (tag, "gTt", s3_))])

        def s3(t):
            gq, tt = t // 8, t % 8
            gs = gq % 2
            s = t % NXT
            s3_ = t % 3
            for half in range(2):
                hs_ = slice(half * 512, (half + 1) * 512)
                k = hcnt["n"] % 2
                hcnt["n"] += 1
                for c in range(8):
                    P.pe(lambda e, c=c, hs_=hs_, k=k: e.matmul(pg[k][:, :], lhsT=gTt[s3_][:, c, :], rhs=wg[:, c, hs_],
                                                              start=(c == 0), stop=(c == 7)),
                         reads=[(tag, "gTt", s3_), (tag, "wg")], writes=[("PS", tag, "pg", k)])
                for c in range(2):
                    P.pe(lambda e, c=c, hs_=hs_, k=k: e.matmul(pp[k][:, :], lhsT=pTg[gs][:, c, tt * 128:(tt + 1) * 128],
                                                              rhs=wp[:, c, hs_], start=(c == 0), stop=(c == 1)),
                         reads=[(tag, "pTg", gs), (tag, "wp")], writes=[("PS", tag, "pp", k)])
                P.act(lambda e, k=k: e.activation(out=sg[k][:], in_=pg[k][:, :], func=AF.Sigmoid),
                      reads=[("PS", tag, "pg", k)], writes=[(tag, "sg", k)])
                P.dve(lambda e, k=k: e.tensor_tensor(sg[k][:], sg[k][:], pp[k][:, :], ALU.mult),
                      reads=[(tag, "sg", k), ("PS", tag, "pp", k)], writes=[(tag, "sg", k)])
                P.dve(lambda e, k=k, hs_=hs_: e.tensor_tensor(xt[s][:, hs_], xt[s][:, hs_], sg[k][:], ALU.add),
                      reads=[(tag, "xt", s), (tag, "sg", k)], writes=[(tag, "xt", s)])
            xkey = (tag, "xt", s)
            if not final:
                P.dma("pool", lambda e: e.dma_start(out=g.xs[t * 128:(t + 1) * 128, :], in_=xt[s][:]),
                      reads=[xkey], writes=[("xs", t)], semkey=(tag, "xst", s))

        def s3b(t):
            s = t % NXT
            xkey = (tag, "xt", s)
            if final:
                s2_ = t % 2
                ns = NU2.stats(xt[s][:], xkey)
                P.dve(lambda e: e.scalar_tensor_tensor(out=ot[s2_][:], in0=xt[s][:], scalar=NU2.rr[:, ns:ns + 1], in1=fg[:],
                                                       op0=ALU.mult, op1=ALU.mult),
                      reads=[xkey, (tag + "m", "rr", ns), (tag, "fg")], writes=[(tag, "ot", s2_)])
                P.dma("pool", lambda e: e.dma_start(out=g.out[t * 128:(t + 1) * 128, :], in_=ot[s2_][:]),
                      reads=[(tag, "ot", s2_)], writes=[("out", t)], semkey=(tag, "ost", s2_))
            else:
                sl2[t] = NU2.run_a(xt[s][:], xkey)

        def s4(t):
            if final:
                return
            gq, tt = t // 8, t % 8
            gs = gq % 2
            NU2.run_b(sl2[t], [(3, hk[gs][:, :, tt * 128:(tt + 1) * 128], (tag, "hk", gs)),
                               (4, ha[gs][:, :, tt * 128:(tt + 1) * 128], (tag, "ha", gs))])
            if tt == 7:
                cs = slice(gq * 1024, (gq + 1) * 1024)
                P.dma("pool", lambda e: e.dma_start(out=g.hkv[:, :, cs], in_=hk[gs][:]), reads=[(tag, "hk", gs)],
                      writes=[("hkv", gq)], semkey=(tag, "hkst", gs))
                P.dma("pool", lambda e: e.dma_start(out=g.hat[:, :, cs], in_=ha[gs][:]), reads=[(tag, "ha", gs)],
                      writes=[("hat", gq)], semkey=(tag, "hast", gs))

        for i in range(NT + 4):
            if i < NT:
                s1(i)
            if 0 <= i - 1 < NT:
                s2(i - 1)
            if 0 <= i - 2 < NT:
                s3(i - 2)
            if 0 <= i - 3 < NT:
                s3b(i - 3)
            if 0 <= i - 4 < NT:
                s4(i - 4)
        P.fence()
        P.flush(nc)


def phase_D(P, nc, g, C):
    tag = "D"
    with ExitStack() as es:
        sb = lambda n, s, d: es.enter_context(_sbt(nc, f"{tag}_{n}", s, d))
        B = attn_buffers(nc, es, "D")
        B.idb = C.idb
        load_rope(P, g, B)
        wk2 = sb("wk2", [128, 8, 2, 128], BF16)
        wv = sb("wv", [128, 8, 128], BF16)
        wq = sb("wq", [128, 8, D], BF16)
        KT2 = [sb(f"KT{i}", [128, T], BF16) for i in range(2)]
        QTd = [sb(f"QTd{i}", [128, 2, T], BF16) for i in range(2)]
        VTs = sb("VTs", [128, T], BF16)
        for i in range(2):
            P.pool(lambda e, i=i: e.memset(QTd[i][:], 0.0), writes=[("QT", i)])
        obf = [sb(f"obf{i}", [128, T], BF16) for i in range(2)]
        hs = [sb(f"hs{i}", [128, 8, 512], BF16) for i in range(6)]
        esk = sb("esk", [128, 16], F32)
        tmp = sb("tmp", [128, 512], F32)
        rec = sb("rec", [128, 512], F32)
        kvv = g.kvw.rearrange("(c p) n -> p c n", p=128)
        for kvh in range(2):
            for dup in range(2):
                P.dma("pool", lambda e, kvh=kvh, dup=dup: e.dma_start(
                    out=wk2[:, :, kvh, dup * 64:(dup + 1) * 64], in_=kvv[:, :, kvh * 64:(kvh + 1) * 64]),
                    writes=["wk2"], semkey=("wk2", kvh, dup))
        P.dma("pool", lambda e: e.dma_start(out=wv[:], in_=kvv[:, :, 128:256]), writes=["wv"], semkey="wv")
        wqv = g.wq1.rearrange("(c p) n -> p c n", p=128)
        for h in range(2):
            P.dma("pool", lambda e, h=h: e.dma_start(out=wq[:, :, h * 512:(h + 1) * 512], in_=wqv[:, :, h * 512:(h + 1) * 512]),
                  writes=["wq1"], semkey=("wq1", h))
        P.dma("sp", lambda e: e.dma_start(out=esk[:], in_=g.sinks[:, :]), writes=["esk"], semkey="esk")
        P.act(lambda e: e.activation(out=esk[:], in_=esk[:], func=AF.Exp), reads=["esk"], writes=["esk"])
        P.pool(lambda e: e.memset(B.V[:, :, :, 64:128], 1.0), writes=["V"])
        hst = {"cnt": 0, "slot": {}}

        def mk_pre(src, srckey):
            def pre(s_):
                sl = hst["cnt"] % 6
                hst["cnt"] += 1
                hst["slot"][s_] = sl
                P.dma("sp", lambda e: e.dma_start(out=hs[sl][:], in_=src[:, :, s_ * 512:(s_ + 1) * 512]),
                      reads=[(srckey, s_ // 2)], writes=[("hs", sl)], semkey=("hs", sl))
                return sl
            return pre

        for kvh in range(2):
            rope_proj(P, nc, B, "D", 8,
                      lambda e, s_, c, o, hx, kvh=kvh: e.matmul(o, lhsT=wk2[:, c, kvh, :], rhs=hs[hx][:, c, :],
                                                                start=(c == 0), stop=(c == 7)),
                      lambda s_, hx: ["wk2", ("hs", hx)], B.ropeC, B.ropeS,
                      lambda s_, kvh=kvh: [(KT2[kvh][:, s_ * 512:(s_ + 1) * 512], slice(0, 128))], ("KT", kvh), C.psw, C.idb,
                      pre=mk_pre(g.hkv, "hkv"))
        prev = mk_pre(g.hkv, "hkv")
        for sv in range(8):
            hx = prev(sv)
            sl = sv % 2
            for c in range(8):
                P.pe(lambda e, c=c, sl=sl, hx=hx: e.matmul(B.pq[sl][:, :], lhsT=wv[:, c, :], rhs=hs[hx][:, c, :],
                                                          start=(c == 0), stop=(c == 7)),
                     reads=[("hs", hx), "wv"], writes=[("PS", "bk", sl)])
            P.act(lambda e, sv=sv, sl=sl: e.copy(VTs[:, sv * 512:(sv + 1) * 512], B.pq[sl][:, :]),
                  reads=[("PS", "bk", sl)], writes=["VTs"])
        v_from_vt(P, B, 1, VTs, "VTs")
        for pair in range(8):
            qs = pair % 2
            rope_proj(P, nc, B, "D", 8,
                      lambda e, s_, c, o, hx, pair=pair: e.matmul(o, lhsT=wq[:, c, pair * 128:(pair + 1) * 128],
                                                                  rhs=hs[hx][:, c, :], start=(c == 0), stop=(c == 7)),
                      lambda s_, hx: ["wq1", ("hs", hx)], B.ropeC, B.ropeS,
                      lambda s_, qs=qs: [(QTd[qs][0:64, 0, s_ * 512:(s_ + 1) * 512], slice(0, 64)),
                                         (QTd[qs][64:128, 1, s_ * 512:(s_ + 1) * 512], slice(64, 128))], ("QT", qs), C.psw, C.idb,
                      pre=mk_pre(g.hat, "hat"))
            kvh = pair // 4

            def evac(hh, k, pvb, pvkey, pair=pair, qs=qs):
                head = pair * 2 + hh
                cs = slice(k * 512, (k + 1) * 512)
                P.act(lambda e: e.activation(out=tmp[64:128, :], in_=pvb[64:128, :], func=AF.Ln, bias=esk[64:128, head:head + 1]),
                      reads=[pvkey, "esk"], writes=["tmp"])
                P.act(lambda e: e.activation(out=rec[0:64, :], in_=tmp[64:128, :], func=AF.Exp, scale=-1.0),
                      reads=["tmp"], writes=["rec"])
                P.dve(lambda e: e.tensor_tensor(obf[qs][hh * 64:(hh + 1) * 64, cs], pvb[0:64, :], rec[0:64, :], ALU.mult),
                      reads=[pvkey, "rec"], writes=[("obf", qs)])

            attention(P, nc, B, "D", 1,
                      lambda r, n0, cnt, kvh=kvh: KT2[kvh][:, n0:n0 + cnt],
                      lambda r, n0, cnt, qs=qs: QTd[qs][:, :, n0:n0 + cnt],
                      lambda hh, b, kvh=kvh: B.V[:, b, kvh, :],
                      [("KT", kvh), "V"], [("QT", qs)], C.masks[:, 1, :], evac, C.idb)
            P.dma("pool", lambda e, pair=pair, qs=qs: e.dma_start(out=g.oT[pair, :, :], in_=obf[qs][:]), reads=[("obf", qs)],
                  writes=[("oT", pair)], semkey=("obf_st1", qs))
        P.fence()
        P.flush(nc)


def phase_F1(P, nc, g, C):
    tag = "F1"
    NH = 14
    with ExitStack() as es:
        sb = lambda n, s, d: es.enter_context(_sbt(nc, f"{tag}_{n}", s, d))
        ps = lambda n, s, d: es.enter_context(_pst(nc, f"{tag}_{n}", s, d))
        xg = sb("xg", [128, 8, D], F32)
        hTg = [sb(f"hTg{i}", [128, 8, 1024], BF16) for i in range(2)]
        actT = sb("actT", [128, NH, 1024], BF16)
        w2h = [sb(f"w2h{i}", [128, NH, D], BF16) for i in range(2)]
        wb1 = [sb(f"wb1{i}", [128, 8, 512], BF16) for i in range(2)]
        wb3 = [sb(f"wb3{i}", [128, 8, 512], BF16) for i in range(2)]
        sa = [sb(f"sa{i}", [128, 512], BF16) for i in range(2)]
        pa = [ps(f"pa{i}", [128, 512], F32) for i in range(2)]
        pb = [ps(f"pb{i}", [128, 512], F32) for i in range(2)]
        py = [ps(f"py{i}", [128, D], F32) for i in range(2)]
        wit = {"w": 0, "p": 0}
        it = 0
        pyc = 0
        for gq in range(4):
            gs = gq % 2
            P.dma("sp", lambda e, gq=gq, gs=gs: e.dma_start(out=hTg[gs][:], in_=g.hsc[:, :, gq * 1024:(gq + 1) * 1024]),
                  reads=[("hsc", gq)], writes=[(tag, "hTg", gs)], semkey=(tag, "hTg", gs))
            for q in range(2):
                P.dma("sp", lambda e, gq=gq, q=q: e.dma_start(
                    out=xg[:, q * 4:(q + 1) * 4, :],
                    in_=g.xs[gq * 1024 + q * 512:gq * 1024 + (q + 1) * 512, :].rearrange("(t p) n -> p t n", p=128)),
                    reads=[("xs", gq * 8 + q * 4 + i) for i in range(4)], writes=[(tag, "xg")], semkey=(tag, "xg", q))
            for ex in range(NEXP):
                for hf in range(2):
                    w2s = it % 2
                    it += 1
                    f0 = hf * NH * 128
                    w2v = g.w2m[ex].rearrange("(j p) n -> p j n", p=128)
                    for q in range(0, NH, 7):
                        P.dma("pool", lambda e, q=q, w2s=w2s, w2v=w2v, hf=hf: e.dma_start(
                            out=w2h[w2s][:, q:q + 7, :], in_=w2v[:, hf * NH + q:hf * NH + q + 7, :]),
                            writes=[(tag, "w2h", w2s)], semkey=(tag, "w2h", w2s, q))
                    w1v = g.w1m[ex].rearrange("(c p) n -> p c n", p=128)
                    w3v = g.w3m[ex].rearrange("(c p) n -> p c n", p=128)
                    ffn_stage(P, nc, tag, hTg[gs], (tag, "hTg", gs), NH,
                              lambda c0, n, w1v=w1v, f0=f0: w1v[:, :, f0 + c0:f0 + c0 + n],
                              lambda c0, n, w3v=w3v, f0=f0: w3v[:, :, f0 + c0:f0 + c0 + n],
                              wb1, wb3, pa, pb, sa, actT, wit)
                    for tt in range(8):
                        t = gq * 8 + tt
                        pbk = pyc % 2
                        pyc += 1
                        for half in range(2):
                            for j in range(NH):
                                P.pe(lambda e, j=j, half=half, tt=tt, pbk=pbk, w2s=w2s: e.matmul(
                                    py[pbk][:, half * 512:(half + 1) * 512], lhsT=actT[:, j, tt * 128:(tt + 1) * 128],
                                    rhs=w2h[w2s][:, j, half * 512:(half + 1) * 512], start=(j == 0), stop=(j == NH - 1)),
                                    reads=[(tag, "actT"), (tag, "w2h", w2s)], writes=[("PS", tag, "py", pbk)])
                        P.dve(lambda e, tt=tt, t=t, pbk=pbk, ex=ex: e.scalar_tensor_tensor(
                            out=xg[:, tt, :], in0=py[pbk][:, :], scalar=C.gates[:, t, ex:ex + 1], in1=xg[:, tt, :],
                            op0=ALU.mult, op1=ALU.add), reads=[("PS", tag, "py", pbk), "gates", (tag, "xg")], writes=[(tag, "xg")])
            for q in range(2):
                P.dma("sp", lambda e, gq=gq, q=q: e.dma_start(
                    out=g.xs[gq * 1024 + q * 512:gq * 1024 + (q + 1) * 512, :].rearrange("(t p) n -> p t n", p=128),
                    in_=xg[:, q * 4:(q + 1) * 4, :]),
                    reads=[(tag, "xg")], writes=[("xs", gq * 8 + q * 4 + i) for i in range(4)], semkey=(tag, "xgst", q))
        P.fence()
        P.flush(nc)


STOP = {"n": 99}
import os, json
if os.environ.get("KSTOP"):
    STOP.update(json.loads(os.environ["KSTOP"]))


def program(P, nc, g):
    with ExitStack() as es:
        C = load_consts(nc, es, P, g)
        stages = [
            lambda: phase_A(P, nc, g, C),
            lambda: phase_B(P, nc, g, C, g.wo0, g.x, 1, g.hsc),
            lambda: phase_C1(P, nc, g, C),
            lambda: phase_PLE(P, nc, g, C, 0, 2, False),
            lambda: phase_D(P, nc, g, C),
            lambda: phase_B(P, nc, g, C, g.wo1, g.xs, 5, g.hsc, router=True),
            lambda: phase_F1(P, nc, g, C),
            lambda: phase_PLE(P, nc, g, C, 1, 6, True),
        ]
        nst = min(STOP["n"], len(stages))
        for st in stages[:nst]:
            st()
        if nst < len(stages):
            for q in range(4):
                P.dma("sp", lambda e, q=q: e.dma_start(out=g.out[q * 1024:(q + 1) * 1024, :], in_=g.xs[q * 1024:(q + 1) * 1024, :]),
                      reads=[("xs", q * 8 + i) for i in range(8)], writes=[("out", q * 8 + i) for i in range(8)],
                      semkey=("dbg", q))
        P.add("sp", lambda e: e.nop(), reads=[("out", t) for t in range(NT)])
        P.flush(nc)


def build_nc():
    nc = bass.Bass("TRN2", target_bir_lowering=False)
    g = declare_dram(nc)
    P = Prog()
    program(P, nc, g)
    P.analyze()
    P.mode = "emit"
    P.count = 0
    with ExitStack() as es:
        P.alloc_sems(nc, es)
        program(P, nc, g)
    return nc


def _consts():
    half = 32
    inv = (1.0 / (np.float32(10000.0) ** (np.arange(half, dtype=np.float32) / np.float32(half)))).astype(np.float32)
    pos = np.arange(T, dtype=np.float32)
    ang = (pos[:, None] * inv[None, :]).astype(np.float32)
    cos = np.cos(ang).astype(np.float32).T
    sin = np.sin(ang).astype(np.float32).T
    Ct = np.ascontiguousarray(np.tile(cos, (4, 1)))
    sign = np.where((np.arange(128) % 64) < 32, -1.0, 1.0).astype(np.float32)[:, None]
    St = np.ascontiguousarray(np.tile(sin, (4, 1)) * (-sign))
    k = np.arange(128)[:, None]
    q = np.arange(128)[None, :]
    m_a = (q >= k)
    masks = np.stack([np.concatenate([m_a, q <= k], 1), np.concatenate([m_a, q < k], 1)]).astype(np.float32)
    ident = np.eye(128, dtype=np.float32)
    m = np.arange(128)
    sw = np.where((m % 64) < 32, m + 32, m - 32)
    pswap = np.zeros((128, 128), np.float32)
    pswap[sw, m] = 1.0
    return Ct, St, masks, ident, pswap


def make_in_maps(inp):
    f = lambda a: np.ascontiguousarray(np.asarray(a, dtype=np.float32))
    Ct, St, masks, ident, pswap = _consts()
    gl = [inp["attn_norm"][0], inp["ffn_norm"][0], inp["ple_norm"][0], inp["kv_norm"], inp["attn_norm"][1],
          inp["ffn_norm"][1], inp["ple_norm"][1]]
    gT = np.concatenate([f(v).reshape(8, 128).T for v in gl], axis=1)
    shared = dict(
        wqkv=f(inp["a_w_qkv"][0]), wo0=f(inp["a_w_o"][0]), kvw=f(inp["kv_w"]), wq1=f(inp["b_w_q"][0]),
        wo1=f(inp["b_w_o"][0]), w1d=f(inp["dense_w1"][0]), w3d=f(inp["dense_w3"][0]), w2d=f(inp["dense_w2"][0]),
        wr=f(inp["moe_router"][0]), w1m=f(inp["moe_w1"][0]), w3m=f(inp["moe_w3"][0]), w2m=f(inp["moe_w2"][0]),
        wg=f(inp["ple_w_gate"]), wp=f(inp["ple_w_proj"]), gT=f(gT),
        fgain=f(np.broadcast_to(f(inp["final_norm"])[None, :], (128, D))),
        sinks=f(np.broadcast_to(f(inp["b_sinks"][0])[None, :], (128, 16))),
        ropeC=Ct, ropeS=St, masks=masks, ident=ident, pswap=pswap)
    x = np.asarray(inp["x"], dtype=np.float32)
    p = np.asarray(inp["p"], dtype=np.float32)
    maps = []
    for b in range(x.shape[0]):
        m = dict(shared)
        m["x"] = f(x[b])
        m["pT"] = f(np.transpose(p[:, b], (0, 2, 1)))
        maps.append(m)
    return maps


_NC_CACHE = {}


def kernel(**inputs):
    maps = make_in_maps(inputs)
    if "nc" not in _NC_CACHE:
        _NC_CACHE["nc"] = build_nc()
    nc = _NC_CACHE["nc"]
    res = run_bass_kernel_spmd(nc, maps, core_ids=list(range(len(maps))))
    return np.stack([np.asarray(r["out"], dtype=np.float32) for r in res.results], axis=0)
```
